# Optimizing a Trainium2 kernel written in Bass

```python
import functools
import jax
import jax.numpy as jnp
from jax import lax
import numpy as np

D_MODEL = 1024
BATCH = 16
SEQ = 256
DEPTH = 2
DEC_BATCH = 8
DEC_SEQ = 2048
PAST_LEN = 256

GRID_W = 64
N_DIR = 2
A_HEADS = 16
A_HEAD_DIM = 64
A_WIDTH = A_HEADS * A_HEAD_DIM
DECAY_LORA = 64
ICLR_LORA = 64
GATE_LORA = 128
A_SHIFTED = 3 * A_WIDTH + DECAY_LORA + ICLR_LORA
A_COLS = A_SHIFTED + GATE_LORA
LNX_EPS = 64e-5
B_WIDTH = D_MODEL
B_BLOCKS = 8
B_BLOCK = B_WIDTH // B_BLOCKS
CONV_W = 4
LRU_C = 8.0
N_BRANCH = 2
IN_COLS = A_COLS + 2 * B_WIDTH + N_BRANCH * D_MODEL
DENSE_FF = 4 * D_MODEL
N_EXPERTS = 8
TOP_K = 2
EXPERT_FF = 7 * D_MODEL // 2
N_DENSE = (DEPTH + 1) // 2
N_MOE = DEPTH // 2
N_MOD = 6
EPS = 1e-6

kernel_name = "bidir_rwkv7_rglru_diffusion_step"


def rmsnorm(x, g):
    xf = x.astype(jnp.float32)
    y = xf * lax.rsqrt(jnp.mean(xf * xf, axis=-1, keepdims=True) + EPS)
    return (y * g.astype(jnp.float32)).astype(x.dtype)


def grid_rows(x, rows):
    b, l, ch = x.shape
    return x.reshape(b, rows, l // rows, ch)


def shift_prev(x, rows):
    xr = grid_rows(x, rows)
    return jnp.pad(xr[:, :, :-1], ((0, 0), (0, 0), (1, 0), (0, 0))).reshape(x.shape)


def shift_next(x, rows):
    xr = grid_rows(x, rows)
    return jnp.pad(xr[:, :, 1:], ((0, 0), (0, 0), (0, 1), (0, 0))).reshape(x.shape)


def dwconv_centred(x, w, b, rows):
    xr = grid_rows(x, rows)
    n = xr.shape[2]
    left = CONV_W // 2
    xp = jnp.pad(xr, ((0, 0), (0, 0), (left, CONV_W - 1 - left), (0, 0)))
    y = xp[:, :, 0:n] * w[0]
    for j in range(1, CONV_W):
        y = y + xp[:, :, j:j + n] * w[j]
    return (y + b).reshape(x.shape)


def to_scan(z):
    z = jnp.stack([z[0], jnp.flip(z[1], axis=1)])
    return jnp.moveaxis(z, 2, 0)


def from_scan(z):
    z = jnp.moveaxis(z, 0, 2)
    return jnp.stack([z[0], jnp.flip(z[1], axis=1)])


def rwkv7_mix(pa, rows, s0, mu, w0, w_up, a0, a_up, k_k, k_a, r_k, g_up, lnx_w, lnx_b):
    f32 = jnp.float32
    pa = pa.astype(f32)
    b, l, _ = pa.shape
    ps, gd = pa[..., :A_SHIFTED], pa[..., A_SHIFTED:]
    shifted = jnp.stack([shift_prev(ps, rows), shift_next(ps, rows)])
    xd = ps + (shifted - ps) * mu[:, None, None, :]
    r, k, v, wd, ad = jnp.split(
        xd, [A_WIDTH, 2 * A_WIDTH, 3 * A_WIDTH, 3 * A_WIDTH + DECAY_LORA], axis=-1)
    w_log = -jax.nn.softplus(-(w0[:, None, None, :]
                               + jnp.einsum("dblr,drc->dblc", jnp.tanh(wd), w_up))) - 0.5
    decay = jnp.exp(-jnp.exp(w_log))
    a = jax.nn.sigmoid(a0[:, None, None, :] + jnp.einsum("dblr,drc->dblc", ad, a_up))

    def heads(z):
        return z.reshape(N_DIR, b, l, A_HEADS, A_HEAD_DIM)

    kk = heads(k * k_k)
    kk = kk / jnp.maximum(jnp.linalg.norm(kk, axis=-1, keepdims=True), 1e-12)
    k = heads(k * (1.0 + (a - 1.0) * k_a))
    r, v, decay, a = heads(r), heads(v), heads(decay), heads(a)

    def step(S, inp):
        r_t, k_t, v_t, w_t, kk_t, a_t = inp
        s_kk = jnp.einsum("dbhij,dbhj->dbhi", S, kk_t)
        S = (S * w_t[..., None, :]
             - s_kk[..., None] * (kk_t * a_t)[..., None, :]
             + v_t[..., None] * k_t[..., None, :])
        return S, jnp.einsum("dbhij,dbhj->dbhi", S, r_t)

    s_final, ys = lax.scan(step, jnp.moveaxis(s0.astype(f32), 1, 0),
                           tuple(to_scan(z) for z in (r, k, v, decay, kk, a)))
    y = from_scan(ys).sum(0)
    mean = jnp.mean(y, axis=-1, keepdims=True)
    var = jnp.mean(jnp.square(y - mean), axis=-1, keepdims=True)
    y = ((y - mean) * lax.rsqrt(var + LNX_EPS)).reshape(b, l, A_WIDTH) * lnx_w + lnx_b
    bonus = jnp.sum(jnp.sum(r * k * r_k, axis=-1, keepdims=True) * v, axis=0).reshape(b, l, A_WIDTH)
    g = jax.nn.sigmoid(gd) @ g_up
    return (y + bonus) * g, jnp.moveaxis(s_final, 0, 1)


def rglru_mix(pb, rows, h0, conv_w, conv_b, w_rg, b_rg, w_ig, b_ig, lam):
    f32 = jnp.float32
    pb = pb.astype(f32)
    xin, gate = pb[..., :B_WIDTH], pb[..., B_WIDTH:]
    xc = dwconv_centred(xin, conv_w, conv_b, rows)
    b, l, _ = xc.shape
    xb = xc.reshape(b, l, B_BLOCKS, B_BLOCK)

    def block_diag(w, bias):
        return (jnp.einsum("blnc,dnce->dblne", xb, w).reshape(N_DIR, b, l, B_WIDTH)
                + bias[:, None, None, :])

    rg = jax.nn.sigmoid(block_diag(w_rg, b_rg))
    ig = jax.nn.sigmoid(block_diag(w_ig, b_ig))
    log_a = -LRU_C * rg * jax.nn.softplus(-lam)[:, None, None, :]
    a = jnp.exp(log_a)
    u = jnp.sqrt(-jnp.expm1(2.0 * log_a)) * ig * xc

    def combine(e1, e2):
        return e1[0] * e2[0], e2[0] * e1[1] + e2[1]

    a_cum, u_cum = lax.associative_scan(combine, (to_scan(a), to_scan(u)), axis=0)
    h = a_cum * jnp.moveaxis(h0.astype(f32), 1, 0) + u_cum
    y = from_scan(h).sum(0) * jax.nn.gelu(gate)
    return y, jnp.moveaxis(h[-1], 0, 1)


def token_mix(h, rows, sa0, sb0, lp):
    p = h @ lp["w_in"]
    pa = p[..., :A_COLS]
    pb = p[..., A_COLS:A_COLS + 2 * B_WIDTH]
    pg = p[..., A_COLS + 2 * B_WIDTH:]
    ya, sa = rwkv7_mix(pa, rows, sa0, lp["rwkv_mu"], lp["rwkv_w0"], lp["rwkv_w_up"],
                       lp["rwkv_a0"], lp["rwkv_a_up"], lp["rwkv_k_k"], lp["rwkv_k_a"],
                       lp["rwkv_r_k"], lp["rwkv_g_up"], lp["rwkv_lnx_w"], lp["rwkv_lnx_b"])
    yb, sb = rglru_mix(pb, rows, sb0, lp["lru_conv_w"], lp["lru_conv_b"], lp["lru_w_rg"],
                       lp["lru_b_rg"], lp["lru_w_ig"], lp["lru_b_ig"], lp["lru_lam"])
    gates = jax.nn.sigmoid((pg + lp["b_merge"]).astype(jnp.float32))
    ga, gb = gates[..., :D_MODEL], gates[..., D_MODEL:]
    m = (ga * (ya.astype(h.dtype) @ lp["w_proj_a"])
         + gb * (yb.astype(h.dtype) @ lp["w_proj_b"]))
    return m.astype(h.dtype) @ lp["w_out"], sa, sb


def swiglu(h, w1, w3, w2):
    return (jax.nn.silu(h @ w1) * (h @ w3)) @ w2


def moe_swiglu(h, router, w1, w3, w2):
    f32 = jnp.float32
    logits = (h @ router).astype(f32)
    top_v, top_i = lax.top_k(logits, TOP_K)
    top_w = jax.nn.softmax(top_v, axis=-1)
    gates = jnp.sum(jax.nn.one_hot(top_i, N_EXPERTS, dtype=f32) * top_w[..., None], axis=-2)
    out = jnp.zeros(h.shape, f32)
    for e in range(N_EXPERTS):
        out = out + gates[..., e:e + 1] * swiglu(h, w1[e], w3[e], w2[e]).astype(f32)
    return out.astype(h.dtype)


def layer(x, cond, rows, sa0, sb0, lp, channel_mix):
    mod = jax.nn.silu(cond) @ lp["mod_w"] + lp["mod_b"]
    sh1, sc1, g1, sh2, sc2, g2 = jnp.split(mod[:, None, :], N_MOD, axis=-1)
    h = rmsnorm(x, lp["norm_pre_mix"]) * (1.0 + sc1) + sh1
    m, sa, sb = token_mix(h, rows, sa0, sb0, lp)
    x = x + g1 * rmsnorm(m, lp["norm_post_mix"])
    h = rmsnorm(x, lp["norm_pre_ffn"]) * (1.0 + sc2) + sh2
    x = x + g2 * rmsnorm(channel_mix(h), lp["norm_post_ffn"])
    return x, sa, sb


def setup_inputs(seed: int = 0) -> dict:
    key = jax.random.key(seed)
    keys = jax.random.split(key, 64)
    ks = (keys[i] for i in range(64))
    f32 = jnp.float32

    def nrm(shape, scale):
        return jax.random.normal(next(ks), shape, f32) * scale

    def unif(shape, lo, hi):
        return jax.random.uniform(next(ks), shape, f32, lo, hi)

    def gain(shape):
        return 1.0 + nrm(shape, 0.05)

    L, DR, E = DEPTH, N_DIR, N_EXPERTS
    a_init = unif((L, DR, B_WIDTH), 0.9, 0.999)
    a_root = a_init ** (1.0 / LRU_C)
    return {
        "x_prompt": nrm((BATCH, SEQ, D_MODEL), 1.0),
        "x_sample": nrm((DEC_BATCH, DEC_SEQ, D_MODEL), 1.0),
        "c": nrm((DEC_BATCH, D_MODEL), 1.0),
        "c_ctx": nrm((D_MODEL,), 1.0),
        "state_rwkv": nrm((DEC_BATCH, DEPTH, N_DIR, A_HEADS, A_HEAD_DIM, A_HEAD_DIM), 0.5),
        "state_lru": nrm((DEC_BATCH, DEPTH, N_DIR, B_WIDTH), 0.5),
        "mod_w": nrm((L, D_MODEL, N_MOD * D_MODEL), 0.5 * D_MODEL ** -0.5),
        "mod_b": nrm((L, N_MOD * D_MODEL), 0.02),
        "norm_pre_mix": gain((L, D_MODEL)),
        "norm_post_mix": gain((L, D_MODEL)),
        "norm_pre_ffn": gain((L, D_MODEL)),
        "norm_post_ffn": gain((L, D_MODEL)),
        "w_in": nrm((L, D_MODEL, IN_COLS), D_MODEL ** -0.5),
        "b_merge": nrm((L, N_BRANCH * D_MODEL), 0.02),
        "rwkv_mu": unif((L, DR, A_SHIFTED), 0.0, 1.0),
        "rwkv_w0": unif((L, DR, A_WIDTH), -6.0, -1.0),
        "rwkv_w_up": nrm((L, DR, DECAY_LORA, A_WIDTH), 0.5 * DECAY_LORA ** -0.5),
        "rwkv_a0": nrm((L, DR, A_WIDTH), 0.5),
        "rwkv_a_up": nrm((L, DR, ICLR_LORA, A_WIDTH), 0.5 * ICLR_LORA ** -0.5),
        "rwkv_k_k": 0.85 + nrm((L, A_WIDTH), 0.05),
        "rwkv_k_a": gain((L, A_WIDTH)),
        "rwkv_r_k": nrm((L, A_HEADS, A_HEAD_DIM), 0.1),
        "rwkv_g_up": nrm((L, GATE_LORA, A_WIDTH), GATE_LORA ** -0.5),
        "rwkv_lnx_w": gain((L, A_WIDTH)),
        "rwkv_lnx_b": nrm((L, A_WIDTH), 0.02),
        "lru_conv_w": nrm((L, CONV_W, B_WIDTH), CONV_W ** -0.5),
        "lru_conv_b": nrm((L, B_WIDTH), 0.02),
        "lru_w_rg": nrm((L, DR, B_BLOCKS, B_BLOCK, B_BLOCK), B_BLOCK ** -0.5),
        "lru_b_rg": nrm((L, DR, B_WIDTH), 0.02),
        "lru_w_ig": nrm((L, DR, B_BLOCKS, B_BLOCK, B_BLOCK), B_BLOCK ** -0.5),
        "lru_b_ig": nrm((L, DR, B_WIDTH), 0.02),
        "lru_lam": jnp.log(a_root) - jnp.log1p(-a_root),
        "w_proj_a": nrm((L, A_WIDTH, D_MODEL), A_WIDTH ** -0.5),
        "w_proj_b": nrm((L, B_WIDTH, D_MODEL), B_WIDTH ** -0.5),
        "w_out": nrm((L, D_MODEL, D_MODEL), D_MODEL ** -0.5),
        "ffn_w1": nrm((N_DENSE, D_MODEL, DENSE_FF), D_MODEL ** -0.5),
        "ffn_w3": nrm((N_DENSE, D_MODEL, DENSE_FF), D_MODEL ** -0.5),
        "ffn_w2": nrm((N_DENSE, DENSE_FF, D_MODEL), DENSE_FF ** -0.5),
        "moe_router": nrm((N_MOE, D_MODEL, N_EXPERTS), D_MODEL ** -0.5),
        "moe_w1": nrm((N_MOE, E, D_MODEL, EXPERT_FF), D_MODEL ** -0.5),
        "moe_w3": nrm((N_MOE, E, D_MODEL, EXPERT_FF), D_MODEL ** -0.5),
        "moe_w2": nrm((N_MOE, E, EXPERT_FF, D_MODEL), EXPERT_FF ** -0.5),
    }


def reference(x_prompt, x_sample, c, c_ctx, state_rwkv, state_lru,
              mod_w, mod_b, norm_pre_mix, norm_post_mix, norm_pre_ffn, norm_post_ffn,
              w_in, b_merge,
              rwkv_mu, rwkv_w0, rwkv_w_up, rwkv_a0, rwkv_a_up, rwkv_k_k, rwkv_k_a,
              rwkv_r_k, rwkv_g_up, rwkv_lnx_w, rwkv_lnx_b,
              lru_conv_w, lru_conv_b, lru_w_rg, lru_b_rg, lru_w_ig, lru_b_ig, lru_lam,
              w_proj_a, w_proj_b, w_out,
              ffn_w1, ffn_w3, ffn_w2,
              moe_router, moe_w1, moe_w3, moe_w2):
    rows_sample = x_sample.shape[1] // GRID_W
    rows_ctx = 1
    b_p = x_prompt.shape[0]
    zero_rwkv = jnp.zeros((b_p, N_DIR, A_HEADS, A_HEAD_DIM, A_HEAD_DIM), jnp.float32)
    zero_lru = jnp.zeros((b_p, N_DIR, B_WIDTH), jnp.float32)
    cond_ctx = jnp.broadcast_to(c_ctx, (b_p, D_MODEL))

    y_prompt, y_sample = x_prompt, x_sample
    ctx_rwkv, ctx_lru = [], []
    for l in range(DEPTH):
        lp = dict(
            mod_w=mod_w[l], mod_b=mod_b[l],
            norm_pre_mix=norm_pre_mix[l], norm_post_mix=norm_post_mix[l],
            norm_pre_ffn=norm_pre_ffn[l], norm_post_ffn=norm_post_ffn[l],
            w_in=w_in[l], b_merge=b_merge[l],
            rwkv_mu=rwkv_mu[l], rwkv_w0=rwkv_w0[l], rwkv_w_up=rwkv_w_up[l],
            rwkv_a0=rwkv_a0[l], rwkv_a_up=rwkv_a_up[l], rwkv_k_k=rwkv_k_k[l],
            rwkv_k_a=rwkv_k_a[l], rwkv_r_k=rwkv_r_k[l], rwkv_g_up=rwkv_g_up[l],
            rwkv_lnx_w=rwkv_lnx_w[l], rwkv_lnx_b=rwkv_lnx_b[l],
            lru_conv_w=lru_conv_w[l], lru_conv_b=lru_conv_b[l],
            lru_w_rg=lru_w_rg[l], lru_b_rg=lru_b_rg[l],
            lru_w_ig=lru_w_ig[l], lru_b_ig=lru_b_ig[l], lru_lam=lru_lam[l],
            w_proj_a=w_proj_a[l], w_proj_b=w_proj_b[l], w_out=w_out[l],
        )
        i = l // 2
        if l % 2 == 0:
            channel_mix = functools.partial(swiglu, w1=ffn_w1[i], w3=ffn_w3[i], w2=ffn_w2[i])
        else:
            channel_mix = functools.partial(moe_swiglu, router=moe_router[i],
                                            w1=moe_w1[i], w3=moe_w3[i], w2=moe_w2[i])
        y_prompt, s_rwkv, s_lru = layer(y_prompt, cond_ctx, rows_ctx, zero_rwkv, zero_lru,
                                        lp, channel_mix)
        ctx_rwkv.append(s_rwkv)
        ctx_lru.append(s_lru)
        y_sample, _, _ = layer(y_sample, c, rows_sample, state_rwkv[:, l], state_lru[:, l],
                               lp, channel_mix)

    new_state_rwkv = jnp.stack(ctx_rwkv, axis=1).astype(x_prompt.dtype)
    new_state_lru = jnp.stack(ctx_lru, axis=1).astype(x_prompt.dtype)
    return (y_prompt, y_sample, new_state_rwkv, new_state_lru)
```

```python
import contextlib
import numpy as np
import concourse.bass as bass
import concourse.mybir as mybir
from concourse.ap import AP
from concourse.bass_utils import run_bass_kernel_spmd

F32 = mybir.dt.float32
BF16 = mybir.dt.bfloat16
ALU = mybir.AluOpType
AF = mybir.ActivationFunctionType
AX = mybir.AxisListType

ENGS = ("pe", "act", "dve", "pool", "sp")
NCORES = 8
T = 2560
NBLK = 5
SEQS = [(0, 2048, 64), (2048, 256, 256), (2304, 256, 256)]
D = 1024
INC = 7424
L = 2


class Buf:
    __slots__ = ("name", "last_write", "reads")

    def __init__(self, name=""):
        self.name = name
        self.last_write = None
        self.reads = []


class Prog:
    def __init__(self, nc, n_dma_sems=(24, 8, 24)):
        self.nc = nc
        self.ops = {e: [] for e in ENGS}
        self.count = {e: 0 for e in ENGS}
        self.waited = {e: {} for e in ENGS}
        self.sems = {}
        self._ctx = []
        for e in ENGS:
            self.sems["E_" + e] = self._sem("E_" + e)
        self.dma_pool = {}
        for q, n in zip(("sp", "act", "pool"), n_dma_sems):
            self.dma_pool[q] = []
            for i in range(n):
                self.sems[f"D_{q}{i}"] = self._sem(f"D_{q}{i}")
                self.dma_pool[q].append([f"D_{q}{i}", 0])
        self.dma_rr = {q: 0 for q in ("sp", "act", "pool")}
        self.n_ops = 0

    def _sem(self, name):
        cm = self.nc.semaphore(name)
        s = cm.__enter__()
        self._ctx.append(cm)
        return s

    def sbuf(self, name, shape, dtype):
        self._uid = getattr(self, "_uid", 0) + 1
        name = f"s{self._uid}_{name}"
        cm = self.nc.sbuf_tensor(name, list(shape), dtype)
        t = cm.__enter__()
        self._ctx.append(cm)
        return t

    def psum(self, name, shape, dtype):
        cm = self.nc.psum_tensor(name, list(shape), dtype)
        t = cm.__enter__()
        self._ctx.append(cm)
        return t

    def _deps(self, eng, reads, writes):
        deps = {}
        own = "E_" + eng

        def add(tok, same_ok):
            if tok is None:
                return
            s, v = tok
            if s == own and not same_ok:
                return
            if deps.get(s, 0) < v:
                deps[s] = v

        for b in reads:
            add(b.last_write, eng != "pe")
        for b in writes:
            add(b.last_write, False)
            for t in b.reads:
                add(t, False)
        waits = []
        w = self.waited[eng]
        for s, v in deps.items():
            if w.get(s, 0) >= v:
                continue
            w[s] = v
            waits.append((s, v))
        return waits

    def _commit(self, tok, reads, writes):
        for b in reads:
            b.reads.append(tok)
            if len(b.reads) > 48:
                latest = {}
                for s, v in b.reads:
                    if latest.get(s, 0) < v:
                        latest[s] = v
                b.reads = list(latest.items())
        for b in writes:
            b.last_write = tok
            b.reads = []

    def op(self, eng, fn, reads=(), writes=()):
        waits = self._deps(eng, reads, writes)
        self.count[eng] += 1
        tok = ("E_" + eng, self.count[eng])
        self.ops[eng].append((waits, fn, tok))
        self._commit(tok, reads, writes)
        self.n_ops += 1
        return tok

    def dma(self, q, out, in_, reads=(), writes=(), **kw):
        pool = self.dma_pool[q]
        i = self.dma_rr[q]
        self.dma_rr[q] = (i + 1) % len(pool)
        ent = pool[i]
        semname = ent[0]
        waits = self._deps(q, reads, writes)
        if ent[1] > 0 and self.waited[q].get(semname, 0) < ent[1]:
            self.waited[q][semname] = ent[1]
            waits.append((semname, ent[1]))
        ent[1] += 16
        tok = (semname, ent[1])

        def fn(e, out=out, in_=in_, kw=kw):
            return e.dma_start(out=out, in_=in_, **kw)
        self.ops[q].append((waits, fn, tok))
        self._commit(tok, reads, writes)
        self.n_ops += 1
        return tok

    def barrier(self):
        targets = []
        for e in ENGS:
            if self.count[e] > 0:
                targets.append(("E_" + e, self.count[e]))
        for q in self.dma_pool:
            for name, v in self.dma_pool[q]:
                if v > 0:
                    targets.append((name, v))
        for e in ENGS:
            waits = []
            for s, v in targets:
                if s == "E_" + e:
                    continue
                if self.waited[e].get(s, 0) >= v:
                    continue
                self.waited[e][s] = v
                waits.append((s, v))
            if waits:
                self.ops[e].append((waits, None, None))

    @contextlib.contextmanager
    def scope(self):
        n0 = len(self._ctx)
        yield
        self.barrier()
        while len(self._ctx) > n0:
            cm = self._ctx.pop()
            cm.__exit__(None, None, None)

    def emit(self):
        nc = self.nc
        engmap = {"pe": "tensor", "act": "scalar", "dve": "vector", "pool": "gpsimd", "sp": "sync"}
        with nc.Block() as block:
            for e in ENGS:
                lst = self.ops[e]
                if not lst:
                    continue

                def body(eng, lst=lst):
                    for waits, fn, tok in lst:
                        if fn is None:
                            for s, v in waits:
                                eng.wait_ge(self.sems[s], v)
                            continue
                        for s, v in waits[1:]:
                            eng.wait_ge(self.sems[s], v)
                        ins = fn(eng)
                        if waits:
                            ins._wait_ge(self.sems[waits[0][0]], waits[0][1])
                        ins.then_inc(self.sems[tok[0]], 16 if tok[0].startswith("D_") else 1)
                getattr(block, engmap[e])(body)

    def close(self):
        while self._ctx:
            cm = self._ctx.pop()
            cm.__exit__(None, None, None)


def rev_last(ap):
    dims = [list(d) for d in ap.ap]
    step, cnt = dims[-1]
    return AP(ap.tensor, ap.offset + step * (cnt - 1), dims[:-1] + [[-step, cnt]])


def bcast(ap, axis, n):
    dims = [list(d) for d in ap.ap]
    dims.insert(axis, [0, n])
    return AP(ap.tensor, ap.offset, dims)


WEIGHT_SHAPES = [
    ("mod_w", (L, D, 6 * D)), ("mod_b", (L, 6 * D)),
    ("norm_pre_mix", (L, D)), ("norm_post_mix", (L, D)), ("norm_pre_ffn", (L, D)), ("norm_post_ffn", (L, D)),
    ("w_in", (L, D, INC)), ("b_merge", (L, 2 * D)),
    ("rwkv_mu", (L, 2, 3200)), ("rwkv_w0", (L, 2, D)), ("rwkv_w_up", (L, 2, 64, D)),
    ("rwkv_a0", (L, 2, D)), ("rwkv_a_up", (L, 2, 64, D)), ("rwkv_k_k", (L, D)), ("rwkv_k_a", (L, D)),
    ("rwkv_r_k", (L, 16, 64)), ("rwkv_g_up", (L, 128, D)), ("rwkv_lnx_w", (L, D)), ("rwkv_lnx_b", (L, D)),
    ("lru_conv_w", (L, 4, D)), ("lru_conv_b", (L, D)), ("lru_w_rg", (L, 2, 8, 128, 128)), ("lru_b_rg", (L, 2, D)),
    ("lru_w_ig", (L, 2, 8, 128, 128)), ("lru_b_ig", (L, 2, D)), ("lru_lam", (L, 2, D)),
    ("w_proj_a", (L, D, D)), ("w_proj_b", (L, D, D)), ("w_out", (L, D, D)),
    ("ffn_w1", (1, D, 4096)), ("ffn_w3", (1, D, 4096)), ("ffn_w2", (1, 4096, D)),
    ("moe_router", (1, D, 8)), ("moe_w1", (1, 8, D, 3584)), ("moe_w3", (1, 8, D, 3584)), ("moe_w2", (1, 8, 3584, D)),
]

C_ID = 0
C_US = 128
C_UI = 256
C_LS = 384
C_LI = 512
C_MF = 640
C_MB = 1152
C_ES = 1664
C_LM = C_ES + 8 * 128
C_LMT = C_LM + 7 * 128
NCST32 = C_LM
NCST = C_LMT + 7 * 128


def make_consts():
    c = np.zeros((128, NCST), np.float32)
    p = np.arange(128)[:, None]
    q = np.arange(128)[None, :]
    c[:, C_ID:C_ID + 128] = (p == q)
    c[:, C_US:C_US + 128] = (p < q)
    c[:, C_UI:C_UI + 128] = (p <= q)
    c[:, C_LS:C_LS + 128] = (p > q)
    c[:, C_LI:C_LI + 128] = (p >= q)
    t = np.arange(512)
    c[:, C_MF:C_MF + 512] = (t % 128 != 0)[None, :]
    c[:, C_MB:C_MB + 512] = (t % 128 != 127)[None, :]
    for e in range(8):
        c[e, C_ES + e * 128:C_ES + (e + 1) * 128] = 1.0
    for i in range(7):
        b = 1 << i
        m = ((p // (2 * b)) == (q // (2 * b))) & ((p % (2 * b)) < b) & ((q % (2 * b)) >= b)
        c[:, C_LM + i * 128:C_LM + (i + 1) * 128] = m
        c[:, C_LMT + i * 128:C_LMT + (i + 1) * 128] = m.T
    return c


def build_program(debug=False):
    nc = bass.Bass("TRN2", target_bir_lowering=False)

    def din(name, shape):
        return nc.dram_tensor(name, list(shape), F32, kind="ExternalInput").ap()

    def dout(name, shape):
        return nc.dram_tensor(name, list(shape), F32, kind="ExternalOutput").ap()

    xin = din("xin", [T, D])
    cond = din("cond", [2, D])
    st_rwkv = din("st_rwkv", [L, 2, 16, 64, 64])
    st_lru = din("st_lru", [L, 2, D])
    Wd = {n: din(n, s) for n, s in WEIGHT_SHAPES}
    cstd = din("cst", [128, NCST])
    y_out = dout("y", [T, D])
    ns_rwkv = dout("ns_rwkv", [2, L, 2, 16, 64, 64])
    ns_lru = dout("ns_lru", [2, L, 2, D])
    xs = nc.dram_tensor("xs_scr", [128, 8, T], F32).ap()
    ya_s = nc.dram_tensor("ya_scr", [128, 8, T], BF16).ap()

    P = Prog(nc)
    out_bufs = []

    def ACT(out, in_, func, R, Wr, scale=None, bias=None):
        kw = {}
        if scale is not None:
            kw["scale"] = scale
        if bias is not None:
            kw["bias"] = bias
        P.op("act", lambda e: e.activation(out=out, in_=in_, func=func, **kw), R, Wr)

    def TT(eng, out, in0, in1, op, R, Wr):
        P.op(eng, lambda e: e.tensor_tensor(out, in0, in1, op), R, Wr)

    def TS(eng, out, in0, s1, s2, op0, op1, R, Wr):
        if s2 is None:
            P.op(eng, lambda e: e.tensor_scalar(out, in0, s1, None, op0), R, Wr)
        else:
            P.op(eng, lambda e: e.tensor_scalar(out, in0, s1, s2, op0, op1), R, Wr)

    def STT(eng, out, in0, sc, in1, op0, op1, R, Wr):
        P.op(eng, lambda e: e.scalar_tensor_tensor(out, in0, sc, in1, op0, op1), R, Wr)

    def CP(eng, out, in_, R, Wr):
        if eng == "act":
            P.op("act", lambda e: e.activation(out=out, in_=in_, func=AF.Copy), R, Wr)
        else:
            P.op(eng, lambda e: e.tensor_copy(out, in_), R, Wr)

    def MM(out, lhsT, rhs, start, stop, R, Wr):
        P.op("pe", lambda e: e.matmul(out, lhsT, rhs, start=start, stop=stop), R, Wr)

    def TR(out, in_, ident, R, Wr):
        P.op("pe", lambda e: e.transpose(out, in_, ident), R, Wr)

    def MSET(eng, ap, val, Wr):
        P.op(eng, lambda e: e.memset(ap, val), (), Wr)

    class Tl:
        def __init__(self, name, shape, dtype):
            self.t = P.sbuf(name, shape, dtype)
            self.b = Buf(name)

    banks = [P.psum(f"bank{i}", [128, 512], F32) for i in range(8)]
    bbufs = [Buf(f"bank{i}") for i in range(8)]
    rr = [0]

    def bank():
        i = rr[0]
        rr[0] = (i + 1) % 8
        return banks[i], bbufs[i]

    NE = 1.0 / D

    cst = Tl("cst", [128, NCST32], F32)
    P.dma("sp", cst.t[:, :], cstd[:, 0:NCST32], writes=[cst.b])
    lmk = Tl("lmk", [128, 14 * 128], BF16)
    P.dma("pool", lmk.t[:, :], cstd[:, C_LM:NCST], writes=[lmk.b])
    ident = cst.t[:, C_ID:C_ID + 128]
    ones_bf = Tl("ones_bf", [128, 128], BF16)
    MSET("pool", ones_bf.t[:, :], 1.0, [ones_bf.b])
    bd_bf = Tl("bd_bf", [128, 128], BF16)
    MSET("pool", bd_bf.t[:, :], 0.0, [bd_bf.b])
    MSET("pool", bd_bf.t[0:64, 0:64], 1.0, [bd_bf.b])
    MSET("pool", bd_bf.t[64:128, 64:128], 1.0, [bd_bf.b])
    ident_bf = Tl("ident_bf", [128, 128], BF16)
    CP("dve", ident_bf.t[:, :], ident, [cst.b], [ident_bf.b])

    def load_fm(name, src2d, n):
        cols = src2d.shape[1] // 128
        tl = Tl(name, [128, n, cols], F32)
        for i in range(n):
            P.dma("sp", tl.t[:, i, :], src2d[i].rearrange("(k p) -> p k", p=128), writes=[tl.b],
                  allow_slow_non_contiguous=True)
        return tl

    npm = load_fm("npm", Wd["norm_pre_mix"], L)
    npo = load_fm("npo", Wd["norm_post_mix"], L)
    npf = load_fm("npf", Wd["norm_pre_ffn"], L)
    npg = load_fm("npg", Wd["norm_post_ffn"], L)
    modb = load_fm("modb", Wd["mod_b"], L)
    bmer = load_fm("bmer", Wd["b_merge"], L)
    mu = load_fm("mu", Wd["rwkv_mu"].rearrange("l d c -> (l d) c"), 2 * L)
    w0 = load_fm("w0", Wd["rwkv_w0"].rearrange("l d c -> (l d) c"), 2 * L)
    a0 = load_fm("a0", Wd["rwkv_a0"].rearrange("l d c -> (l d) c"), 2 * L)
    kk_ = load_fm("kk_", Wd["rwkv_k_k"], L)
    ka_ = load_fm("ka_", Wd["rwkv_k_a"], L)
    rk_ = load_fm("rk_", Wd["rwkv_r_k"].rearrange("l h c -> l (h c)"), L)
    lnw = load_fm("lnw", Wd["rwkv_lnx_w"], L)
    lnb = load_fm("lnb", Wd["rwkv_lnx_b"], L)
    cw = load_fm("cw", Wd["lru_conv_w"].rearrange("l j c -> (l j) c"), 4 * L)
    cb = load_fm("cb", Wd["lru_conv_b"], L)
    brg = load_fm("brg", Wd["lru_b_rg"].rearrange("l d c -> (l d) c"), 2 * L)
    big = load_fm("big", Wd["lru_b_ig"].rearrange("l d c -> (l d) c"), 2 * L)
    lam = load_fm("lam", Wd["lru_lam"].rearrange("l d c -> (l d) c"), 2 * L)
    h0l = load_fm("h0l", st_lru.rearrange("l d c -> (l d) c"), 2 * L)

    omu = Tl("omu", [128, 2 * L, 25], F32)
    TS("dve", omu.t[:, :, :], mu.t[:, :, :], -1.0, 1.0, ALU.mult, ALU.add, [mu.b], [omu.b])
    omka = Tl("omka", [128, L, 8], F32)
    TS("dve", omka.t[:, :, :], ka_.t[:, :, :], -1.0, 1.0, ALU.mult, ALU.add, [ka_.b], [omka.b])
    c8 = Tl("c8", [128, 2 * L, 8], F32)
    ACT(c8.t[:, :, :], lam.t[:, :, :], AF.Exp, [lam.b], [c8.b], scale=-1.0)
    ACT(c8.t[:, :, :], c8.t[:, :, :], AF.Ln, [c8.b], [c8.b], bias=1.0)
    TS("dve", c8.t[:, :, :], c8.t[:, :, :], -8.0, None, ALU.mult, None, [c8.b], [c8.b])

    A1 = Tl("A1", [128, L, 2, 8], F32)
    B1 = Tl("B1", [128, L, 2, 8], F32)
    G1 = Tl("G1", [128, L, 2, 8], F32)
    A2 = Tl("A2", [128, L, 2, 8], F32)
    B2 = Tl("B2", [128, L, 2, 8], F32)
    G2 = Tl("G2", [128, L, 2, 8], F32)
    with P.scope():
        condT = Tl("condT", [128, 8, 2], F32)
        for g in range(2):
            P.dma("sp", condT.t[:, :, g], cond[g].rearrange("(k p) -> p k", p=128), writes=[condT.b],
                  allow_slow_non_contiguous=True)
        scond = Tl("scond", [128, 8, 2], F32)
        ACT(scond.t[:, :, :], condT.t[:, :, :], AF.Silu, [condT.b], [scond.b])
        modv = Tl("modv", [128, L, 48, 2], F32)
        mwt = [Tl(f"mwt{i}", [128, 8, 512], F32) for i in range(2)]
        for l in range(L):
            pb, pbb = bank()
            for cbk in range(12):
                wt = mwt[cbk % 2]
                src = Wd["mod_w"][l].rearrange("(k p) c -> p k c", p=128)[:, :, cbk * 512:(cbk + 1) * 512]
                P.dma("sp", wt.t[:, :, :], src, writes=[wt.b])
                for jj in range(4):
                    col = (cbk * 4 + jj) * 2
                    for k in range(8):
                        MM(pb[:, col:col + 2], wt.t[:, k, jj * 128:(jj + 1) * 128], scond.t[:, k, :],
                           k == 0, k == 7, [wt.b, scond.b], [pbb])
            for g in range(2):
                src = pb[:, 0:96].rearrange("p (c g) -> p c g", g=2)[:, :, g]
                TT("dve", modv.t[:, l, :, g], src, modb.t[:, l, :], ALU.add, [pbb, modb.b], [modv.b])
            for g in range(2):
                mv = modv.t[:, l, :, g]
                STT("dve", A1.t[:, l, g, :], mv[:, 8:16], 1.0, npm.t[:, l, :], ALU.add, ALU.mult, [modv.b, npm.b], [A1.b])
                CP("dve", B1.t[:, l, g, :], mv[:, 0:8], [modv.b], [B1.b])
                TT("dve", G1.t[:, l, g, :], mv[:, 16:24], npo.t[:, l, :], ALU.mult, [modv.b, npo.b], [G1.b])
                STT("dve", A2.t[:, l, g, :], mv[:, 32:40], 1.0, npf.t[:, l, :], ALU.add, ALU.mult, [modv.b, npf.b], [A2.b])
                CP("dve", B2.t[:, l, g, :], mv[:, 24:32], [modv.b], [B2.b])
                TT("dve", G2.t[:, l, g, :], mv[:, 40:48], npg.t[:, l, :], ALU.mult, [modv.b, npg.b], [G2.b])

    xs_b = [Buf(f"xs{i}") for i in range(NBLK)]
    with P.scope():
        xrow = [Tl(f"xrow{i}", [128, D], F32) for i in range(2)]
        xfm = [Tl(f"xfm{i}", [128, 8, 512], F32) for i in range(2)]
        for tb in range(NBLK):
            xf = xfm[tb % 2]
            for tt in range(4):
                xr_ = xrow[(tb * 4 + tt) % 2]
                r0 = tb * 512 + tt * 128
                P.dma("sp", xr_.t[:, :], xin[r0:r0 + 128, :], writes=[xr_.b])
                for half in range(2):
                    pb, pbb = bank()
                    for kk2 in range(4):
                        k = half * 4 + kk2
                        TR(pb[:, kk2 * 128:(kk2 + 1) * 128], xr_.t[:, k * 128:(k + 1) * 128], ident, [xr_.b, cst.b], [pbb])
                    dst = xf.t[:, half * 4:(half + 1) * 4, tt * 128:(tt + 1) * 128]
                    src = pb[:, :].rearrange("p (k t) -> p k t", t=128)
                    if half == 0:
                        CP("act", dst, src, [pbb], [xf.b])
                    else:
                        CP("dve", dst, src, [pbb], [xf.b])
            P.dma("sp", xs[:, :, tb * 512:(tb + 1) * 512], xf.t[:, :, :], reads=[xf.b], writes=[xs_b[tb]])

    def blk_group(t0):
        return 0 if t0 < 2048 else 1

    def SCAN(out, d0, d1, init, R, Wr):
        P.op("dve", lambda e: e.tensor_tensor_scan(out, d0, d1, init, ALU.mult, ALU.add), R, Wr)

    def RSUM(out, in_, R, Wr):
        P.op("dve", lambda e: e.reduce_sum(out, in_, AX.X), R, Wr)

    def RMAX(out, in_, R, Wr):
        P.op("dve", lambda e: e.reduce_max(out, in_, AX.X), R, Wr)

    def hbuf(Hb, t0):
        return Hb[t0 // 512] if isinstance(Hb, list) else Hb

    def norm_mod(l, A, B, H, Hb, blocks, hoff, tmp, hf32=None, cbk=None):
        xbt, sq_t, rstd_t, xn_t = tmp
        for bi, (t0, n) in enumerate(blocks):
            g = blk_group(t0)
            xb = xbt[bi % 2]
            P.dma("sp", xb.t[:, :, 0:n], xs[:, :, t0:t0 + n], reads=[xs_b[t0 // 512]], writes=[xb.b])
            ACT(sq_t.t[:, :, 0:n], xb.t[:, :, 0:n], AF.Square, [xb.b], [sq_t.b])
            pb, pbb = bank()
            for k in range(8):
                MM(pb[:, 0:n], ones_bf.t[:, :], sq_t.t[:, k, 0:n], k == 0, k == 7, [ones_bf.b, sq_t.b], [pbb])
            TS("dve", rstd_t.t[:, 0:n], pb[:, 0:n], NE, 1e-6, ALU.mult, ALU.add, [pbb], [rstd_t.b])
            ACT(rstd_t.t[:, 0:n], rstd_t.t[:, 0:n], AF.Ln, [rstd_t.b], [rstd_t.b])
            ACT(rstd_t.t[:, 0:n], rstd_t.t[:, 0:n], AF.Exp, [rstd_t.b], [rstd_t.b], scale=-0.5)
            TT("dve", xn_t.t[:, :, 0:n], xb.t[:, :, 0:n], bcast(rstd_t.t[:, 0:n], 1, 8), ALU.mult,
               [xb.b, rstd_t.b], [xn_t.b])
            for k in range(8):
                dst = H.t[:, k, t0 - hoff:t0 - hoff + n]
                ACT(dst, xn_t.t[:, k, 0:n], AF.Identity, [xn_t.b, A.b, B.b], [hbuf(Hb, t0)],
                    scale=A.t[:, l, g, k:k + 1], bias=B.t[:, l, g, k:k + 1])
                if hf32 is not None:
                    ACT(hf32.t[:, k, 0:n], xn_t.t[:, k, 0:n], AF.Identity, [xn_t.b, A.b, B.b], [hf32.b],
                        scale=A.t[:, l, g, k:k + 1], bias=B.t[:, l, g, k:k + 1])
            if cbk is not None:
                cbk(t0, n)

    def norm_tmps():
        return ([Tl(f"xb{i}", [128, 8, 512], F32) for i in range(2)], Tl("sq", [128, 8, 512], BF16),
                Tl("rstd", [128, 512], F32), Tl("xn", [128, 8, 512], F32))

    def win_cols(l, c0, n=128):
        return Wd["w_in"][l].rearrange("(k p) c -> p k c", p=128)[:, :, c0:c0 + n]

    def proj(wt3, wb, H, Hb, t0, n, hoff=0):
        pb, pbb = bank()
        for k in range(8):
            MM(pb[:, 0:n], wt3[:, k, :], H.t[:, k, t0 - hoff:t0 - hoff + n], k == 0, k == 7, [wb, hbuf(Hb, t0)], [pbb])
        return pb, pbb

    def rowview(ap2d, t0):
        w = 64 if t0 < 2048 else 256
        return ap2d.rearrange("p (r w) -> p r w", w=w)

    def mix(pb, pbb, dst, mu_ap, omu_ap, d, t0, n):
        ACT(dst.t[:, 0:n], pb[:, 0:n], AF.Identity, [pbb, mu.b, omu.b], [dst.b], scale=omu_ap)
        dv = rowview(dst.t[:, 0:n], t0)
        sv = rowview(pb[:, 0:n], t0)
        if d == 0:
            STT("dve", dv[:, :, 1:], sv[:, :, :-1], mu_ap, dv[:, :, 1:], ALU.mult, ALU.add, [pbb, dst.b, mu.b], [dst.b])
        else:
            STT("dve", dv[:, :, :-1], sv[:, :, 1:], mu_ap, dv[:, :, :-1], ALU.mult, ALU.add, [pbb, dst.b, mu.b], [dst.b])

    BLK5 = [(i * 512, 512) for i in range(NBLK)]
    lruo = Tl("lruo", [128, 2, L, 2, 8], F32)

    def residual_update(l, O, Ob, G, t0, n, final, tmp):
        xb, sq_t, rstd_t, on_t, yrow = tmp
        g = blk_group(t0)
        P.dma("sp", xb.t[:, :, 0:n], xs[:, :, t0:t0 + n], reads=[xs_b[t0 // 512]], writes=[xb.b])
        ACT(sq_t.t[:, :, 0:n], O, AF.Square, [Ob], [sq_t.b])
        pb, pbb = bank()
        for k in range(8):
            MM(pb[:, 0:n], ones_bf.t[:, :], sq_t.t[:, k, 0:n], k == 0, k == 7, [ones_bf.b, sq_t.b], [pbb])
        TS("dve", rstd_t.t[:, 0:n], pb[:, 0:n], NE, 1e-6, ALU.mult, ALU.add, [pbb], [rstd_t.b])
        ACT(rstd_t.t[:, 0:n], rstd_t.t[:, 0:n], AF.Ln, [rstd_t.b], [rstd_t.b])
        ACT(rstd_t.t[:, 0:n], rstd_t.t[:, 0:n], AF.Exp, [rstd_t.b], [rstd_t.b], scale=-0.5)
        TT("dve", on_t.t[:, :, 0:n], O, bcast(rstd_t.t[:, 0:n], 1, 8), ALU.mult, [Ob, rstd_t.b], [on_t.b])
        for k in range(8):
            STT("dve", xb.t[:, k, 0:n], on_t.t[:, k, 0:n], G.t[:, l, g, k:k + 1], xb.t[:, k, 0:n],
                ALU.mult, ALU.add, [on_t.b, G.b, xb.b], [xb.b])
        if not final:
            P.dma("sp", xs[:, :, t0:t0 + n], xb.t[:, :, 0:n], reads=[xb.b], writes=[xs_b[t0 // 512]])
        else:
            for tt in range(n // 128):
                yr = yrow[tt % 2]
                for half in range(2):
                    pt, ptb = bank()
                    for k4 in range(4):
                        k = half * 4 + k4
                        TR(pt[:, k4 * 128:(k4 + 1) * 128], xb.t[:, k, tt * 128:(tt + 1) * 128], ident, [xb.b, cst.b], [ptb])
                    CP("act" if half else "dve", yr.t[:, half * 512:(half + 1) * 512], pt[:, :], [ptb], [yr.b])
                ob = Buf("yout")
                out_bufs.append(ob)
                r0 = t0 + tt * 128
                P.dma("sp", y_out[r0:r0 + 128, :], yr.t[:, :], reads=[yr.b], writes=[ob])

    def res_tmps(final):
        return (Tl("rxb", [128, 8, 512], F32), Tl("rsq", [128, 8, 512], BF16), Tl("rrs", [128, 512], F32),
                Tl("ron", [128, 8, 512], F32), [Tl(f"yrow{i}", [128, D], F32) for i in range(2)] if final else None)

    def rwkv_phase(l, H, Hb, YB):
        LORA = Tl("LORA", [128, 2, T], BF16)
        SG = Tl("SG", [128, T], BF16)
        LW = Tl("LW", [128, 2, D], BF16)
        GUP = Tl("GUP", [128, D], BF16)
        for d in range(2):
            P.dma("pool", LW.t[0:64, d, :], Wd["rwkv_w_up"][l, d], writes=[LW.b])
            P.dma("pool", LW.t[64:128, d, :], Wd["rwkv_a_up"][l, d], writes=[LW.b])
        P.dma("pool", GUP.t[:, :], Wd["rwkv_g_up"][l], writes=[GUP.b])
        wch = [Tl(f"wch{i}", [128, 8, 128], BF16) for i in range(4)]
        wrr = [0]

        def next_w(c0):
            tl = wch[wrr[0] % 4]
            wrr[0] += 1
            P.dma("pool", tl.t[:, :, :], win_cols(l, c0), writes=[tl.b])
            return tl

        tn = ["xr", "xk", "xv", "lw", "aa", "cum", "e1", "e2", "kq", "kk", "kf", "bb"]
        tm = {n_: Tl("t_" + n_, [128, 512], F32) for n_ in tn}
        sqb = Tl("sqb", [128, 512], BF16)
        rkb = Tl("rkb", [128, 512], BF16)

        ybf = YB.t[:, :, :].rearrange("p a t -> p (a t)")
        ybuf = Buf("ybalias")

        class V:
            def __init__(self, ap):
                self.t = ap
                self.b = Buf()
        QR = V(ybf[:, 0:5120].rearrange("p (c x) -> p c x", x=256))
        KTz = V(ybf[:, 5120:10240].rearrange("p (h t) -> p h t", h=2))
        BTz = V(ybf[:, 10240:15360].rearrange("p (h t) -> p h t", h=2))
        VT = V(ybf[:, 15360:17920].rearrange("p (c x) -> p c x", x=128))
        KH = V(ybf[:, 17920:20480].rearrange("p (c x) -> p c x", x=128))
        BH = Tl("BH", [128, 20, 128], BF16)
        WEND = Tl("WEND", [128, 20], F32)
        MSET("pool", KTz.t[:, :, :], 0.0, [KTz.b])
        MSET("pool", BTz.t[:, :, :], 0.0, [BTz.b])

        wt = next_w(3072)
        x0 = tm["xr"]
        for (t0, n) in BLK5:
            pb, pbb = proj(wt.t, wt.b, H, Hb, t0, n)
            for d in range(2):
                mi = 2 * l + d
                mix(pb, pbb, x0, mu.t[:, mi, 24:25], omu.t[:, mi, 24:25], d, t0, n)
                ACT(LORA.t[0:64, d, t0:t0 + n], x0.t[0:64, 0:n], AF.Tanh, [x0.b], [LORA.b])
                CP("dve", LORA.t[64:128, d, t0:t0 + n], x0.t[64:128, 0:n], [x0.b], [LORA.b])
        wt = next_w(3200)
        for (t0, n) in BLK5:
            pb, pbb = proj(wt.t, wt.b, H, Hb, t0, n)
            ACT(SG.t[:, t0:t0 + n], pb[:, 0:n], AF.Sigmoid, [pbb], [SG.b])

        BON = Tl("BON", [128, T], F32)
        YACC = Tl("YACC", [128, 20, 128], F32)
        YAst = Tl("YAst", [128, T], BF16)
        NPt = Tl("NPt", [128, 2, 256], BF16)
        MOt = Tl("MOt", [128, 2, 256], BF16)
        NTt = Tl("NTt", [128, 2, 128], BF16)
        TNs = [Tl(f"TN{i}", [128, 2, 128], BF16) for i in range(4)]
        Xa = Tl("Xa", [128, 2, 128], BF16)
        Xb = Tl("Xb", [128, 2, 128], BF16)
        Btb = Tl("Btb", [128, 128], BF16)
        Utb = Tl("Utb", [128, 128], BF16)
        S32 = Tl("S32", [128, 64], F32)
        SBz = Tl("SBz", [128, 2, 64], BF16)
        BD = Tl("BD", [128, 128], F32)
        OUTS = Tl("OUTS", [128, 64], F32)
        MSET("pool", SBz.t[:, :, :], 0.0, [SBz.b])
        MSET("pool", BD.t[:, :], 0.0, [BD.b])
        st1 = Tl("st1", [128, 8], F32)
        st2 = Tl("st2", [128, 8], F32)

        def chunk_step(c, d):
            cc = slice(c * 128, (c + 1) * 128)
            mNP = cst.t[:, C_US:C_US + 256] if d == 0 else cst.t[:, C_LS:C_LS + 256]
            mNT = cst.t[:, C_LS:C_LS + 128] if d == 0 else cst.t[:, C_US:C_US + 128]
            pNP, bNP = bank()
            for h in range(2):
                MM(pNP[:, h * 256:(h + 1) * 256], BTz.t[:, h, cc], QR.t[:, c, :], True, True, [BTz.b, QR.b], [bNP])
            pMO, bMO = bank()
            for h in range(2):
                MM(pMO[:, h * 256:(h + 1) * 256], KTz.t[:, h, cc], QR.t[:, c, :], True, True, [KTz.b, QR.b], [bMO])
            pNT, bNT = bank()
            for h in range(2):
                MM(pNT[:, h * 128:(h + 1) * 128], QR.t[:, c, 0:128], BTz.t[:, h, cc], True, True, [BTz.b, QR.b], [bNT])
            v3 = lambda ap, x: ap.rearrange("p (h x) -> p h x", x=x)
            TT("dve", NPt.t[:, :, :], v3(pNP[:, 0:512], 256), bcast(mNP, 1, 2), ALU.mult, [bNP, cst.b], [NPt.b])
            TT("dve", MOt.t[:, :, :], v3(pMO[:, 0:512], 256), bcast(mNP, 1, 2), ALU.mult, [bMO, cst.b], [MOt.b])
            TT("dve", NTt.t[:, :, :], v3(pNT[:, 0:256], 128), bcast(mNT, 1, 2), ALU.mult, [bNT, cst.b], [NTt.b])
            def lm(i, tr):
                nat = (d == 0) != tr
                base = (0 if nat else 7 * 128) + i * 128
                return lmk.t[:, base:base + 128]
            Tc, TTc = TNs[0], TNs[1]
            TT("pool", Xa.t[:, :, :], NPt.t[:, :, 0:128], bcast(lm(0, False), 1, 2), ALU.mult, [NPt.b, lmk.b], [Xa.b])
            TT("pool", Tc.t[:, :, 0:128], Xa.t[:, :, :], bcast(ident_bf.t[:, :], 1, 2), ALU.add, [Xa.b, ident_bf.b], [Tc.b])
            TT("pool", Xb.t[:, :, :], NTt.t[:, :, :], bcast(lm(0, True), 1, 2), ALU.mult, [NTt.b, lmk.b], [Xb.b])
            TT("pool", TTc.t[:, :, 0:128], Xb.t[:, :, :], bcast(ident_bf.t[:, :], 1, 2), ALU.add, [Xb.b, ident_bf.b], [TTc.b])
            for lev in range(1, 7):
                Tn, TTn = (TNs[2], TNs[3]) if lev % 2 == 1 else (TNs[0], TNs[1])
                pX, bX = bank()
                for h in range(2):
                    MM(pX[:, h * 128:(h + 1) * 128], NTt.t[:, h, :], Tc.t[:, h, 0:128], True, True, [NTt.b, Tc.b], [bX])
                for h in range(2):
                    MM(pX[:, 256 + h * 128:256 + (h + 1) * 128], NPt.t[:, h, 0:128], TTc.t[:, h, 0:128], True, True,
                       [NPt.b, TTc.b], [bX])
                TT("dve", Xa.t[:, :, :], v3(pX[:, 0:256], 128), bcast(lm(lev, False), 1, 2), ALU.mult, [bX, lmk.b], [Xa.b])
                TT("dve", Xb.t[:, :, :], v3(pX[:, 256:512], 128), bcast(lm(lev, True), 1, 2), ALU.mult, [bX, lmk.b], [Xb.b])
                pY, bY_ = bank()
                for h in range(2):
                    hc2 = slice(h * 128, (h + 1) * 128)
                    MM(pY[:, hc2], TTc.t[:, h, 0:128], Xa.t[:, h, :], True, False, [TTc.b, Xa.b], [bY_])
                    MM(pY[:, hc2], ident_bf.t[:, :], Tc.t[:, h, 0:128], False, True, [ident_bf.b, Tc.b], [bY_])
                for h in range(2):
                    hc2 = slice(256 + h * 128, 256 + (h + 1) * 128)
                    MM(pY[:, hc2], Tc.t[:, h, 0:128], Xb.t[:, h, :], True, False, [Tc.b, Xb.b], [bY_])
                    MM(pY[:, hc2], ident_bf.t[:, :], TTc.t[:, h, 0:128], False, True, [ident_bf.b, TTc.b], [bY_])
                CP("act", Tn.t[:, :, 0:128], v3(pY[:, 0:256], 128), [bY_], [Tn.b])
                CP("act", TTn.t[:, :, 0:128], v3(pY[:, 256:512], 128), [bY_], [TTn.b])
                Tc, TTc = Tn, TTn
            Tfin = Tc
            pBt, bBt = bank()
            for h in range(2):
                hc = slice(h * 64, (h + 1) * 64)
                MM(pBt[:, hc], QR.t[:, c, 0:128], SBz.t[:, h, :], True, False, [QR.b, SBz.b], [bBt])
                MM(pBt[:, hc], MOt.t[:, h, 0:128], VT.t[:, c, hc], False, True, [MOt.b, VT.b], [bBt])
            CP("act", Btb.t[:, :], pBt[:, 0:128], [bBt], [Btb.b])
            pU, bU = bank()
            for h in range(2):
                hc = slice(h * 64, (h + 1) * 64)
                MM(pU[:, hc], Tfin.t[:, h, 0:128], Btb.t[:, hc], True, True, [Tfin.b, Btb.b], [bU])
            CP("dve", Utb.t[:, :], pU[:, 0:128], [bU], [Utb.b])
            pY, bY = bank()
            for h in range(2):
                hc = slice(h * 64, (h + 1) * 64)
                MM(pY[:, hc], QR.t[:, c, 128:256], SBz.t[:, h, :], True, False, [QR.b, SBz.b], [bY])
                MM(pY[:, hc], NPt.t[:, h, 128:256], Utb.t[:, hc], False, False, [NPt.b, Utb.b], [bY])
                MM(pY[:, hc], MOt.t[:, h, 128:256], VT.t[:, c, hc], False, True, [MOt.b, VT.b], [bY])
            if d == 0:
                CP("act", YACC.t[:, c, :], pY[:, 0:128], [bY], [YACC.b])
            else:
                TT("dve", YACC.t[:, c, :], pY[:, 0:128], YACC.t[:, c, :], ALU.add, [bY, YACC.b], [YACC.b])
            pS, bS = bank()
            for h in range(2):
                hc = slice(h * 64, (h + 1) * 64)
                MM(pS[:, hc], BH.t[:, c, :], Utb.t[:, hc], True, False, [BH.b, Utb.b], [bS])
                MM(pS[:, hc], KH.t[:, c, :], VT.t[:, c, hc], False, True, [KH.b, VT.b], [bS])
            for h in range(2):
                hs = slice(h * 64, (h + 1) * 64)
                STT("dve", S32.t[hs, :], S32.t[hs, :], WEND.t[hs, c:c + 1], pS[hs, h * 64:(h + 1) * 64], ALU.mult, ALU.add,
                    [S32.b, WEND.b, bS], [S32.b])
            CP("act", SBz.t[0:64, 0, :], S32.t[0:64, :], [S32.b], [SBz.b])
            CP("act", SBz.t[64:128, 1, :], S32.t[64:128, :], [S32.b], [SBz.b])

        for hp in range(8):
            hcols = slice(hp * 128, (hp + 1) * 128)
            for d in range(2):
                mi = 2 * l + d
                wr = next_w(hp * 128)
                wk = next_w(1024 + hp * 128)
                wv = next_w(2048 + hp * 128)
                for (t0, n) in BLK5:
                    c4 = t0 // 128
                    bl = slice(t0, t0 + n)
                    xr, xk, xv, lw, aa, cum, e1, e2, kq, kkt, kf, bb = [tm[x_] for x_ in tn]
                    pr, prb = proj(wr.t, wr.b, H, Hb, t0, n)
                    mix(pr, prb, xr, mu.t[:, mi, hp:hp + 1], omu.t[:, mi, hp:hp + 1], d, t0, n)
                    pk, pkb = proj(wk.t, wk.b, H, Hb, t0, n)
                    mix(pk, pkb, xk, mu.t[:, mi, 8 + hp:9 + hp], omu.t[:, mi, 8 + hp:9 + hp], d, t0, n)
                    pv, pvb = proj(wv.t, wv.b, H, Hb, t0, n)
                    mix(pv, pvb, xv, mu.t[:, mi, 16 + hp:17 + hp], omu.t[:, mi, 16 + hp:17 + hp], d, t0, n)
                    pw, pwb = bank()
                    MM(pw[:, 0:n], LW.t[0:64, d, hcols], LORA.t[0:64, d, bl], True, True, [LW.b, LORA.b], [pwb])
                    pa, pab = bank()
                    MM(pa[:, 0:n], LW.t[64:128, d, hcols], LORA.t[64:128, d, bl], True, True, [LW.b, LORA.b], [pab])
                    ACT(lw.t[:, :], pw[:, 0:n], AF.Sigmoid, [pwb, w0.b], [lw.b], bias=w0.t[:, mi, hp:hp + 1])
                    ACT(aa.t[:, :], pa[:, 0:n], AF.Sigmoid, [pab, a0.b], [aa.b], bias=a0.t[:, mi, hp:hp + 1])
                    TS("pool", lw.t[:, :], lw.t[:, :], -0.6065306597126334, None, ALU.mult, None, [lw.b], [lw.b])
                    if d == 0:
                        SCAN(cum.t[:, :], cst.t[:, C_MF:C_MF + 512], lw.t[:, :], 0.0, [cst.b, lw.b], [cum.b])
                        cend = cum.t[:, :].rearrange("p (c x) -> p c x", x=128)[:, :, 127]
                    else:
                        SCAN(rev_last(cum.t[:, :]), rev_last(cst.t[:, C_MB:C_MB + 512]), rev_last(lw.t[:, :]), 0.0,
                             [cst.b, lw.b], [cum.b])
                        cend = cum.t[:, :].rearrange("p (c x) -> p c x", x=128)[:, :, 0]
                    c3 = lambda ap: ap.rearrange("p (c x) -> p c x", x=128)
                    ACT(WEND.t[:, c4:c4 + 4], cend, AF.Exp, [cum.b], [WEND.b])
                    TS("pool", kq.t[:, :], xk.t[:, :], kk_.t[:, l, hp:hp + 1], None, ALU.mult, None, [xk.b, kk_.b], [kq.b])
                    ACT(sqb.t[:, :], kq.t[:, :], AF.Square, [kq.b], [sqb.b])
                    pn, pnb = bank()
                    MM(pn[:, 0:n], bd_bf.t[:, :], sqb.t[:, :], True, True, [bd_bf.b, sqb.b], [pnb])
                    TS("dve", kkt.t[:, :], pn[:, 0:n], 1e-24, None, ALU.add, None, [pnb], [kkt.b])
                    ACT(kkt.t[:, :], kkt.t[:, :], AF.Ln, [kkt.b], [kkt.b])
                    ACT(kkt.t[:, :], kkt.t[:, :], AF.Exp, [kkt.b], [kkt.b], scale=-0.5)
                    TT("dve", kkt.t[:, :], kkt.t[:, :], kq.t[:, :], ALU.mult, [kkt.b, kq.b], [kkt.b])
                    TS("dve", kf.t[:, :], aa.t[:, :], ka_.t[:, l, hp:hp + 1], omka.t[:, l, hp:hp + 1], ALU.mult, ALU.add,
                       [aa.b, ka_.b, omka.b], [kf.b])
                    TT("pool", kf.t[:, :], kf.t[:, :], xk.t[:, :], ALU.mult, [kf.b, xk.b], [kf.b])
                    TT("pool", bb.t[:, :], kkt.t[:, :], aa.t[:, :], ALU.mult, [kkt.b, aa.b], [bb.b])
                    STT("dve", rkb.t[:, :], xr.t[:, :], rk_.t[:, l, hp:hp + 1], kf.t[:, :], ALU.mult, ALU.mult,
                        [xr.b, rk_.b, kf.b], [rkb.b])
                    pbn, pbnb = bank()
                    MM(pbn[:, 0:n], bd_bf.t[:, :], rkb.t[:, :], True, True, [bd_bf.b, rkb.b], [pbnb])
                    if d == 0:
                        TT("dve", BON.t[:, bl], pbn[:, 0:n], xv.t[:, :], ALU.mult, [pbnb, xv.b], [BON.b])
                    else:
                        TT("dve", kq.t[:, :], pbn[:, 0:n], xv.t[:, :], ALU.mult, [pbnb, xv.b], [kq.b])
                        TT("pool", BON.t[:, bl], BON.t[:, bl], kq.t[:, :], ALU.add, [BON.b, kq.b], [BON.b])
                    pt, ptb = bank()
                    for c_ in range(4):
                        TR(pt[:, c_ * 128:(c_ + 1) * 128], xv.t[:, c_ * 128:(c_ + 1) * 128], ident, [xv.b, cst.b], [ptb])
                    CP("act", VT.t[:, c4:c4 + 4, :], c3(pt[:, 0:512]), [ptb], [VT.b])
                    ACT(e1.t[:, :], cum.t[:, :], AF.Exp, [cum.b], [e1.b])
                    TT("dve", QR.t[:, c4:c4 + 4, 128:256], c3(xr.t[:, :]), c3(e1.t[:, :]), ALU.mult, [xr.b, e1.b], [QR.b])
                    TT("pool", e2.t[:, :], cum.t[:, :], lw.t[:, :], ALU.subtract, [cum.b, lw.b], [e2.b])
                    ACT(e2.t[:, :], e2.t[:, :], AF.Exp, [e2.b], [e2.b])
                    TT("dve", QR.t[:, c4:c4 + 4, 0:128], c3(kkt.t[:, :]), c3(e2.t[:, :]), ALU.mult, [kkt.b, e2.b], [QR.b])
                    ACT(e1.t[:, :], cum.t[:, :], AF.Exp, [cum.b], [e1.b], scale=-1.0)
                    for h in range(2):
                        hs = slice(h * 64, (h + 1) * 64)
                        TT("pool", KTz.t[hs, h, bl], kf.t[hs, :], e1.t[hs, :], ALU.mult, [kf.b, e1.b], [KTz.b])
                        STT("dve", BTz.t[hs, h, bl], bb.t[hs, :], -1.0, e1.t[hs, :], ALU.mult, ALU.mult, [bb.b, e1.b], [BTz.b])
                    TT("dve", c3(e2.t[:, :]), bcast(cend, 2, 128), c3(cum.t[:, :]), ALU.subtract, [cum.b], [e2.b])
                    ACT(e2.t[:, :], e2.t[:, :], AF.Exp, [e2.b], [e2.b])
                    TT("pool", kf.t[:, :], kf.t[:, :], e2.t[:, :], ALU.mult, [kf.b, e2.b], [kf.b])
                    STT("dve", bb.t[:, :], bb.t[:, :], -1.0, e2.t[:, :], ALU.mult, ALU.mult, [bb.b, e2.b], [bb.b])
                    pt, ptb = bank()
                    for c_ in range(4):
                        TR(pt[:, c_ * 128:(c_ + 1) * 128], kf.t[:, c_ * 128:(c_ + 1) * 128], ident, [kf.b, cst.b], [ptb])
                    CP("act", KH.t[:, c4:c4 + 4, :], c3(pt[:, 0:512]), [ptb], [KH.b])
                    pt, ptb = bank()
                    for c_ in range(4):
                        TR(pt[:, c_ * 128:(c_ + 1) * 128], bb.t[:, c_ * 128:(c_ + 1) * 128], ident, [bb.b, cst.b], [ptb])
                    CP("dve", BH.t[:, c4:c4 + 4, :], c3(pt[:, 0:512]), [ptb], [BH.b])
                for si, (s0, slen, _) in enumerate(SEQS):
                    nch = slen // 128
                    cbase = s0 // 128
                    if si == 0:
                        for h in range(2):
                            hs = slice(h * 64, (h + 1) * 64)
                            P.dma("sp", BD.t[hs, hs], st_rwkv[l, d, 2 * hp + h], writes=[BD.b])
                        pt, ptb = bank()
                        TR(pt[:, 0:128], BD.t[:, :], ident, [BD.b, cst.b], [ptb])
                        for h in range(2):
                            hs = slice(h * 64, (h + 1) * 64)
                            CP("dve", S32.t[hs, :], pt[hs, hs], [ptb], [S32.b])
                    else:
                        MSET("pool", S32.t[:, :], 0.0, [S32.b])
                    CP("act", SBz.t[0:64, 0, :], S32.t[0:64, :], [S32.b], [SBz.b])
                    CP("act", SBz.t[64:128, 1, :], S32.t[64:128, :], [S32.b], [SBz.b])
                    order = range(nch) if d == 0 else range(nch - 1, -1, -1)
                    for ci in order:
                        chunk_step(cbase + ci, d)
                    if si > 0:
                        for h in range(2):
                            hs = slice(h * 64, (h + 1) * 64)
                            CP("dve", BD.t[hs, hs], S32.t[hs, :], [S32.b], [BD.b])
                        pt, ptb = bank()
                        TR(pt[:, 0:128], BD.t[:, :], ident, [BD.b, cst.b], [ptb])
                        for h in range(2):
                            hs = slice(h * 64, (h + 1) * 64)
                            CP("dve", OUTS.t[hs, :], pt[hs, hs], [ptb], [OUTS.b])
                        ob = Buf("nsr")
                        out_bufs.append(ob)
                        P.dma("sp", ns_rwkv[si - 1, l, d, 2 * hp:2 * hp + 2].rearrange("h i j -> (h i) j"), OUTS.t[:, :],
                              reads=[OUTS.b], writes=[ob])
            yc, ysq, yn, t1 = tm["xr"], tm["xk"], tm["xv"], tm["lw"]
            for (t0, n) in BLK5:
                c4 = t0 // 128
                bl = slice(t0, t0 + n)
                yv = YACC.t[:, c4:c4 + 4, :].rearrange("p c (h i) -> p (c h) i", i=64)
                v8 = lambda ap: ap.rearrange("p (g i) -> p g i", i=64)
                RSUM(st1.t[:, :], yv, [YACC.b], [st1.b])
                TS("dve", st1.t[:, :], st1.t[:, :], 1.0 / 64, None, ALU.mult, None, [st1.b], [st1.b])
                TT("dve", v8(yc.t[:, :]), yv, bcast(st1.t[:, :], 2, 64), ALU.subtract, [YACC.b, st1.b], [yc.b])
                TT("pool", ysq.t[:, :], yc.t[:, :], yc.t[:, :], ALU.mult, [yc.b], [ysq.b])
                RSUM(st2.t[:, :], v8(ysq.t[:, :]), [ysq.b], [st2.b])
                TS("dve", st2.t[:, :], st2.t[:, :], 1.0 / 64, 64e-5, ALU.mult, ALU.add, [st2.b], [st2.b])
                ACT(st2.t[:, :], st2.t[:, :], AF.Ln, [st2.b], [st2.b])
                ACT(st2.t[:, :], st2.t[:, :], AF.Exp, [st2.b], [st2.b], scale=-0.5)
                TT("dve", v8(yn.t[:, :]), v8(yc.t[:, :]), bcast(st2.t[:, :], 2, 64), ALU.mult, [yc.b, st2.b], [yn.b])
                pt, ptb = bank()
                for c_ in range(4):
                    TR(pt[:, c_ * 128:(c_ + 1) * 128], yn.t[:, c_ * 128:(c_ + 1) * 128], ident, [yn.b, cst.b], [ptb])
                pg, pgb = bank()
                MM(pg[:, 0:n], GUP.t[:, hcols], SG.t[:, bl], True, True, [GUP.b, SG.b], [pgb])
                ACT(t1.t[:, :], pt[:, 0:n], AF.Identity, [ptb, lnw.b, lnb.b], [t1.b],
                    scale=lnw.t[:, l, hp:hp + 1], bias=lnb.t[:, l, hp:hp + 1])
                TT("pool", t1.t[:, :], t1.t[:, :], BON.t[:, bl], ALU.add, [t1.b, BON.b], [t1.b])
                TT("dve", YAst.t[:, bl], t1.t[:, :], pg[:, 0:n], ALU.mult, [t1.b, pgb], [YAst.b])
            P.dma("sp", ya_s[:, hp, :], YAst.t[:, :], reads=[YAst.b], writes=[ya_sb])

    ya_sb = Buf("ya_s")

    def lru_phase(l, H, Hb, YB, YBb):
        wch = [Tl(f"lwch{i}", [128, 8, 128], BF16) for i in range(4)]
        wrr = [0]

        def next_w(c0):
            tl = wch[wrr[0] % 4]
            wrr[0] += 1
            P.dma("pool", tl.t[:, :, :], win_cols(l, c0), writes=[tl.b])
            return tl
        wrg = [Tl(f"wrg{i}", [128, 2, 2, 128], BF16) for i in range(2)]
        GT = 2048
        Aa = [Tl(f"lA{d}", [128, GT], F32) for d in range(2)]
        Uu = [Tl(f"lU{d}", [128, GT], F32) for d in range(2)]
        HF = Tl("lHF", [128, GT], F32)
        GL = Tl("lGL", [128, GT], F32)
        xc = Tl("lxc", [128, 512], F32)
        xcb = Tl("lxcb", [128, 512], BF16)
        g1 = Tl("lg1", [128, 512], F32)
        rg = Tl("lrg", [128, 512], F32)
        ig = Tl("lig", [128, 512], F32)
        a2 = Tl("la2", [128, 512], F32)
        groups = [(BLK5[0:4], [(0, SEQS[0])], 0, 2048), (BLK5[4:5], [(1, SEQS[1]), (2, SEQS[2])], 2048, 512)]
        for j in range(8):
            jc = slice(j, j + 1)
            wx = next_w(3328 + j * 128)
            wg = next_w(4352 + j * 128)
            wr_ = wrg[j % 2]
            for d in range(2):
                P.dma("pool", wr_.t[:, d, 0, :], Wd["lru_w_rg"][l, d, j], writes=[wr_.b])
                P.dma("pool", wr_.t[:, d, 1, :], Wd["lru_w_ig"][l, d, j], writes=[wr_.b])
            for (gblocks, gseqs, g0, gn) in groups:
                for (t0, n) in gblocks:
                    o = t0 - g0
                    ol = slice(o, o + n)
                    px, pxb = proj(wx.t, wx.b, H, Hb, t0, n)
                    pg_, pgb = proj(wg.t, wg.b, H, Hb, t0, n)
                    ACT(xc.t[:, :], px[:, 0:n], AF.Identity, [pxb, cw.b, cb.b], [xc.b],
                        scale=cw.t[:, 4 * l + 2, jc], bias=cb.t[:, l, jc])
                    xv3 = rowview(xc.t[:, 0:n], t0)
                    pv3 = rowview(px[:, 0:n], t0)
                    STT("dve", xv3[:, :, 2:], pv3[:, :, :-2], cw.t[:, 4 * l + 0, jc], xv3[:, :, 2:], ALU.mult, ALU.add,
                        [pxb, xc.b, cw.b], [xc.b])
                    STT("dve", xv3[:, :, 1:], pv3[:, :, :-1], cw.t[:, 4 * l + 1, jc], xv3[:, :, 1:], ALU.mult, ALU.add,
                        [pxb, xc.b, cw.b], [xc.b])
                    STT("dve", xv3[:, :, :-1], pv3[:, :, 1:], cw.t[:, 4 * l + 3, jc], xv3[:, :, :-1], ALU.mult, ALU.add,
                        [pxb, xc.b, cw.b], [xc.b])
                    CP("pool", xcb.t[:, :], xc.t[:, :], [xc.b], [xcb.b])
                    ACT(g1.t[:, :], pg_[:, 0:n], AF.Square, [pgb], [g1.b])
                    TS("pool", g1.t[:, :], g1.t[:, :], 0.044715, 1.0, ALU.mult, ALU.add, [g1.b], [g1.b])
                    TT("dve", g1.t[:, :], g1.t[:, :], pg_[:, 0:n], ALU.mult, [g1.b, pgb], [g1.b])
                    ACT(g1.t[:, :], g1.t[:, :], AF.Sigmoid, [g1.b], [g1.b], scale=1.5957691216057308)
                    TT("dve", GL.t[:, ol], g1.t[:, :], pg_[:, 0:n], ALU.mult, [g1.b, pgb], [GL.b])
                    for d in range(2):
                        mi = 2 * l + d
                        pr_, prb = bank()
                        MM(pr_[:, 0:n], wr_.t[:, d, 0, :], xcb.t[:, :], True, True, [wr_.b, xcb.b], [prb])
                        pi_, pib = bank()
                        MM(pi_[:, 0:n], wr_.t[:, d, 1, :], xcb.t[:, :], True, True, [wr_.b, xcb.b], [pib])
                        ACT(rg.t[:, :], pr_[:, 0:n], AF.Sigmoid, [prb, brg.b], [rg.b], bias=brg.t[:, mi, jc])
                        ACT(ig.t[:, :], pi_[:, 0:n], AF.Sigmoid, [pib, big.b], [ig.b], bias=big.t[:, mi, jc])
                        ACT(Aa[d].t[:, ol], rg.t[:, :], AF.Exp, [rg.b, c8.b], [Aa[d].b], scale=c8.t[:, mi, jc])
                        TT("pool", a2.t[:, :], Aa[d].t[:, ol], Aa[d].t[:, ol], ALU.mult, [Aa[d].b], [a2.b])
                        ACT(a2.t[:, :], a2.t[:, :], AF.Sqrt, [a2.b], [a2.b], scale=-1.0, bias=1.0)
                        TT("pool", a2.t[:, :], a2.t[:, :], ig.t[:, :], ALU.mult, [a2.b, ig.b], [a2.b])
                        TT("dve", Uu[d].t[:, ol], a2.t[:, :], xc.t[:, :], ALU.mult, [a2.b, xc.b], [Uu[d].b])
                for (si, (s0, slen, _)) in gseqs:
                    o = s0 - g0
                    sl = slice(o, o + slen)
                    i0 = h0l.t[:, 2 * l + 0, jc] if si == 0 else 0.0
                    i1 = h0l.t[:, 2 * l + 1, jc] if si == 0 else 0.0
                    SCAN(HF.t[:, sl], Aa[0].t[:, sl], Uu[0].t[:, sl], i0, [Aa[0].b, Uu[0].b, h0l.b], [HF.b])
                    SCAN(rev_last(Aa[0].t[:, sl]), rev_last(Aa[1].t[:, sl]), rev_last(Uu[1].t[:, sl]), i1,
                         [Aa[1].b, Uu[1].b, h0l.b, HF.b], [Aa[0].b])
                    if si > 0:
                        CP("act", lruo.t[:, si - 1, l, 0, jc], HF.t[:, o + slen - 1:o + slen], [HF.b], [lruo.b])
                        CP("act", lruo.t[:, si - 1, l, 1, jc], Aa[0].t[:, o:o + 1], [Aa[0].b], [lruo.b])
                TT("pool", HF.t[:, 0:gn], HF.t[:, 0:gn], Aa[0].t[:, 0:gn], ALU.add, [HF.b, Aa[0].b], [HF.b])
                TT("dve", YB.t[:, j, g0:g0 + gn], HF.t[:, 0:gn], GL.t[:, 0:gn], ALU.mult, [HF.b, GL.b],
                   [YBb[i] for i in range(g0 // 512, (g0 + gn) // 512)])
        for b in range(2):
            for d in range(2):
                ob = Buf("nsl")
                out_bufs.append(ob)
                P.dma("sp", ns_lru[b, l, d].rearrange("(k p) -> p k", p=128), lruo.t[:, b, l, d, :], reads=[lruo.b],
                      writes=[ob], allow_slow_non_contiguous=True)

    def merge_b(l, H, Hb, YB, YBb):
        WB = Tl("WB", [128, 8, D], BF16)
        WGB = Tl("WGB", [128, 8, D], BF16)
        mtmp = Tl("mtmp", [128, 8, 512], BF16)
        sg = Tl("msg", [128, 512], F32)
        for k in range(8):
            P.dma("pool", WB.t[:, k, :], Wd["w_proj_b"][l, k * 128:(k + 1) * 128, :], writes=[WB.b])
            P.dma("pool", WGB.t[:, k, :], Wd["w_in"][l, k * 128:(k + 1) * 128, 6400:7424], writes=[WGB.b])
        for (t0, n) in BLK5:
            ybb = YBb[t0 // 512]
            for j in range(8):
                js = slice(j * 128, (j + 1) * 128)
                pg_, pgb = proj(WGB.t[:, :, js], WGB.b, H, Hb, t0, n)
                pb_, pbb = proj(WB.t[:, :, js], WB.b, YB, ybb, t0, n)
                ACT(sg.t[:, :], pg_[:, 0:n], AF.Sigmoid, [pgb, bmer.b], [sg.b], bias=bmer.t[:, l, 8 + j:9 + j])
                TT("dve", mtmp.t[:, j, :], sg.t[:, :], pb_[:, 0:n], ALU.mult, [sg.b, pbb], [mtmp.b])
            CP("pool", YB.t[:, :, t0:t0 + n], mtmp.t[:, :, :], [mtmp.b], [ybb])

    def merge_a(l, H, Hb, YB, YBb):
        YA = Tl("YA", [128, 8, T], BF16)
        for k in range(8):
            P.dma("sp", YA.t[:, k, :], ya_s[:, k, :], reads=[ya_sb], writes=[YA.b])
        wch = [Tl(f"mwch{i}", [128, 8, 128], BF16) for i in range(4)]
        sg = Tl("msg2", [128, 512], F32)
        tt_ = Tl("mtt", [128, 512], F32)
        for j in range(8):
            wa = wch[(2 * j) % 4]
            wga = wch[(2 * j + 1) % 4]
            P.dma("pool", wa.t[:, :, :], Wd["w_proj_a"][l].rearrange("(k p) c -> p k c", p=128)[:, :, j * 128:(j + 1) * 128],
                  writes=[wa.b])
            P.dma("pool", wga.t[:, :, :], win_cols(l, 5376 + j * 128), writes=[wga.b])
            for (t0, n) in BLK5:
                ybb = YBb[t0 // 512]
                pg_, pgb = proj(wga.t, wga.b, H, Hb, t0, n)
                pa_, pab = proj(wa.t, wa.b, YA, YA.b, t0, n)
                ACT(sg.t[:, :], pg_[:, 0:n], AF.Sigmoid, [pgb, bmer.b], [sg.b], bias=bmer.t[:, l, j:j + 1])
                TT("dve", tt_.t[:, :], sg.t[:, :], pa_[:, 0:n], ALU.mult, [sg.b, pab], [tt_.b])
                TT("pool", YB.t[:, j, t0:t0 + n], tt_.t[:, :], YB.t[:, j, t0:t0 + n], ALU.add, [tt_.b, ybb], [ybb])

    def out_proj(l, YB, YBb):
        WO = Tl("WO", [128, 8, D], BF16)
        for k in range(8):
            P.dma("pool", WO.t[:, k, :], Wd["w_out"][l, k * 128:(k + 1) * 128, :], writes=[WO.b])
        O = Tl("O", [128, 8, 512], F32)
        rt = res_tmps(False)
        for (t0, n) in BLK5:
            for j in range(8):
                po, pob = proj(WO.t[:, :, j * 128:(j + 1) * 128], WO.b, YB, YBb[t0 // 512], t0, n)
                CP("act" if j % 2 else "dve", O.t[:, j, 0:n], po[:, 0:n], [pob], [O.b])
            residual_update(l, O.t[:, :, 0:n], O.b, G1, t0, n, False, rt)

    def ffn_phase(l):
        moe = (l % 2 == 1)
        final = (l == L - 1)
        if not moe:
            i = l // 2
            groups = [(Wd["ffn_w1"][i][:, g * 2048:(g + 1) * 2048], Wd["ffn_w3"][i][:, g * 2048:(g + 1) * 2048],
                       Wd["ffn_w2"][i][g * 2048:(g + 1) * 2048, :], 16, None) for g in range(2)]
        else:
            i = l // 2
            groups = [(Wd["moe_w1"][i, e], Wd["moe_w3"][i, e], Wd["moe_w2"][i, e], 28, e) for e in range(8)]
        halves = [[(0, 512), (512, 512), (1024, 256)], [(1280, 256), (1536, 512), (2048, 512)]]
        HT = 1280
        for hf, blocks in enumerate(halves):
            hoff = hf * HT
            with P.scope():
                H2 = Tl("H2", [128, 8, HT], BF16)
                OACC = Tl("OACC", [128, 8, HT], F32)
                GTt = Tl("GTt", [8, HT], F32)
                GB = Tl("GB", [128, HT], F32)
                with P.scope():
                    tmp = norm_tmps()
                    if moe:
                        hf32 = Tl("hf32", [128, 8, 512], F32)
                        RT = Tl("RT", [128, 8, 8], F32)
                        P.dma("sp", RT.t[:, :, :], Wd["moe_router"][i].rearrange("(k p) e -> p k e", p=128), writes=[RT.b])
                        lg = Tl("lg", [128, 8], F32)
                        lg2 = Tl("lg2", [128, 8], F32)
                        m1 = Tl("m1", [128, 1], F32)
                        m2 = Tl("m2", [128, 1], F32)

                        def gates(t0, n):
                            for tt in range(n // 128):
                                ts_ = slice(tt * 128, (tt + 1) * 128)
                                pl, plb = bank()
                                for k in range(8):
                                    MM(pl[:, 0:8], hf32.t[:, k, ts_], RT.t[:, k, :], k == 0, k == 7, [hf32.b, RT.b], [plb])
                                CP("act", lg.t[:, :], pl[:, 0:8], [plb], [lg.b])
                                RMAX(m1.t[:, :], lg.t[:, :], [lg.b], [m1.b])
                                TS("dve", lg2.t[:, :], lg.t[:, :], m1.t[:, 0:1], None, ALU.is_equal, None, [lg.b, m1.b], [lg2.b])
                                STT("dve", lg2.t[:, :], lg2.t[:, :], -1e30, lg.t[:, :], ALU.mult, ALU.add, [lg2.b, lg.b], [lg2.b])
                                RMAX(m2.t[:, :], lg2.t[:, :], [lg2.b], [m2.b])
                                TS("dve", lg2.t[:, :], lg.t[:, :], m2.t[:, 0:1], None, ALU.is_ge, None, [lg.b, m2.b], [lg2.b])
                                TS("dve", m1.t[:, :], m1.t[:, :], -1.0, None, ALU.mult, None, [m1.b], [m1.b])
                                ACT(lg.t[:, :], lg.t[:, :], AF.Exp, [lg.b, m1.b], [lg.b], bias=m1.t[:, 0:1])
                                TT("dve", lg.t[:, :], lg.t[:, :], lg2.t[:, :], ALU.mult, [lg.b, lg2.b], [lg.b])
                                RSUM(m2.t[:, :], lg.t[:, :], [lg.b], [m2.b])
                                P.op("dve", lambda e: e.reciprocal(m2.t[:, :], m2.t[:, :]), [m2.b], [m2.b])
                                TS("dve", lg.t[:, :], lg.t[:, :], m2.t[:, 0:1], None, ALU.mult, None, [lg.b, m2.b], [lg.b])
                                pt, ptb = bank()
                                TR(pt[0:8, 0:128], lg.t[:, :], ident, [lg.b, cst.b], [ptb])
                                o = t0 - hoff + tt * 128
                                CP("act", GTt.t[0:8, o:o + 128], pt[0:8, 0:128], [ptb], [GTt.b])
                        norm_mod(l, A2, B2, H2, H2.b, blocks, hoff, tmp, hf32=hf32, cbk=gates)
                    else:
                        norm_mod(l, A2, B2, H2, H2.b, blocks, hoff, tmp)
                with P.scope():
                    HID = Tl("HID", [128, 28, HT], BF16)
                    w13 = [Tl(f"w13_{i_}", [128, 8, 128], BF16) for i_ in range(4)]
                    w2t = [Tl(f"w2t{i_}", [128, 28, 128], BF16) for i_ in range(2)]
                    sil = [Tl(f"sil{i_}", [128, 512], F32) for i_ in range(2)]
                    wc = [0]
                    for gi, (w1, w3, w2, nf, ex) in enumerate(groups):
                        if ex is not None:
                            for (t0, n) in blocks:
                                o = t0 - hoff
                                pg, pgb = bank()
                                MM(pg[:, 0:n], cst.t[0:8, C_ES + ex * 128:C_ES + (ex + 1) * 128], GTt.t[0:8, o:o + n], True, True,
                                   [cst.b, GTt.b], [pgb])
                                CP("act", GB.t[:, o:o + n], pg[:, 0:n], [pgb], [GB.b])
                        w1v = w1.rearrange("(k p) c -> p k c", p=128)
                        w3v = w3.rearrange("(k p) c -> p k c", p=128)
                        for f in range(nf):
                            fs = slice(f * 128, (f + 1) * 128)
                            w1t = w13[wc[0] % 4]
                            w3t = w13[(wc[0] + 1) % 4]
                            wc[0] += 2
                            P.dma("pool", w1t.t[:, :, :], w1v[:, :, fs], writes=[w1t.b])
                            P.dma("pool", w3t.t[:, :, :], w3v[:, :, fs], writes=[w3t.b])
                            for bi, (t0, n) in enumerate(blocks):
                                o = t0 - hoff
                                s_ = sil[(f + bi) % 2]
                                p1, p1b = proj(w1t.t, w1t.b, H2, H2.b, t0, n, hoff)
                                p3, p3b = proj(w3t.t, w3t.b, H2, H2.b, t0, n, hoff)
                                ACT(s_.t[:, 0:n], p1[:, 0:n], AF.Silu, [p1b], [s_.b])
                                if ex is None:
                                    TT("dve", HID.t[:, f, o:o + n], s_.t[:, 0:n], p3[:, 0:n], ALU.mult, [s_.b, p3b], [HID.b])
                                else:
                                    TT("dve", s_.t[:, 0:n], s_.t[:, 0:n], p3[:, 0:n], ALU.mult, [s_.b, p3b], [s_.b])
                                    TT("pool", HID.t[:, f, o:o + n], s_.t[:, 0:n], GB.t[:, o:o + n], ALU.mult, [s_.b, GB.b], [HID.b])
                        w2v = w2.rearrange("(f p) c -> p f c", p=128)
                        for j in range(8):
                            wt2 = w2t[j % 2]
                            P.dma("pool", wt2.t[:, 0:nf, :], w2v[:, :, j * 128:(j + 1) * 128], writes=[wt2.b])
                            for (t0, n) in blocks:
                                o = t0 - hoff
                                po, pob = bank()
                                for f in range(nf):
                                    MM(po[:, 0:n], wt2.t[:, f, :], HID.t[:, f, o:o + n], f == 0, f == nf - 1, [wt2.b, HID.b], [pob])
                                if gi == 0:
                                    CP("act", OACC.t[:, j, o:o + n], po[:, 0:n], [pob], [OACC.b])
                                else:
                                    TT("dve", OACC.t[:, j, o:o + n], po[:, 0:n], OACC.t[:, j, o:o + n], ALU.add, [pob, OACC.b], [OACC.b])
                with P.scope():
                    rt = res_tmps(final)
                    for (t0, n) in blocks:
                        o = t0 - hoff
                        residual_update(l, OACC.t[:, :, o:o + n], OACC.b, G2, t0, n, final, rt)

    for l in range(L):
        with P.scope():
            YB = Tl("YB", [128, 8, T], BF16)
            YBb = [Buf(f"YB{i}") for i in range(NBLK)]
            with P.scope():
                H = Tl("H", [128, 8, T], BF16)
                Hb = [Buf(f"H{i}") for i in range(NBLK)]
                with P.scope():
                    norm_mod(l, A1, B1, H, Hb, BLK5, 0, norm_tmps())
                with P.scope():
                    rwkv_phase(l, H, Hb, YB)
                with P.scope():
                    lru_phase(l, H, Hb, YB, YBb)
                with P.scope():
                    merge_b(l, H, Hb, YB, YBb)
                with P.scope():
                    merge_a(l, H, Hb, YB, YBb)
            with P.scope():
                out_proj(l, YB, YBb)
        with P.scope():
            ffn_phase(l)

    P.barrier()
    P.emit()
    P.close()
    return nc


_CACHE = {}


def kernel(**inputs):
    inp = {k: np.ascontiguousarray(np.asarray(v)) for k, v in inputs.items()}
    if "nc" not in _CACHE:
        _CACHE["nc"] = build_program()
    nc = _CACHE["nc"]
    cst = make_consts()
    in_maps = []
    for c in range(NCORES):
        m = {}
        m["xin"] = np.ascontiguousarray(np.concatenate(
            [inp["x_sample"][c], inp["x_prompt"][2 * c], inp["x_prompt"][2 * c + 1]], axis=0))
        m["cond"] = np.ascontiguousarray(np.stack([inp["c"][c], inp["c_ctx"]], axis=0))
        m["st_rwkv"] = np.ascontiguousarray(inp["state_rwkv"][c])
        m["st_lru"] = np.ascontiguousarray(inp["state_lru"][c])
        for n_, _s in WEIGHT_SHAPES:
            m[n_] = inp[n_]
        m["cst"] = cst
        in_maps.append(m)
    res = run_bass_kernel_spmd(nc, in_maps, core_ids=list(range(NCORES)))
    R = res.results
    y_sample = np.stack([R[c]["y"][0:2048] for c in range(NCORES)], axis=0)
    y_prompt = np.concatenate([R[c]["y"][2048:2560].reshape(2, 256, D) for c in range(NCORES)], axis=0)
    ns_rwkv = np.concatenate([R[c]["ns_rwkv"] for c in range(NCORES)], axis=0)
    ns_lru = np.concatenate([R[c]["ns_lru"] for c in range(NCORES)], axis=0)
    return (y_prompt.astype(np.float32), y_sample.astype(np.float32), ns_rwkv.astype(np.float32), ns_lru.astype(np.float32))
```

```python
import contextlib
import numpy as np
import concourse.bass as bass
import concourse.mybir as mybir
from concourse.ap import AP
from concourse.bass_utils import run_bass_kernel_spmd

F32 = mybir.dt.float32
BF16 = mybir.dt.bfloat16
ALU = mybir.AluOpType
AF = mybir.ActivationFunctionType
AX = mybir.AxisListType

ENGS = ("pe", "act", "dve", "pool", "sp")
NCORES = 8
T = 2560
NBLK = 5
SEQS = [(0, 2048, 64), (2048, 256, 256), (2304, 256, 256)]
D = 1024
INC = 7424
L = 2


class Buf:
    __slots__ = ("name", "last_write", "reads")

    def __init__(self, name=""):
        self.name = name
        self.last_write = None
        self.reads = []


class Prog:
    def __init__(self, nc, n_dma_sems=(24, 8, 24)):
        self.nc = nc
        self.ops = {e: [] for e in ENGS}
        self.count = {e: 0 for e in ENGS}
        self.waited = {e: {} for e in ENGS}
        self.sems = {}
        self._ctx = []
        for e in ENGS:
            self.sems["E_" + e] = self._sem("E_" + e)
        self.dma_pool = {}
        for q, n in zip(("sp", "act", "pool"), n_dma_sems):
            self.dma_pool[q] = []
            for i in range(n):
                self.sems[f"D_{q}{i}"] = self._sem(f"D_{q}{i}")
                self.dma_pool[q].append([f"D_{q}{i}", 0])
        self.dma_rr = {q: 0 for q in ("sp", "act", "pool")}
        self.n_ops = 0

    def _sem(self, name):
        cm = self.nc.semaphore(name)
        s = cm.__enter__()
        self._ctx.append(cm)
        return s

    def sbuf(self, name, shape, dtype):
        self._uid = getattr(self, "_uid", 0) + 1
        name = f"s{self._uid}_{name}"
        cm = self.nc.sbuf_tensor(name, list(shape), dtype)
        t = cm.__enter__()
        self._ctx.append(cm)
        return t

    def psum(self, name, shape, dtype):
        cm = self.nc.psum_tensor(name, list(shape), dtype)
        t = cm.__enter__()
        self._ctx.append(cm)
        return t

    def _deps(self, eng, reads, writes):
        deps = {}
        own = "E_" + eng

        def add(tok, same_ok):
            if tok is None:
                return
            s, v = tok
            if s == own and not same_ok:
                return
            if deps.get(s, 0) < v:
                deps[s] = v

        for b in reads:
            add(b.last_write, eng != "pe")
        for b in writes:
            add(b.last_write, False)
            for t in b.reads:
                add(t, False)
        waits = []
        w = self.waited[eng]
        for s, v in deps.items():
            if w.get(s, 0) >= v:
                continue
            w[s] = v
            waits.append((s, v))
        return waits

    def _commit(self, tok, reads, writes):
        for b in reads:
            b.reads.append(tok)
            if len(b.reads) > 48:
                latest = {}
                for s, v in b.reads:
                    if latest.get(s, 0) < v:
                        latest[s] = v
                b.reads = list(latest.items())
        for b in writes:
            b.last_write = tok
            b.reads = []

    def op(self, eng, fn, reads=(), writes=()):
        waits = self._deps(eng, reads, writes)
        self.count[eng] += 1
        tok = ("E_" + eng, self.count[eng])
        self.ops[eng].append((waits, fn, tok))
        self._commit(tok, reads, writes)
        self.n_ops += 1
        return tok

    def dma(self, q, out, in_, reads=(), writes=(), **kw):
        pool = self.dma_pool[q]
        i = self.dma_rr[q]
        self.dma_rr[q] = (i + 1) % len(pool)
        ent = pool[i]
        semname = ent[0]
        waits = self._deps(q, reads, writes)
        if ent[1] > 0 and self.waited[q].get(semname, 0) < ent[1]:
            self.waited[q][semname] = ent[1]
            waits.append((semname, ent[1]))
        ent[1] += 16
        tok = (semname, ent[1])

        def fn(e, out=out, in_=in_, kw=kw):
            return e.dma_start(out=out, in_=in_, **kw)
        self.ops[q].append((waits, fn, tok))
        self._commit(tok, reads, writes)
        self.n_ops += 1
        return tok

    def mark(self, name):
        if not hasattr(self, "marks"):
            self.marks = []
        self.marks.append((name, dict(self.count)))

    def barrier(self):
        targets = []
        for e in ENGS:
            if self.count[e] > 0:
                targets.append(("E_" + e, self.count[e]))
        for q in self.dma_pool:
            for name, v in self.dma_pool[q]:
                if v > 0:
                    targets.append((name, v))
        for e in ENGS:
            waits = []
            for s, v in targets:
                if s == "E_" + e:
                    continue
                if self.waited[e].get(s, 0) >= v:
                    continue
                self.waited[e][s] = v
                waits.append((s, v))
            if waits:
                self.ops[e].append((waits, None, None))

    @contextlib.contextmanager
    def scope(self):
        n0 = len(self._ctx)
        yield
        self.barrier()
        while len(self._ctx) > n0:
            cm = self._ctx.pop()
            cm.__exit__(None, None, None)

    def emit(self):
        nc = self.nc
        engmap = {"pe": "tensor", "act": "scalar", "dve": "vector", "pool": "gpsimd", "sp": "sync"}
        with nc.Block() as block:
            for e in ENGS:
                lst = self.ops[e]
                if not lst:
                    continue

                def body(eng, lst=lst):
                    for waits, fn, tok in lst:
                        if fn is None:
                            for s, v in waits:
                                eng.wait_ge(self.sems[s], v)
                            continue
                        for s, v in waits[1:]:
                            eng.wait_ge(self.sems[s], v)
                        ins = fn(eng)
                        if waits:
                            ins._wait_ge(self.sems[waits[0][0]], waits[0][1])
                        ins.then_inc(self.sems[tok[0]], 16 if tok[0].startswith("D_") else 1)
                getattr(block, engmap[e])(body)

    def close(self):
        while self._ctx:
            cm = self._ctx.pop()
            cm.__exit__(None, None, None)


def rev_last(ap):
    dims = [list(d) for d in ap.ap]
    step, cnt = dims[-1]
    return AP(ap.tensor, ap.offset + step * (cnt - 1), dims[:-1] + [[-step, cnt]])


def bcast(ap, axis, n):
    dims = [list(d) for d in ap.ap]
    dims.insert(axis, [0, n])
    return AP(ap.tensor, ap.offset, dims)


WEIGHT_SHAPES = [
    ("mod_w", (L, D, 6 * D)), ("mod_b", (L, 6 * D)),
    ("norm_pre_mix", (L, D)), ("norm_post_mix", (L, D)), ("norm_pre_ffn", (L, D)), ("norm_post_ffn", (L, D)),
    ("w_in", (L, D, INC)), ("b_merge", (L, 2 * D)),
    ("rwkv_mu", (L, 2, 3200)), ("rwkv_w0", (L, 2, D)), ("rwkv_w_up", (L, 2, 64, D)),
    ("rwkv_a0", (L, 2, D)), ("rwkv_a_up", (L, 2, 64, D)), ("rwkv_k_k", (L, D)), ("rwkv_k_a", (L, D)),
    ("rwkv_r_k", (L, 16, 64)), ("rwkv_g_up", (L, 128, D)), ("rwkv_lnx_w", (L, D)), ("rwkv_lnx_b", (L, D)),
    ("lru_conv_w", (L, 4, D)), ("lru_conv_b", (L, D)), ("lru_w_rg", (L, 2, 8, 128, 128)), ("lru_b_rg", (L, 2, D)),
    ("lru_w_ig", (L, 2, 8, 128, 128)), ("lru_b_ig", (L, 2, D)), ("lru_lam", (L, 2, D)),
    ("w_proj_a", (L, D, D)), ("w_proj_b", (L, D, D)), ("w_out", (L, D, D)),
    ("ffn_w1", (1, D, 4096)), ("ffn_w3", (1, D, 4096)), ("ffn_w2", (1, 4096, D)),
    ("moe_router", (1, D, 8)), ("moe_w1", (1, 8, D, 3584)), ("moe_w3", (1, 8, D, 3584)), ("moe_w2", (1, 8, 3584, D)),
]

C_ID = 0
C_US = 128
C_UI = 256
C_LS = 384
C_LI = 512
C_MF = 640
C_MB = 1152
C_ES = 1664
C_LM = C_ES + 8 * 128
C_LMT = C_LM + 7 * 128
NCST32 = C_LM
NCST = C_LMT + 7 * 128


def make_consts():
    c = np.zeros((128, NCST), np.float32)
    p = np.arange(128)[:, None]
    q = np.arange(128)[None, :]
    c[:, C_ID:C_ID + 128] = (p == q)
    c[:, C_US:C_US + 128] = (p < q)
    c[:, C_UI:C_UI + 128] = (p <= q)
    c[:, C_LS:C_LS + 128] = (p > q)
    c[:, C_LI:C_LI + 128] = (p >= q)
    t = np.arange(512)
    c[:, C_MF:C_MF + 512] = (t % 128 != 0)[None, :]
    c[:, C_MB:C_MB + 512] = (t % 128 != 127)[None, :]
    for e in range(8):
        c[e, C_ES + e * 128:C_ES + (e + 1) * 128] = 1.0
    for i in range(7):
        b = 1 << i
        m = ((p // (2 * b)) == (q // (2 * b))) & ((p % (2 * b)) < b) & ((q % (2 * b)) >= b)
        c[:, C_LM + i * 128:C_LM + (i + 1) * 128] = m
        c[:, C_LMT + i * 128:C_LMT + (i + 1) * 128] = m.T
    return c


def build_program(debug=False):
    nc = bass.Bass("TRN2", target_bir_lowering=False)

    def din(name, shape):
        return nc.dram_tensor(name, list(shape), F32, kind="ExternalInput").ap()

    def dout(name, shape):
        return nc.dram_tensor(name, list(shape), F32, kind="ExternalOutput").ap()

    xin = din("xin", [T, D])
    cond = din("cond", [2, D])
    st_rwkv = din("st_rwkv", [L, 2, 16, 64, 64])
    st_lru = din("st_lru", [L, 2, D])
    Wd = {n: din(n, s) for n, s in WEIGHT_SHAPES}
    cstd = din("cst", [128, NCST])
    y_out = dout("y", [T, D])
    ns_rwkv = dout("ns_rwkv", [2, L, 2, 16, 64, 64])
    ns_lru = dout("ns_lru", [2, L, 2, D])
    xs = nc.dram_tensor("xs_scr", [128, 8, T], F32).ap()
    ya_s = nc.dram_tensor("ya_scr", [128, 8, T], BF16).ap()

    P = Prog(nc)
    out_bufs = []

    def ACT(out, in_, func, R, Wr, scale=None, bias=None):
        kw = {}
        if scale is not None:
            kw["scale"] = scale
        if bias is not None:
            kw["bias"] = bias
        P.op("act", lambda e: e.activation(out=out, in_=in_, func=func, **kw), R, Wr)

    def TT(eng, out, in0, in1, op, R, Wr):
        P.op(eng, lambda e: e.tensor_tensor(out, in0, in1, op), R, Wr)

    def TS(eng, out, in0, s1, s2, op0, op1, R, Wr):
        if s2 is None:
            P.op(eng, lambda e: e.tensor_scalar(out, in0, s1, None, op0), R, Wr)
        else:
            P.op(eng, lambda e: e.tensor_scalar(out, in0, s1, s2, op0, op1), R, Wr)

    def STT(eng, out, in0, sc, in1, op0, op1, R, Wr):
        P.op(eng, lambda e: e.scalar_tensor_tensor(out, in0, sc, in1, op0, op1), R, Wr)

    def CP(eng, out, in_, R, Wr):
        if eng == "act":
            P.op("act", lambda e: e.activation(out=out, in_=in_, func=AF.Copy), R, Wr)
        else:
            P.op(eng, lambda e: e.tensor_copy(out, in_), R, Wr)

    def MM(out, lhsT, rhs, start, stop, R, Wr):
        P.op("pe", lambda e: e.matmul(out, lhsT, rhs, start=start, stop=stop), R, Wr)

    def TR(out, in_, ident, R, Wr):
        P.op("pe", lambda e: e.transpose(out, in_, ident), R, Wr)

    def MSET(eng, ap, val, Wr):
        P.op(eng, lambda e: e.memset(ap, val), (), Wr)

    class Tl:
        def __init__(self, name, shape, dtype):
            self.t = P.sbuf(name, shape, dtype)
            self.b = Buf(name)

    banks = [P.psum(f"bank{i}", [128, 512], F32) for i in range(8)]
    bbufs = [Buf(f"bank{i}") for i in range(8)]
    rr = [0]

    def bank():
        i = rr[0]
        rr[0] = (i + 1) % 8
        return banks[i], bbufs[i]

    NE = 1.0 / D

    cst = Tl("cst", [128, NCST32], F32)
    P.dma("sp", cst.t[:, :], cstd[:, 0:NCST32], writes=[cst.b])
    lmk = Tl("lmk", [128, 14 * 128], BF16)
    P.dma("pool", lmk.t[:, :], cstd[:, C_LM:NCST], writes=[lmk.b])
    ident = cst.t[:, C_ID:C_ID + 128]
    ones_bf = Tl("ones_bf", [128, 128], BF16)
    MSET("pool", ones_bf.t[:, :], 1.0, [ones_bf.b])
    bd_bf = Tl("bd_bf", [128, 128], BF16)
    MSET("pool", bd_bf.t[:, :], 0.0, [bd_bf.b])
    MSET("pool", bd_bf.t[0:64, 0:64], 1.0, [bd_bf.b])
    MSET("pool", bd_bf.t[64:128, 64:128], 1.0, [bd_bf.b])
    ident_bf = Tl("ident_bf", [128, 128], BF16)
    CP("dve", ident_bf.t[:, :], ident, [cst.b], [ident_bf.b])

    def load_fm(name, src2d, n):
        cols = src2d.shape[1] // 128
        tl = Tl(name, [128, n, cols], F32)
        for i in range(n):
            P.dma("sp", tl.t[:, i, :], src2d[i].rearrange("(k p) -> p k", p=128), writes=[tl.b],
                  allow_slow_non_contiguous=True)
        return tl

    npm = load_fm("npm", Wd["norm_pre_mix"], L)
    npo = load_fm("npo", Wd["norm_post_mix"], L)
    npf = load_fm("npf", Wd["norm_pre_ffn"], L)
    npg = load_fm("npg", Wd["norm_post_ffn"], L)
    modb = load_fm("modb", Wd["mod_b"], L)
    bmer = load_fm("bmer", Wd["b_merge"], L)
    mu = load_fm("mu", Wd["rwkv_mu"].rearrange("l d c -> (l d) c"), 2 * L)
    w0 = load_fm("w0", Wd["rwkv_w0"].rearrange("l d c -> (l d) c"), 2 * L)
    a0 = load_fm("a0", Wd["rwkv_a0"].rearrange("l d c -> (l d) c"), 2 * L)
    kk_ = load_fm("kk_", Wd["rwkv_k_k"], L)
    ka_ = load_fm("ka_", Wd["rwkv_k_a"], L)
    rk_ = load_fm("rk_", Wd["rwkv_r_k"].rearrange("l h c -> l (h c)"), L)
    lnw = load_fm("lnw", Wd["rwkv_lnx_w"], L)
    lnb = load_fm("lnb", Wd["rwkv_lnx_b"], L)
    cw = load_fm("cw", Wd["lru_conv_w"].rearrange("l j c -> (l j) c"), 4 * L)
    cb = load_fm("cb", Wd["lru_conv_b"], L)
    brg = load_fm("brg", Wd["lru_b_rg"].rearrange("l d c -> (l d) c"), 2 * L)
    big = load_fm("big", Wd["lru_b_ig"].rearrange("l d c -> (l d) c"), 2 * L)
    lam = load_fm("lam", Wd["lru_lam"].rearrange("l d c -> (l d) c"), 2 * L)
    h0l = load_fm("h0l", st_lru.rearrange("l d c -> (l d) c"), 2 * L)

    omu = Tl("omu", [128, 2 * L, 25], F32)
    TS("dve", omu.t[:, :, :], mu.t[:, :, :], -1.0, 1.0, ALU.mult, ALU.add, [mu.b], [omu.b])
    omka = Tl("omka", [128, L, 8], F32)
    TS("dve", omka.t[:, :, :], ka_.t[:, :, :], -1.0, 1.0, ALU.mult, ALU.add, [ka_.b], [omka.b])
    c8 = Tl("c8", [128, 2 * L, 8], F32)
    ACT(c8.t[:, :, :], lam.t[:, :, :], AF.Exp, [lam.b], [c8.b], scale=-1.0)
    ACT(c8.t[:, :, :], c8.t[:, :, :], AF.Ln, [c8.b], [c8.b], bias=1.0)
    TS("dve", c8.t[:, :, :], c8.t[:, :, :], -8.0, None, ALU.mult, None, [c8.b], [c8.b])

    A1 = Tl("A1", [128, L, 2, 8], F32)
    B1 = Tl("B1", [128, L, 2, 8], F32)
    G1 = Tl("G1", [128, L, 2, 8], F32)
    A2 = Tl("A2", [128, L, 2, 8], F32)
    B2 = Tl("B2", [128, L, 2, 8], F32)
    G2 = Tl("G2", [128, L, 2, 8], F32)
    with P.scope():
        condT = Tl("condT", [128, 8, 2], F32)
        for g in range(2):
            P.dma("sp", condT.t[:, :, g], cond[g].rearrange("(k p) -> p k", p=128), writes=[condT.b],
                  allow_slow_non_contiguous=True)
        scond = Tl("scond", [128, 8, 2], F32)
        ACT(scond.t[:, :, :], condT.t[:, :, :], AF.Silu, [condT.b], [scond.b])
        modv = Tl("modv", [128, L, 48, 2], F32)
        mwt = [Tl(f"mwt{i}", [128, 8, 512], F32) for i in range(2)]
        for l in range(L):
            pb, pbb = bank()
            for cbk in range(12):
                wt = mwt[cbk % 2]
                src = Wd["mod_w"][l].rearrange("(k p) c -> p k c", p=128)[:, :, cbk * 512:(cbk + 1) * 512]
                P.dma("sp", wt.t[:, :, :], src, writes=[wt.b])
                for jj in range(4):
                    col = (cbk * 4 + jj) * 2
                    for k in range(8):
                        MM(pb[:, col:col + 2], wt.t[:, k, jj * 128:(jj + 1) * 128], scond.t[:, k, :],
                           k == 0, k == 7, [wt.b, scond.b], [pbb])
            for g in range(2):
                src = pb[:, 0:96].rearrange("p (c g) -> p c g", g=2)[:, :, g]
                TT("dve", modv.t[:, l, :, g], src, modb.t[:, l, :], ALU.add, [pbb, modb.b], [modv.b])
            for g in range(2):
                mv = modv.t[:, l, :, g]
                STT("dve", A1.t[:, l, g, :], mv[:, 8:16], 1.0, npm.t[:, l, :], ALU.add, ALU.mult, [modv.b, npm.b], [A1.b])
                CP("dve", B1.t[:, l, g, :], mv[:, 0:8], [modv.b], [B1.b])
                TT("dve", G1.t[:, l, g, :], mv[:, 16:24], npo.t[:, l, :], ALU.mult, [modv.b, npo.b], [G1.b])
                STT("dve", A2.t[:, l, g, :], mv[:, 32:40], 1.0, npf.t[:, l, :], ALU.add, ALU.mult, [modv.b, npf.b], [A2.b])
                CP("dve", B2.t[:, l, g, :], mv[:, 24:32], [modv.b], [B2.b])
                TT("dve", G2.t[:, l, g, :], mv[:, 40:48], npg.t[:, l, :], ALU.mult, [modv.b, npg.b], [G2.b])

    xs_b = [Buf(f"xs{i}") for i in range(NBLK)]
    with P.scope():
        xrow = [Tl(f"xrow{i}", [128, D], F32) for i in range(2)]
        xfm = [Tl(f"xfm{i}", [128, 8, 512], F32) for i in range(2)]
        for tb in range(NBLK):
            xf = xfm[tb % 2]
            for tt in range(4):
                xr_ = xrow[(tb * 4 + tt) % 2]
                r0 = tb * 512 + tt * 128
                P.dma("sp", xr_.t[:, :], xin[r0:r0 + 128, :], writes=[xr_.b])
                for half in range(2):
                    pb, pbb = bank()
                    for kk2 in range(4):
                        k = half * 4 + kk2
                        TR(pb[:, kk2 * 128:(kk2 + 1) * 128], xr_.t[:, k * 128:(k + 1) * 128], ident, [xr_.b, cst.b], [pbb])
                    dst = xf.t[:, half * 4:(half + 1) * 4, tt * 128:(tt + 1) * 128]
                    src = pb[:, :].rearrange("p (k t) -> p k t", t=128)
                    if half == 0:
                        CP("act", dst, src, [pbb], [xf.b])
                    else:
                        CP("dve", dst, src, [pbb], [xf.b])
            P.dma("sp", xs[:, :, tb * 512:(tb + 1) * 512], xf.t[:, :, :], reads=[xf.b], writes=[xs_b[tb]])

    def blk_group(t0):
        return 0 if t0 < 2048 else 1

    def SCAN(out, d0, d1, init, R, Wr):
        P.op("dve", lambda e: e.tensor_tensor_scan(out, d0, d1, init, ALU.mult, ALU.add), R, Wr)

    def RSUM(out, in_, R, Wr):
        P.op("dve", lambda e: e.reduce_sum(out, in_, AX.X), R, Wr)

    def RMAX(out, in_, R, Wr):
        P.op("dve", lambda e: e.reduce_max(out, in_, AX.X), R, Wr)

    def hbuf(Hb, t0):
        return Hb[t0 // 512] if isinstance(Hb, list) else Hb

    def norm_mod(l, A, B, H, Hb, blocks, hoff, tmp, hf32=None, cbk=None):
        xbt, sq_t, rstd_t, xn_t = tmp
        for bi, (t0, n) in enumerate(blocks):
            g = blk_group(t0)
            xb = xbt[bi % 2]
            P.dma("sp", xb.t[:, :, 0:n], xs[:, :, t0:t0 + n], reads=[xs_b[t0 // 512]], writes=[xb.b])
            ACT(sq_t.t[:, :, 0:n], xb.t[:, :, 0:n], AF.Square, [xb.b], [sq_t.b])
            pb, pbb = bank()
            for k in range(8):
                MM(pb[:, 0:n], ones_bf.t[:, :], sq_t.t[:, k, 0:n], k == 0, k == 7, [ones_bf.b, sq_t.b], [pbb])
            TS("dve", rstd_t.t[:, 0:n], pb[:, 0:n], NE, 1e-6, ALU.mult, ALU.add, [pbb], [rstd_t.b])
            ACT(rstd_t.t[:, 0:n], rstd_t.t[:, 0:n], AF.Ln, [rstd_t.b], [rstd_t.b])
            ACT(rstd_t.t[:, 0:n], rstd_t.t[:, 0:n], AF.Exp, [rstd_t.b], [rstd_t.b], scale=-0.5)
            TT("dve", xn_t.t[:, :, 0:n], xb.t[:, :, 0:n], bcast(rstd_t.t[:, 0:n], 1, 8), ALU.mult,
               [xb.b, rstd_t.b], [xn_t.b])
            for k in range(8):
                dst = H.t[:, k, t0 - hoff:t0 - hoff + n]
                ACT(dst, xn_t.t[:, k, 0:n], AF.Identity, [xn_t.b, A.b, B.b], [hbuf(Hb, t0)],
                    scale=A.t[:, l, g, k:k + 1], bias=B.t[:, l, g, k:k + 1])
                if hf32 is not None:
                    ACT(hf32.t[:, k, 0:n], xn_t.t[:, k, 0:n], AF.Identity, [xn_t.b, A.b, B.b], [hf32.b],
                        scale=A.t[:, l, g, k:k + 1], bias=B.t[:, l, g, k:k + 1])
            if cbk is not None:
                cbk(t0, n)

    def norm_tmps():
        return ([Tl(f"xb{i}", [128, 8, 512], F32) for i in range(2)], Tl("sq", [128, 8, 512], BF16),
                Tl("rstd", [128, 512], F32), Tl("xn", [128, 8, 512], F32))

    def win_cols(l, c0, n=128):
        return Wd["w_in"][l].rearrange("(k p) c -> p k c", p=128)[:, :, c0:c0 + n]

    def proj(wt3, wb, H, Hb, t0, n, hoff=0):
        pb, pbb = bank()
        for k in range(8):
            MM(pb[:, 0:n], wt3[:, k, :], H.t[:, k, t0 - hoff:t0 - hoff + n], k == 0, k == 7, [wb, hbuf(Hb, t0)], [pbb])
        return pb, pbb

    def rowview(ap2d, t0):
        w = 64 if t0 < 2048 else 256
        return ap2d.rearrange("p (r w) -> p r w", w=w)

    def mix(pb, pbb, dst, mu_ap, omu_ap, d, t0, n):
        ACT(dst.t[:, 0:n], pb[:, 0:n], AF.Identity, [pbb, mu.b, omu.b], [dst.b], scale=omu_ap)
        dv = rowview(dst.t[:, 0:n], t0)
        sv = rowview(pb[:, 0:n], t0)
        if d == 0:
            STT("dve", dv[:, :, 1:], sv[:, :, :-1], mu_ap, dv[:, :, 1:], ALU.mult, ALU.add, [pbb, dst.b, mu.b], [dst.b])
        else:
            STT("dve", dv[:, :, :-1], sv[:, :, 1:], mu_ap, dv[:, :, :-1], ALU.mult, ALU.add, [pbb, dst.b, mu.b], [dst.b])

    BLK5 = [(i * 512, 512) for i in range(NBLK)]
    lruo = Tl("lruo", [128, 2, L, 2, 8], F32)

    def residual_update(l, O, Ob, G, t0, n, final, tmp):
        xb, sq_t, rstd_t, on_t, yrow = tmp
        g = blk_group(t0)
        P.dma("sp", xb.t[:, :, 0:n], xs[:, :, t0:t0 + n], reads=[xs_b[t0 // 512]], writes=[xb.b])
        ACT(sq_t.t[:, :, 0:n], O, AF.Square, [Ob], [sq_t.b])
        pb, pbb = bank()
        for k in range(8):
            MM(pb[:, 0:n], ones_bf.t[:, :], sq_t.t[:, k, 0:n], k == 0, k == 7, [ones_bf.b, sq_t.b], [pbb])
        TS("dve", rstd_t.t[:, 0:n], pb[:, 0:n], NE, 1e-6, ALU.mult, ALU.add, [pbb], [rstd_t.b])
        ACT(rstd_t.t[:, 0:n], rstd_t.t[:, 0:n], AF.Ln, [rstd_t.b], [rstd_t.b])
        ACT(rstd_t.t[:, 0:n], rstd_t.t[:, 0:n], AF.Exp, [rstd_t.b], [rstd_t.b], scale=-0.5)
        TT("dve", on_t.t[:, :, 0:n], O, bcast(rstd_t.t[:, 0:n], 1, 8), ALU.mult, [Ob, rstd_t.b], [on_t.b])
        for k in range(8):
            STT("dve", xb.t[:, k, 0:n], on_t.t[:, k, 0:n], G.t[:, l, g, k:k + 1], xb.t[:, k, 0:n],
                ALU.mult, ALU.add, [on_t.b, G.b, xb.b], [xb.b])
        if not final:
            P.dma("sp", xs[:, :, t0:t0 + n], xb.t[:, :, 0:n], reads=[xb.b], writes=[xs_b[t0 // 512]])
        else:
            for tt in range(n // 128):
                yr = yrow[tt % len(yrow)]
                for half in range(2):
                    pt, ptb = bank()
                    for k4 in range(4):
                        k = half * 4 + k4
                        TR(pt[:, k4 * 128:(k4 + 1) * 128], xb.t[:, k, tt * 128:(tt + 1) * 128], ident, [xb.b, cst.b], [ptb])
                    CP("act" if half else "dve", yr.t[:, half * 512:(half + 1) * 512], pt[:, :], [ptb], [yr.b])
                ob = Buf("yout")
                out_bufs.append(ob)
                r0 = t0 + tt * 128
                P.dma("sp", y_out[r0:r0 + 128, :], yr.t[:, :], reads=[yr.b], writes=[ob])

    def res_tmps(final):
        return (Tl("rxb", [128, 8, 512], F32), Tl("rsq", [128, 8, 512], BF16), Tl("rrs", [128, 512], F32),
                Tl("ron", [128, 8, 512], F32), [Tl(f"yrow{i}", [128, D], F32) for i in range(1)] if final else None)

    def rwkv_phase(l, H, Hb, YB):
        LORA = Tl("LORA", [128, 2, T], BF16)
        SG = Tl("SG", [128, T], BF16)
        LW = Tl("LW", [128, 2, D], BF16)
        GUP = Tl("GUP", [128, D], BF16)
        for d in range(2):
            P.dma("pool", LW.t[0:64, d, :], Wd["rwkv_w_up"][l, d], writes=[LW.b])
            P.dma("pool", LW.t[64:128, d, :], Wd["rwkv_a_up"][l, d], writes=[LW.b])
        P.dma("pool", GUP.t[:, :], Wd["rwkv_g_up"][l], writes=[GUP.b])
        wch = [Tl(f"wch{i}", [128, 8, 128], BF16) for i in range(4)]
        wrr = [0]

        def next_w(c0):
            tl = wch[wrr[0] % 4]
            wrr[0] += 1
            P.dma("pool", tl.t[:, :, :], win_cols(l, c0), writes=[tl.b])
            return tl

        tn = ["xr", "xk", "xv", "lw", "aa", "cum", "e1", "e2", "kq", "kk", "kf", "bb"]
        tm = {n_: Tl("t_" + n_, [128, 512], F32) for n_ in tn}
        sqb = Tl("sqb", [128, 512], BF16)
        rkb = Tl("rkb", [128, 512], BF16)

        ybf = YB.t[:, :, :].rearrange("p a t -> p (a t)")
        ybuf = Buf("ybalias")

        class V:
            def __init__(self, ap):
                self.t = ap
                self.b = Buf()
        QR = V(ybf[:, 0:5120].rearrange("p (c x) -> p c x", x=256))
        KTz = V(ybf[:, 5120:10240].rearrange("p (h t) -> p h t", h=2))
        BTz = V(ybf[:, 10240:15360].rearrange("p (h t) -> p h t", h=2))
        VT = V(ybf[:, 15360:17920].rearrange("p (c x) -> p c x", x=128))
        KH = V(ybf[:, 17920:20480].rearrange("p (c x) -> p c x", x=128))
        BH = Tl("BH", [128, 20, 128], BF16)
        WEND = Tl("WEND", [128, 20], F32)
        MSET("pool", KTz.t[:, :, :], 0.0, [KTz.b])
        MSET("pool", BTz.t[:, :, :], 0.0, [BTz.b])

        wt = next_w(3072)
        x0 = tm["xr"]
        for (t0, n) in BLK5:
            pb, pbb = proj(wt.t, wt.b, H, Hb, t0, n)
            for d in range(2):
                mi = 2 * l + d
                mix(pb, pbb, x0, mu.t[:, mi, 24:25], omu.t[:, mi, 24:25], d, t0, n)
                ACT(LORA.t[0:64, d, t0:t0 + n], x0.t[0:64, 0:n], AF.Tanh, [x0.b], [LORA.b])
                CP("dve", LORA.t[64:128, d, t0:t0 + n], x0.t[64:128, 0:n], [x0.b], [LORA.b])
        wt = next_w(3200)
        for (t0, n) in BLK5:
            pb, pbb = proj(wt.t, wt.b, H, Hb, t0, n)
            ACT(SG.t[:, t0:t0 + n], pb[:, 0:n], AF.Sigmoid, [pbb], [SG.b])

        BON = Tl("BON", [128, T], F32)
        YACC = Tl("YACC", [128, 20, 128], F32)
        YAst = Tl("YAst", [128, T], BF16)
        NPt = Tl("NPt", [128, 2, 256], BF16)
        MOt = Tl("MOt", [128, 2, 256], BF16)
        NTt = Tl("NTt", [128, 2, 128], BF16)
        TNs = [Tl(f"TN{i}", [128, 2, 128], BF16) for i in range(4)]
        Xa = Tl("Xa", [128, 2, 128], BF16)
        Xb = Tl("Xb", [128, 2, 128], BF16)
        Btb = Tl("Btb", [128, 128], BF16)
        Utb = Tl("Utb", [128, 128], BF16)
        S32 = Tl("S32", [128, 64], F32)
        SBz = Tl("SBz", [128, 2, 64], BF16)
        BD = Tl("BD", [128, 128], F32)
        OUTS = Tl("OUTS", [128, 64], F32)
        MSET("pool", SBz.t[:, :, :], 0.0, [SBz.b])
        MSET("pool", BD.t[:, :], 0.0, [BD.b])
        st1 = Tl("st1", [128, 8], F32)
        st2 = Tl("st2", [128, 8], F32)

        def chunk_step(c, d):
            cc = slice(c * 128, (c + 1) * 128)
            mNP = cst.t[:, C_US:C_US + 256] if d == 0 else cst.t[:, C_LS:C_LS + 256]
            mNT = cst.t[:, C_LS:C_LS + 128] if d == 0 else cst.t[:, C_US:C_US + 128]
            pNP, bNP = bank()
            for h in range(2):
                MM(pNP[:, h * 256:(h + 1) * 256], BTz.t[:, h, cc], QR.t[:, c, :], True, True, [BTz.b, QR.b], [bNP])
            pMO, bMO = bank()
            for h in range(2):
                MM(pMO[:, h * 256:(h + 1) * 256], KTz.t[:, h, cc], QR.t[:, c, :], True, True, [KTz.b, QR.b], [bMO])
            pNT, bNT = bank()
            for h in range(2):
                MM(pNT[:, h * 128:(h + 1) * 128], QR.t[:, c, 0:128], BTz.t[:, h, cc], True, True, [BTz.b, QR.b], [bNT])
            v3 = lambda ap, x: ap.rearrange("p (h x) -> p h x", x=x)
            TT("dve", NPt.t[:, :, :], v3(pNP[:, 0:512], 256), bcast(mNP, 1, 2), ALU.mult, [bNP, cst.b], [NPt.b])
            TT("dve", MOt.t[:, :, :], v3(pMO[:, 0:512], 256), bcast(mNP, 1, 2), ALU.mult, [bMO, cst.b], [MOt.b])
            TT("dve", NTt.t[:, :, :], v3(pNT[:, 0:256], 128), bcast(mNT, 1, 2), ALU.mult, [bNT, cst.b], [NTt.b])
            def lm(i, tr):
                nat = (d == 0) != tr
                base = (0 if nat else 7 * 128) + i * 128
                return lmk.t[:, base:base + 128]
            Tc, TTc = TNs[0], TNs[1]
            TT("pool", Xa.t[:, :, :], NPt.t[:, :, 0:128], bcast(lm(0, False), 1, 2), ALU.mult, [NPt.b, lmk.b], [Xa.b])
            TT("pool", Tc.t[:, :, 0:128], Xa.t[:, :, :], bcast(ident_bf.t[:, :], 1, 2), ALU.add, [Xa.b, ident_bf.b], [Tc.b])
            TT("pool", Xb.t[:, :, :], NTt.t[:, :, :], bcast(lm(0, True), 1, 2), ALU.mult, [NTt.b, lmk.b], [Xb.b])
            TT("pool", TTc.t[:, :, 0:128], Xb.t[:, :, :], bcast(ident_bf.t[:, :], 1, 2), ALU.add, [Xb.b, ident_bf.b], [TTc.b])
            for lev in range(1, 7):
                Tn, TTn = (TNs[2], TNs[3]) if lev % 2 == 1 else (TNs[0], TNs[1])
                pX, bX = bank()
                for h in range(2):
                    MM(pX[:, h * 128:(h + 1) * 128], NTt.t[:, h, :], Tc.t[:, h, 0:128], True, True, [NTt.b, Tc.b], [bX])
                for h in range(2):
                    MM(pX[:, 256 + h * 128:256 + (h + 1) * 128], NPt.t[:, h, 0:128], TTc.t[:, h, 0:128], True, True,
                       [NPt.b, TTc.b], [bX])
                TT("dve", Xa.t[:, :, :], v3(pX[:, 0:256], 128), bcast(lm(lev, False), 1, 2), ALU.mult, [bX, lmk.b], [Xa.b])
                TT("dve", Xb.t[:, :, :], v3(pX[:, 256:512], 128), bcast(lm(lev, True), 1, 2), ALU.mult, [bX, lmk.b], [Xb.b])
                pY, bY_ = bank()
                for h in range(2):
                    hc2 = slice(h * 128, (h + 1) * 128)
                    MM(pY[:, hc2], TTc.t[:, h, 0:128], Xa.t[:, h, :], True, False, [TTc.b, Xa.b], [bY_])
                    MM(pY[:, hc2], ident_bf.t[:, :], Tc.t[:, h, 0:128], False, True, [ident_bf.b, Tc.b], [bY_])
                for h in range(2):
                    hc2 = slice(256 + h * 128, 256 + (h + 1) * 128)
                    MM(pY[:, hc2], Tc.t[:, h, 0:128], Xb.t[:, h, :], True, False, [Tc.b, Xb.b], [bY_])
                    MM(pY[:, hc2], ident_bf.t[:, :], TTc.t[:, h, 0:128], False, True, [ident_bf.b, TTc.b], [bY_])
                CP("act", Tn.t[:, :, 0:128], v3(pY[:, 0:256], 128), [bY_], [Tn.b])
                CP("act", TTn.t[:, :, 0:128], v3(pY[:, 256:512], 128), [bY_], [TTn.b])
                Tc, TTc = Tn, TTn
            Tfin = Tc
            pBt, bBt = bank()
            for h in range(2):
                hc = slice(h * 64, (h + 1) * 64)
                MM(pBt[:, hc], QR.t[:, c, 0:128], SBz.t[:, h, :], True, False, [QR.b, SBz.b], [bBt])
                MM(pBt[:, hc], MOt.t[:, h, 0:128], VT.t[:, c, hc], False, True, [MOt.b, VT.b], [bBt])
            CP("act", Btb.t[:, :], pBt[:, 0:128], [bBt], [Btb.b])
            pU, bU = bank()
            for h in range(2):
                hc = slice(h * 64, (h + 1) * 64)
                MM(pU[:, hc], Tfin.t[:, h, 0:128], Btb.t[:, hc], True, True, [Tfin.b, Btb.b], [bU])
            CP("dve", Utb.t[:, :], pU[:, 0:128], [bU], [Utb.b])
            pY, bY = bank()
            for h in range(2):
                hc = slice(h * 64, (h + 1) * 64)
                MM(pY[:, hc], QR.t[:, c, 128:256], SBz.t[:, h, :], True, False, [QR.b, SBz.b], [bY])
                MM(pY[:, hc], NPt.t[:, h, 128:256], Utb.t[:, hc], False, False, [NPt.b, Utb.b], [bY])
                MM(pY[:, hc], MOt.t[:, h, 128:256], VT.t[:, c, hc], False, True, [MOt.b, VT.b], [bY])
            if d == 0:
                CP("act", YACC.t[:, c, :], pY[:, 0:128], [bY], [YACC.b])
            else:
                TT("dve", YACC.t[:, c, :], pY[:, 0:128], YACC.t[:, c, :], ALU.add, [bY, YACC.b], [YACC.b])
            pS, bS = bank()
            for h in range(2):
                hc = slice(h * 64, (h + 1) * 64)
                MM(pS[:, hc], BH.t[:, c, :], Utb.t[:, hc], True, False, [BH.b, Utb.b], [bS])
                MM(pS[:, hc], KH.t[:, c, :], VT.t[:, c, hc], False, True, [KH.b, VT.b], [bS])
            for h in range(2):
                hs = slice(h * 64, (h + 1) * 64)
                STT("dve", S32.t[hs, :], S32.t[hs, :], WEND.t[hs, c:c + 1], pS[hs, h * 64:(h + 1) * 64], ALU.mult, ALU.add,
                    [S32.b, WEND.b, bS], [S32.b])
            CP("act", SBz.t[0:64, 0, :], S32.t[0:64, :], [S32.b], [SBz.b])
            CP("act", SBz.t[64:128, 1, :], S32.t[64:128, :], [S32.b], [SBz.b])

        for hp in range(8):
            hcols = slice(hp * 128, (hp + 1) * 128)
            for d in range(2):
                mi = 2 * l + d
                wr = next_w(hp * 128)
                wk = next_w(1024 + hp * 128)
                wv = next_w(2048 + hp * 128)
                for (t0, n) in BLK5:
                    c4 = t0 // 128
                    bl = slice(t0, t0 + n)
                    xr, xk, xv, lw, aa, cum, e1, e2, kq, kkt, kf, bb = [tm[x_] for x_ in tn]
                    pr, prb = proj(wr.t, wr.b, H, Hb, t0, n)
                    mix(pr, prb, xr, mu.t[:, mi, hp:hp + 1], omu.t[:, mi, hp:hp + 1], d, t0, n)
                    pk, pkb = proj(wk.t, wk.b, H, Hb, t0, n)
                    mix(pk, pkb, xk, mu.t[:, mi, 8 + hp:9 + hp], omu.t[:, mi, 8 + hp:9 + hp], d, t0, n)
                    pv, pvb = proj(wv.t, wv.b, H, Hb, t0, n)
                    mix(pv, pvb, xv, mu.t[:, mi, 16 + hp:17 + hp], omu.t[:, mi, 16 + hp:17 + hp], d, t0, n)
                    pw, pwb = bank()
                    MM(pw[:, 0:n], LW.t[0:64, d, hcols], LORA.t[0:64, d, bl], True, True, [LW.b, LORA.b], [pwb])
                    pa, pab = bank()
                    MM(pa[:, 0:n], LW.t[64:128, d, hcols], LORA.t[64:128, d, bl], True, True, [LW.b, LORA.b], [pab])
                    ACT(lw.t[:, :], pw[:, 0:n], AF.Sigmoid, [pwb, w0.b], [lw.b], bias=w0.t[:, mi, hp:hp + 1])
                    ACT(aa.t[:, :], pa[:, 0:n], AF.Sigmoid, [pab, a0.b], [aa.b], bias=a0.t[:, mi, hp:hp + 1])
                    TS("pool", lw.t[:, :], lw.t[:, :], -0.6065306597126334, None, ALU.mult, None, [lw.b], [lw.b])
                    if d == 0:
                        SCAN(cum.t[:, :], cst.t[:, C_MF:C_MF + 512], lw.t[:, :], 0.0, [cst.b, lw.b], [cum.b])
                        cend = cum.t[:, :].rearrange("p (c x) -> p c x", x=128)[:, :, 127]
                    else:
                        SCAN(rev_last(cum.t[:, :]), rev_last(cst.t[:, C_MB:C_MB + 512]), rev_last(lw.t[:, :]), 0.0,
                             [cst.b, lw.b], [cum.b])
                        cend = cum.t[:, :].rearrange("p (c x) -> p c x", x=128)[:, :, 0]
                    c3 = lambda ap: ap.rearrange("p (c x) -> p c x", x=128)
                    ACT(WEND.t[:, c4:c4 + 4], cend, AF.Exp, [cum.b], [WEND.b])
                    TS("pool", kq.t[:, :], xk.t[:, :], kk_.t[:, l, hp:hp + 1], None, ALU.mult, None, [xk.b, kk_.b], [kq.b])
                    ACT(sqb.t[:, :], kq.t[:, :], AF.Square, [kq.b], [sqb.b])
                    pn, pnb = bank()
                    MM(pn[:, 0:n], bd_bf.t[:, :], sqb.t[:, :], True, True, [bd_bf.b, sqb.b], [pnb])
                    TS("dve", kkt.t[:, :], pn[:, 0:n], 1e-24, None, ALU.add, None, [pnb], [kkt.b])
                    ACT(kkt.t[:, :], kkt.t[:, :], AF.Ln, [kkt.b], [kkt.b])
                    ACT(kkt.t[:, :], kkt.t[:, :], AF.Exp, [kkt.b], [kkt.b], scale=-0.5)
                    TT("dve", kkt.t[:, :], kkt.t[:, :], kq.t[:, :], ALU.mult, [kkt.b, kq.b], [kkt.b])
                    TS("dve", kf.t[:, :], aa.t[:, :], ka_.t[:, l, hp:hp + 1], omka.t[:, l, hp:hp + 1], ALU.mult, ALU.add,
                       [aa.b, ka_.b, omka.b], [kf.b])
                    TT("pool", kf.t[:, :], kf.t[:, :], xk.t[:, :], ALU.mult, [kf.b, xk.b], [kf.b])
                    TT("pool", bb.t[:, :], kkt.t[:, :], aa.t[:, :], ALU.mult, [kkt.b, aa.b], [bb.b])
                    STT("dve", rkb.t[:, :], xr.t[:, :], rk_.t[:, l, hp:hp + 1], kf.t[:, :], ALU.mult, ALU.mult,
                        [xr.b, rk_.b, kf.b], [rkb.b])
                    pbn, pbnb = bank()
                    MM(pbn[:, 0:n], bd_bf.t[:, :], rkb.t[:, :], True, True, [bd_bf.b, rkb.b], [pbnb])
                    if d == 0:
                        TT("dve", BON.t[:, bl], pbn[:, 0:n], xv.t[:, :], ALU.mult, [pbnb, xv.b], [BON.b])
                    else:
                        TT("dve", kq.t[:, :], pbn[:, 0:n], xv.t[:, :], ALU.mult, [pbnb, xv.b], [kq.b])
                        TT("pool", BON.t[:, bl], BON.t[:, bl], kq.t[:, :], ALU.add, [BON.b, kq.b], [BON.b])
                    pt, ptb = bank()
                    for c_ in range(4):
                        TR(pt[:, c_ * 128:(c_ + 1) * 128], xv.t[:, c_ * 128:(c_ + 1) * 128], ident, [xv.b, cst.b], [ptb])
                    CP("act", VT.t[:, c4:c4 + 4, :], c3(pt[:, 0:512]), [ptb], [VT.b])
                    ACT(e1.t[:, :], cum.t[:, :], AF.Exp, [cum.b], [e1.b])
                    TT("dve", QR.t[:, c4:c4 + 4, 128:256], c3(xr.t[:, :]), c3(e1.t[:, :]), ALU.mult, [xr.b, e1.b], [QR.b])
                    TT("pool", e2.t[:, :], cum.t[:, :], lw.t[:, :], ALU.subtract, [cum.b, lw.b], [e2.b])
                    ACT(e2.t[:, :], e2.t[:, :], AF.Exp, [e2.b], [e2.b])
                    TT("dve", QR.t[:, c4:c4 + 4, 0:128], c3(kkt.t[:, :]), c3(e2.t[:, :]), ALU.mult, [kkt.b, e2.b], [QR.b])
                    ACT(e1.t[:, :], cum.t[:, :], AF.Exp, [cum.b], [e1.b], scale=-1.0)
                    for h in range(2):
                        hs = slice(h * 64, (h + 1) * 64)
                        TT("pool", KTz.t[hs, h, bl], kf.t[hs, :], e1.t[hs, :], ALU.mult, [kf.b, e1.b], [KTz.b])
                        STT("dve", BTz.t[hs, h, bl], bb.t[hs, :], -1.0, e1.t[hs, :], ALU.mult, ALU.mult, [bb.b, e1.b], [BTz.b])
                    TT("dve", c3(e2.t[:, :]), bcast(cend, 2, 128), c3(cum.t[:, :]), ALU.subtract, [cum.b], [e2.b])
                    ACT(e2.t[:, :], e2.t[:, :], AF.Exp, [e2.b], [e2.b])
                    TT("pool", kf.t[:, :], kf.t[:, :], e2.t[:, :], ALU.mult, [kf.b, e2.b], [kf.b])
                    STT("dve", bb.t[:, :], bb.t[:, :], -1.0, e2.t[:, :], ALU.mult, ALU.mult, [bb.b, e2.b], [bb.b])
                    pt, ptb = bank()
                    for c_ in range(4):
                        TR(pt[:, c_ * 128:(c_ + 1) * 128], kf.t[:, c_ * 128:(c_ + 1) * 128], ident, [kf.b, cst.b], [ptb])
                    CP("act", KH.t[:, c4:c4 + 4, :], c3(pt[:, 0:512]), [ptb], [KH.b])
                    pt, ptb = bank()
                    for c_ in range(4):
                        TR(pt[:, c_ * 128:(c_ + 1) * 128], bb.t[:, c_ * 128:(c_ + 1) * 128], ident, [bb.b, cst.b], [ptb])
                    CP("dve", BH.t[:, c4:c4 + 4, :], c3(pt[:, 0:512]), [ptb], [BH.b])
                for si, (s0, slen, _) in enumerate(SEQS):
                    nch = slen // 128
                    cbase = s0 // 128
                    if si == 0:
                        for h in range(2):
                            hs = slice(h * 64, (h + 1) * 64)
                            P.dma("sp", BD.t[hs, hs], st_rwkv[l, d, 2 * hp + h], writes=[BD.b])
                        pt, ptb = bank()
                        TR(pt[:, 0:128], BD.t[:, :], ident, [BD.b, cst.b], [ptb])
                        for h in range(2):
                            hs = slice(h * 64, (h + 1) * 64)
                            CP("dve", S32.t[hs, :], pt[hs, hs], [ptb], [S32.b])
                    else:
                        MSET("pool", S32.t[:, :], 0.0, [S32.b])
                    CP("act", SBz.t[0:64, 0, :], S32.t[0:64, :], [S32.b], [SBz.b])
                    CP("act", SBz.t[64:128, 1, :], S32.t[64:128, :], [S32.b], [SBz.b])
                    order = range(nch) if d == 0 else range(nch - 1, -1, -1)
                    for ci in order:
                        chunk_step(cbase + ci, d)
                    if si > 0:
                        for h in range(2):
                            hs = slice(h * 64, (h + 1) * 64)
                            CP("dve", BD.t[hs, hs], S32.t[hs, :], [S32.b], [BD.b])
                        pt, ptb = bank()
                        TR(pt[:, 0:128], BD.t[:, :], ident, [BD.b, cst.b], [ptb])
                        for h in range(2):
                            hs = slice(h * 64, (h + 1) * 64)
                            CP("dve", OUTS.t[hs, :], pt[hs, hs], [ptb], [OUTS.b])
                        ob = Buf("nsr")
                        out_bufs.append(ob)
                        P.dma("sp", ns_rwkv[si - 1, l, d, 2 * hp:2 * hp + 2].rearrange("h i j -> (h i) j"), OUTS.t[:, :],
                              reads=[OUTS.b], writes=[ob])
            yc, ysq, yn, t1 = tm["xr"], tm["xk"], tm["xv"], tm["lw"]
            for (t0, n) in BLK5:
                c4 = t0 // 128
                bl = slice(t0, t0 + n)
                yv = YACC.t[:, c4:c4 + 4, :].rearrange("p c (h i) -> p (c h) i", i=64)
                v8 = lambda ap: ap.rearrange("p (g i) -> p g i", i=64)
                RSUM(st1.t[:, :], yv, [YACC.b], [st1.b])
                TS("dve", st1.t[:, :], st1.t[:, :], 1.0 / 64, None, ALU.mult, None, [st1.b], [st1.b])
                TT("dve", v8(yc.t[:, :]), yv, bcast(st1.t[:, :], 2, 64), ALU.subtract, [YACC.b, st1.b], [yc.b])
                TT("pool", ysq.t[:, :], yc.t[:, :], yc.t[:, :], ALU.mult, [yc.b], [ysq.b])
                RSUM(st2.t[:, :], v8(ysq.t[:, :]), [ysq.b], [st2.b])
                TS("dve", st2.t[:, :], st2.t[:, :], 1.0 / 64, 64e-5, ALU.mult, ALU.add, [st2.b], [st2.b])
                ACT(st2.t[:, :], st2.t[:, :], AF.Ln, [st2.b], [st2.b])
                ACT(st2.t[:, :], st2.t[:, :], AF.Exp, [st2.b], [st2.b], scale=-0.5)
                TT("dve", v8(yn.t[:, :]), v8(yc.t[:, :]), bcast(st2.t[:, :], 2, 64), ALU.mult, [yc.b, st2.b], [yn.b])
                pt, ptb = bank()
                for c_ in range(4):
                    TR(pt[:, c_ * 128:(c_ + 1) * 128], yn.t[:, c_ * 128:(c_ + 1) * 128], ident, [yn.b, cst.b], [ptb])
                pg, pgb = bank()
                MM(pg[:, 0:n], GUP.t[:, hcols], SG.t[:, bl], True, True, [GUP.b, SG.b], [pgb])
                ACT(t1.t[:, :], pt[:, 0:n], AF.Identity, [ptb, lnw.b, lnb.b], [t1.b],
                    scale=lnw.t[:, l, hp:hp + 1], bias=lnb.t[:, l, hp:hp + 1])
                TT("pool", t1.t[:, :], t1.t[:, :], BON.t[:, bl], ALU.add, [t1.b, BON.b], [t1.b])
                TT("dve", YAst.t[:, bl], t1.t[:, :], pg[:, 0:n], ALU.mult, [t1.b, pgb], [YAst.b])
            P.dma("sp", ya_s[:, hp, :], YAst.t[:, :], reads=[YAst.b], writes=[ya_sb])

    ya_sb = Buf("ya_s")

    def lru_phase(l, H, Hb, YB, YBb):
        wch = [Tl(f"lwch{i}", [128, 8, 128], BF16) for i in range(4)]
        wrr = [0]

        def next_w(c0):
            tl = wch[wrr[0] % 4]
            wrr[0] += 1
            P.dma("pool", tl.t[:, :, :], win_cols(l, c0), writes=[tl.b])
            return tl
        wrg = [Tl(f"wrg{i}", [128, 2, 2, 128], BF16) for i in range(2)]
        GT = 2048
        Aa = [Tl(f"lA{d}", [128, GT], F32) for d in range(2)]
        Uu = [Tl(f"lU{d}", [128, GT], F32) for d in range(2)]
        HF = Tl("lHF", [128, GT], F32)
        GL = Tl("lGL", [128, GT], F32)
        xc = Tl("lxc", [128, 512], F32)
        xcb = Tl("lxcb", [128, 512], BF16)
        g1 = Tl("lg1", [128, 512], F32)
        rg = Tl("lrg", [128, 512], F32)
        ig = Tl("lig", [128, 512], F32)
        a2 = Tl("la2", [128, 512], F32)
        groups = [(BLK5[0:4], [(0, SEQS[0])], 0, 2048), (BLK5[4:5], [(1, SEQS[1]), (2, SEQS[2])], 2048, 512)]
        for j in range(8):
            jc = slice(j, j + 1)
            wx = next_w(3328 + j * 128)
            wg = next_w(4352 + j * 128)
            wr_ = wrg[j % 2]
            for d in range(2):
                P.dma("pool", wr_.t[:, d, 0, :], Wd["lru_w_rg"][l, d, j], writes=[wr_.b])
                P.dma("pool", wr_.t[:, d, 1, :], Wd["lru_w_ig"][l, d, j], writes=[wr_.b])
            for (gblocks, gseqs, g0, gn) in groups:
                for (t0, n) in gblocks:
                    o = t0 - g0
                    ol = slice(o, o + n)
                    px, pxb = proj(wx.t, wx.b, H, Hb, t0, n)
                    pg_, pgb = proj(wg.t, wg.b, H, Hb, t0, n)
                    ACT(xc.t[:, :], px[:, 0:n], AF.Identity, [pxb, cw.b, cb.b], [xc.b],
                        scale=cw.t[:, 4 * l + 2, jc], bias=cb.t[:, l, jc])
                    xv3 = rowview(xc.t[:, 0:n], t0)
                    pv3 = rowview(px[:, 0:n], t0)
                    STT("dve", xv3[:, :, 2:], pv3[:, :, :-2], cw.t[:, 4 * l + 0, jc], xv3[:, :, 2:], ALU.mult, ALU.add,
                        [pxb, xc.b, cw.b], [xc.b])
                    STT("dve", xv3[:, :, 1:], pv3[:, :, :-1], cw.t[:, 4 * l + 1, jc], xv3[:, :, 1:], ALU.mult, ALU.add,
                        [pxb, xc.b, cw.b], [xc.b])
                    STT("dve", xv3[:, :, :-1], pv3[:, :, 1:], cw.t[:, 4 * l + 3, jc], xv3[:, :, :-1], ALU.mult, ALU.add,
                        [pxb, xc.b, cw.b], [xc.b])
                    CP("pool", xcb.t[:, :], xc.t[:, :], [xc.b], [xcb.b])
                    ACT(g1.t[:, :], pg_[:, 0:n], AF.Square, [pgb], [g1.b])
                    TS("pool", g1.t[:, :], g1.t[:, :], 0.044715, 1.0, ALU.mult, ALU.add, [g1.b], [g1.b])
                    TT("dve", g1.t[:, :], g1.t[:, :], pg_[:, 0:n], ALU.mult, [g1.b, pgb], [g1.b])
                    ACT(g1.t[:, :], g1.t[:, :], AF.Sigmoid, [g1.b], [g1.b], scale=1.5957691216057308)
                    TT("dve", GL.t[:, ol], g1.t[:, :], pg_[:, 0:n], ALU.mult, [g1.b, pgb], [GL.b])
                    for d in range(2):
                        mi = 2 * l + d
                        pr_, prb = bank()
                        MM(pr_[:, 0:n], wr_.t[:, d, 0, :], xcb.t[:, :], True, True, [wr_.b, xcb.b], [prb])
                        pi_, pib = bank()
                        MM(pi_[:, 0:n], wr_.t[:, d, 1, :], xcb.t[:, :], True, True, [wr_.b, xcb.b], [pib])
                        ACT(rg.t[:, :], pr_[:, 0:n], AF.Sigmoid, [prb, brg.b], [rg.b], bias=brg.t[:, mi, jc])
                        ACT(ig.t[:, :], pi_[:, 0:n], AF.Sigmoid, [pib, big.b], [ig.b], bias=big.t[:, mi, jc])
                        ACT(Aa[d].t[:, ol], rg.t[:, :], AF.Exp, [rg.b, c8.b], [Aa[d].b], scale=c8.t[:, mi, jc])
                        TT("pool", a2.t[:, :], Aa[d].t[:, ol], Aa[d].t[:, ol], ALU.mult, [Aa[d].b], [a2.b])
                        ACT(a2.t[:, :], a2.t[:, :], AF.Sqrt, [a2.b], [a2.b], scale=-1.0, bias=1.0)
                        TT("pool", a2.t[:, :], a2.t[:, :], ig.t[:, :], ALU.mult, [a2.b, ig.b], [a2.b])
                        TT("dve", Uu[d].t[:, ol], a2.t[:, :], xc.t[:, :], ALU.mult, [a2.b, xc.b], [Uu[d].b])
                for (si, (s0, slen, _)) in gseqs:
                    o = s0 - g0
                    sl = slice(o, o + slen)
                    i0 = h0l.t[:, 2 * l + 0, jc] if si == 0 else 0.0
                    i1 = h0l.t[:, 2 * l + 1, jc] if si == 0 else 0.0
                    SCAN(HF.t[:, sl], Aa[0].t[:, sl], Uu[0].t[:, sl], i0, [Aa[0].b, Uu[0].b, h0l.b], [HF.b])
                    SCAN(rev_last(Aa[0].t[:, sl]), rev_last(Aa[1].t[:, sl]), rev_last(Uu[1].t[:, sl]), i1,
                         [Aa[1].b, Uu[1].b, h0l.b, HF.b], [Aa[0].b])
                    if si > 0:
                        CP("act", lruo.t[:, si - 1, l, 0, jc], HF.t[:, o + slen - 1:o + slen], [HF.b], [lruo.b])
                        CP("act", lruo.t[:, si - 1, l, 1, jc], Aa[0].t[:, o:o + 1], [Aa[0].b], [lruo.b])
                TT("pool", HF.t[:, 0:gn], HF.t[:, 0:gn], Aa[0].t[:, 0:gn], ALU.add, [HF.b, Aa[0].b], [HF.b])
                TT("dve", YB.t[:, j, g0:g0 + gn], HF.t[:, 0:gn], GL.t[:, 0:gn], ALU.mult, [HF.b, GL.b],
                   [YBb[i] for i in range(g0 // 512, (g0 + gn) // 512)])
        for b in range(2):
            for d in range(2):
                ob = Buf("nsl")
                out_bufs.append(ob)
                P.dma("sp", ns_lru[b, l, d].rearrange("(k p) -> p k", p=128), lruo.t[:, b, l, d, :], reads=[lruo.b],
                      writes=[ob], allow_slow_non_contiguous=True)

    def merge_b(l, H, Hb, YB, YBb):
        WB = Tl("WB", [128, 8, D], BF16)
        WGB = Tl("WGB", [128, 8, D], BF16)
        mtmp = Tl("mtmp", [128, 8, 512], BF16)
        sg = Tl("msg", [128, 512], F32)
        for k in range(8):
            P.dma("pool", WB.t[:, k, :], Wd["w_proj_b"][l, k * 128:(k + 1) * 128, :], writes=[WB.b])
            P.dma("pool", WGB.t[:, k, :], Wd["w_in"][l, k * 128:(k + 1) * 128, 6400:7424], writes=[WGB.b])
        for (t0, n) in BLK5:
            ybb = YBb[t0 // 512]
            for j in range(8):
                js = slice(j * 128, (j + 1) * 128)
                pg_, pgb = proj(WGB.t[:, :, js], WGB.b, H, Hb, t0, n)
                pb_, pbb = proj(WB.t[:, :, js], WB.b, YB, ybb, t0, n)
                ACT(sg.t[:, :], pg_[:, 0:n], AF.Sigmoid, [pgb, bmer.b], [sg.b], bias=bmer.t[:, l, 8 + j:9 + j])
                TT("dve", mtmp.t[:, j, :], sg.t[:, :], pb_[:, 0:n], ALU.mult, [sg.b, pbb], [mtmp.b])
            CP("pool", YB.t[:, :, t0:t0 + n], mtmp.t[:, :, :], [mtmp.b], [ybb])

    def merge_a(l, H, Hb, YB, YBb):
        YA = Tl("YA", [128, 8, T], BF16)
        for k in range(8):
            P.dma("sp", YA.t[:, k, :], ya_s[:, k, :], reads=[ya_sb], writes=[YA.b])
        wch = [Tl(f"mwch{i}", [128, 8, 128], BF16) for i in range(4)]
        sg = Tl("msg2", [128, 512], F32)
        tt_ = Tl("mtt", [128, 512], F32)
        for j in range(8):
            wa = wch[(2 * j) % 4]
            wga = wch[(2 * j + 1) % 4]
            P.dma("pool", wa.t[:, :, :], Wd["w_proj_a"][l].rearrange("(k p) c -> p k c", p=128)[:, :, j * 128:(j + 1) * 128],
                  writes=[wa.b])
            P.dma("pool", wga.t[:, :, :], win_cols(l, 5376 + j * 128), writes=[wga.b])
            for (t0, n) in BLK5:
                ybb = YBb[t0 // 512]
                pg_, pgb = proj(wga.t, wga.b, H, Hb, t0, n)
                pa_, pab = proj(wa.t, wa.b, YA, YA.b, t0, n)
                ACT(sg.t[:, :], pg_[:, 0:n], AF.Sigmoid, [pgb, bmer.b], [sg.b], bias=bmer.t[:, l, j:j + 1])
                TT("dve", tt_.t[:, :], sg.t[:, :], pa_[:, 0:n], ALU.mult, [sg.b, pab], [tt_.b])
                TT("pool", YB.t[:, j, t0:t0 + n], tt_.t[:, :], YB.t[:, j, t0:t0 + n], ALU.add, [tt_.b, ybb], [ybb])

    def out_proj(l, YB, YBb):
        WO = Tl("WO", [128, 8, D], BF16)
        for k in range(8):
            P.dma("pool", WO.t[:, k, :], Wd["w_out"][l, k * 128:(k + 1) * 128, :], writes=[WO.b])
        O = Tl("O", [128, 8, 512], F32)
        rt = res_tmps(False)
        for (t0, n) in BLK5:
            for j in range(8):
                po, pob = proj(WO.t[:, :, j * 128:(j + 1) * 128], WO.b, YB, YBb[t0 // 512], t0, n)
                CP("act" if j % 2 else "dve", O.t[:, j, 0:n], po[:, 0:n], [pob], [O.b])
            residual_update(l, O.t[:, :, 0:n], O.b, G1, t0, n, False, rt)

    def ffn_phase(l):
        moe = (l % 2 == 1)
        final = (l == L - 1)
        if not moe:
            i = l // 2
            groups = [(Wd["ffn_w1"][i][:, g * 2048:(g + 1) * 2048], Wd["ffn_w3"][i][:, g * 2048:(g + 1) * 2048],
                       Wd["ffn_w2"][i][g * 2048:(g + 1) * 2048, :], 16, None) for g in range(2)]
        else:
            i = l // 2
            groups = [(Wd["moe_w1"][i, e], Wd["moe_w3"][i, e], Wd["moe_w2"][i, e], 28, e) for e in range(8)]
        halves = [BLK5]
        HT = T
        FG = 4
        for hf, blocks in enumerate(halves):
            hoff = hf * HT
            with P.scope():
                H2 = Tl("H2", [128, 8, HT], BF16)
                GTt = Tl("GTt", [32, HT], F32)
                MSET("pool", GTt.t[:, :], 0.0, [GTt.b])
                GB = Tl("GB", [128, HT], F32)
                with P.scope():
                    tmp = norm_tmps()
                    if moe:
                        hf32 = Tl("hf32", [128, 8, 512], F32)
                        RT = Tl("RT", [128, 8, 8], F32)
                        P.dma("sp", RT.t[:, :, :], Wd["moe_router"][i].rearrange("(k p) e -> p k e", p=128), writes=[RT.b])
                        lg = Tl("lg", [128, 8], F32)
                        lg2 = Tl("lg2", [128, 8], F32)
                        m1 = Tl("m1", [128, 1], F32)
                        m2 = Tl("m2", [128, 1], F32)

                        def gates(t0, n):
                            for tt in range(n // 128):
                                ts_ = slice(tt * 128, (tt + 1) * 128)
                                pl, plb = bank()
                                for k in range(8):
                                    MM(pl[:, 0:8], hf32.t[:, k, ts_], RT.t[:, k, :], k == 0, k == 7, [hf32.b, RT.b], [plb])
                                CP("act", lg.t[:, :], pl[:, 0:8], [plb], [lg.b])
                                RMAX(m1.t[:, :], lg.t[:, :], [lg.b], [m1.b])
                                TS("dve", lg2.t[:, :], lg.t[:, :], m1.t[:, 0:1], None, ALU.is_equal, None, [lg.b, m1.b], [lg2.b])
                                STT("dve", lg2.t[:, :], lg2.t[:, :], -1e30, lg.t[:, :], ALU.mult, ALU.add, [lg2.b, lg.b], [lg2.b])
                                RMAX(m2.t[:, :], lg2.t[:, :], [lg2.b], [m2.b])
                                TS("dve", lg2.t[:, :], lg.t[:, :], m2.t[:, 0:1], None, ALU.is_ge, None, [lg.b, m2.b], [lg2.b])
                                TS("dve", m1.t[:, :], m1.t[:, :], -1.0, None, ALU.mult, None, [m1.b], [m1.b])
                                ACT(lg.t[:, :], lg.t[:, :], AF.Exp, [lg.b, m1.b], [lg.b], bias=m1.t[:, 0:1])
                                TT("dve", lg.t[:, :], lg.t[:, :], lg2.t[:, :], ALU.mult, [lg.b, lg2.b], [lg.b])
                                RSUM(m2.t[:, :], lg.t[:, :], [lg.b], [m2.b])
                                P.op("dve", lambda e: e.reciprocal(m2.t[:, :], m2.t[:, :]), [m2.b], [m2.b])
                                TS("dve", lg.t[:, :], lg.t[:, :], m2.t[:, 0:1], None, ALU.mult, None, [lg.b, m2.b], [lg.b])
                                pt, ptb = bank()
                                TR(pt[0:8, 0:128], lg.t[:, :], ident, [lg.b, cst.b], [ptb])
                                o = t0 - hoff + tt * 128
                                CP("act", GTt.t[0:8, o:o + 128], pt[0:8, 0:128], [ptb], [GTt.b])
                        norm_mod(l, A2, B2, H2, H2.b, blocks, hoff, tmp, hf32=hf32, cbk=gates)
                    else:
                        norm_mod(l, A2, B2, H2, H2.b, blocks, hoff, tmp)
                OACC = Tl("OACC", [128, 8, HT], F32)
                with P.scope():
                    HID = Tl("HID", [128, FG, HT], BF16)
                    w13 = [Tl(f"w13_{i_}", [128, 8, 128], BF16) for i_ in range(4)]
                    w2g = [Tl(f"w2g{i_}", [128, FG, D], BF16) for i_ in range(2)]
                    sil = [Tl(f"sil{i_}", [128, 512], F32) for i_ in range(2)]
                    wc = [0]
                    ng = [0]
                    for gi, (w1, w3, w2, nf, ex) in enumerate(groups):
                        if ex is not None:
                            for (t0, n) in blocks:
                                o = t0 - hoff
                                pg, pgb = bank()
                                MM(pg[:, 0:n], cst.t[0:8, C_ES + ex * 128:C_ES + (ex + 1) * 128], GTt.t[0:8, o:o + n], True, True,
                                   [cst.b, GTt.b], [pgb])
                                CP("act", GB.t[:, o:o + n], pg[:, 0:n], [pgb], [GB.b])
                        w1v = w1.rearrange("(k p) c -> p k c", p=128)
                        w3v = w3.rearrange("(k p) c -> p k c", p=128)
                        w2v = w2.rearrange("(f p) c -> p f c", p=128)
                        for fg in range(nf // FG):
                            wt2 = w2g[ng[0] % 2]
                            P.dma("pool", wt2.t[:, :, :], w2v[:, fg * FG:(fg + 1) * FG, :], writes=[wt2.b])
                            for fl in range(FG):
                                f = fg * FG + fl
                                fs = slice(f * 128, (f + 1) * 128)
                                w1t = w13[wc[0] % 4]
                                w3t = w13[(wc[0] + 1) % 4]
                                wc[0] += 2
                                P.dma("pool", w1t.t[:, :, :], w1v[:, :, fs], writes=[w1t.b])
                                P.dma("pool", w3t.t[:, :, :], w3v[:, :, fs], writes=[w3t.b])
                                for bi, (t0, n) in enumerate(blocks):
                                    o = t0 - hoff
                                    s_ = sil[(f + bi) % 2]
                                    p1, p1b = proj(w1t.t, w1t.b, H2, H2.b, t0, n, hoff)
                                    p3, p3b = proj(w3t.t, w3t.b, H2, H2.b, t0, n, hoff)
                                    ACT(s_.t[:, 0:n], p1[:, 0:n], AF.Silu, [p1b], [s_.b])
                                    if ex is None:
                                        TT("dve", HID.t[:, fl, o:o + n], s_.t[:, 0:n], p3[:, 0:n], ALU.mult, [s_.b, p3b], [HID.b])
                                    else:
                                        TT("dve", s_.t[:, 0:n], s_.t[:, 0:n], p3[:, 0:n], ALU.mult, [s_.b, p3b], [s_.b])
                                        TT("pool", HID.t[:, fl, o:o + n], s_.t[:, 0:n], GB.t[:, o:o + n], ALU.mult, [s_.b, GB.b], [HID.b])
                            for j in range(8):
                                for (t0, n) in blocks:
                                    o = t0 - hoff
                                    po, pob = bank()
                                    for fl in range(FG):
                                        MM(po[:, 0:n], wt2.t[:, fl, j * 128:(j + 1) * 128], HID.t[:, fl, o:o + n], fl == 0, fl == FG - 1,
                                           [wt2.b, HID.b], [pob])
                                    if ng[0] == 0:
                                        CP("act", OACC.t[:, j, o:o + n], po[:, 0:n], [pob], [OACC.b])
                                    else:
                                        TT("dve", OACC.t[:, j, o:o + n], po[:, 0:n], OACC.t[:, j, o:o + n], ALU.add, [pob, OACC.b], [OACC.b])
                            ng[0] += 1
                with P.scope():
                    rt = res_tmps(final)
                    for (t0, n) in blocks:
                        o = t0 - hoff
                        residual_update(l, OACC.t[:, :, o:o + n], OACC.b, G2, t0, n, final, rt)

    for l in range(L):
        with P.scope():
            YB = Tl("YB", [128, 8, T], BF16)
            YBb = [Buf(f"YB{i}") for i in range(NBLK)]
            with P.scope():
                H = Tl("H", [128, 8, T], BF16)
                Hb = [Buf(f"H{i}") for i in range(NBLK)]
                P.mark(f"L{l} start")
                with P.scope():
                    norm_mod(l, A1, B1, H, Hb, BLK5, 0, norm_tmps())
                P.mark(f"L{l} norm1 done")
                with P.scope():
                    rwkv_phase(l, H, Hb, YB)
                P.mark(f"L{l} rwkv done")
                with P.scope():
                    lru_phase(l, H, Hb, YB, YBb)
                P.mark(f"L{l} lru done")
                with P.scope():
                    merge_b(l, H, Hb, YB, YBb)
                P.mark(f"L{l} merge_b done")
                with P.scope():
                    merge_a(l, H, Hb, YB, YBb)
                P.mark(f"L{l} merge_a done")
            with P.scope():
                out_proj(l, YB, YBb)
            P.mark(f"L{l} out_proj done")
        with P.scope():
            ffn_phase(l)
        P.mark(f"L{l} ffn done")

    P.barrier()
    P.emit()
    P.close()
    nc._marks = getattr(P, "marks", [])
    return nc


_CACHE = {}


def kernel(**inputs):
    inp = {k: np.ascontiguousarray(np.asarray(v)) for k, v in inputs.items()}
    if "nc" not in _CACHE:
        _CACHE["nc"] = build_program()
    nc = _CACHE["nc"]
    cst = make_consts()
    in_maps = []
    for c in range(NCORES):
        m = {}
        m["xin"] = np.ascontiguousarray(np.concatenate(
            [inp["x_sample"][c], inp["x_prompt"][2 * c], inp["x_prompt"][2 * c + 1]], axis=0))
        m["cond"] = np.ascontiguousarray(np.stack([inp["c"][c], inp["c_ctx"]], axis=0))
        m["st_rwkv"] = np.ascontiguousarray(inp["state_rwkv"][c])
        m["st_lru"] = np.ascontiguousarray(inp["state_lru"][c])
        for n_, _s in WEIGHT_SHAPES:
            m[n_] = inp[n_]
        m["cst"] = cst
        in_maps.append(m)
    res = run_bass_kernel_spmd(nc, in_maps, core_ids=list(range(NCORES)))
    R = res.results
    y_sample = np.stack([R[c]["y"][0:2048] for c in range(NCORES)], axis=0)
    y_prompt = np.concatenate([R[c]["y"][2048:2560].reshape(2, 256, D) for c in range(NCORES)], axis=0)
    ns_rwkv = np.concatenate([R[c]["ns_rwkv"] for c in range(NCORES)], axis=0)
    ns_lru = np.concatenate([R[c]["ns_lru"] for c in range(NCORES)], axis=0)
    return (y_prompt.astype(np.float32), y_sample.astype(np.float32), ns_rwkv.astype(np.float32), ns_lru.astype(np.float32))
```

```python
import contextlib
import numpy as np
import concourse.bass as bass
import concourse.mybir as mybir
from concourse.ap import AP
from concourse.bass_utils import run_bass_kernel_spmd

F32 = mybir.dt.float32
BF16 = mybir.dt.bfloat16
ALU = mybir.AluOpType
AF = mybir.ActivationFunctionType
AX = mybir.AxisListType

ENGS = ("pe", "act", "dve", "pool", "sp")
NCORES = 8
T = 2560
NBLK = 5
SEQS = [(0, 2048, 64), (2048, 256, 256), (2304, 256, 256)]
D = 1024
INC = 7424
L = 2


class Buf:
    __slots__ = ("name", "last_write", "reads")

    def __init__(self, name=""):
        self.name = name
        self.last_write = None
        self.reads = []


class Prog:
    def __init__(self, nc, n_dma_sems=(24, 8, 24)):
        self.nc = nc
        self.ops = {e: [] for e in ENGS}
        self.count = {e: 0 for e in ENGS}
        self.waited = {e: {} for e in ENGS}
        self.sems = {}
        self._ctx = []
        for e in ENGS:
            self.sems["E_" + e] = self._sem("E_" + e)
        self.dma_pool = {}
        for q, n in zip(("sp", "act", "pool"), n_dma_sems):
            self.dma_pool[q] = []
            for i in range(n):
                self.sems[f"D_{q}{i}"] = self._sem(f"D_{q}{i}")
                self.dma_pool[q].append([f"D_{q}{i}", 0])
        self.dma_rr = {q: 0 for q in ("sp", "act", "pool")}
        self.n_ops = 0

    def _sem(self, name):
        cm = self.nc.semaphore(name)
        s = cm.__enter__()
        self._ctx.append(cm)
        return s

    def sbuf(self, name, shape, dtype):
        self._uid = getattr(self, "_uid", 0) + 1
        name = f"s{self._uid}_{name}"
        cm = self.nc.sbuf_tensor(name, list(shape), dtype)
        t = cm.__enter__()
        self._ctx.append(cm)
        return t

    def psum(self, name, shape, dtype):
        cm = self.nc.psum_tensor(name, list(shape), dtype)
        t = cm.__enter__()
        self._ctx.append(cm)
        return t

    def _deps(self, eng, reads, writes):
        deps = {}
        own = "E_" + eng

        def add(tok, same_ok):
            if tok is None:
                return
            s, v = tok
            if s == own and not same_ok:
                return
            if deps.get(s, 0) < v:
                deps[s] = v

        for b in reads:
            add(b.last_write, eng != "pe")
        for b in writes:
            add(b.last_write, False)
            for t in b.reads:
                add(t, False)
        waits = []
        w = self.waited[eng]
        for s, v in deps.items():
            if w.get(s, 0) >= v:
                continue
            w[s] = v
            waits.append((s, v))
        return waits

    def _commit(self, tok, reads, writes):
        for b in reads:
            b.reads.append(tok)
            if len(b.reads) > 48:
                latest = {}
                for s, v in b.reads:
                    if latest.get(s, 0) < v:
                        latest[s] = v
                b.reads = list(latest.items())
        for b in writes:
            b.last_write = tok
            b.reads = []

    def op(self, eng, fn, reads=(), writes=()):
        waits = self._deps(eng, reads, writes)
        self.count[eng] += 1
        tok = ("E_" + eng, self.count[eng])
        self.ops[eng].append((waits, fn, tok))
        self._commit(tok, reads, writes)
        self.n_ops += 1
        return tok

    def dma(self, q, out, in_, reads=(), writes=(), **kw):
        pool = self.dma_pool[q]
        i = self.dma_rr[q]
        self.dma_rr[q] = (i + 1) % len(pool)
        ent = pool[i]
        semname = ent[0]
        waits = self._deps(q, reads, writes)
        if ent[1] > 0 and self.waited[q].get(semname, 0) < ent[1]:
            self.waited[q][semname] = ent[1]
            waits.append((semname, ent[1]))
        ent[1] += 16
        tok = (semname, ent[1])

        def fn(e, out=out, in_=in_, kw=kw):
            return e.dma_start(out=out, in_=in_, **kw)
        self.ops[q].append((waits, fn, tok))
        self._commit(tok, reads, writes)
        self.n_ops += 1
        return tok

    def mark(self, name):
        if not hasattr(self, "marks"):
            self.marks = []
        self.marks.append((name, dict(self.count)))

    def barrier(self):
        targets = []
        for e in ENGS:
            if self.count[e] > 0:
                targets.append(("E_" + e, self.count[e]))
        for q in self.dma_pool:
            for name, v in self.dma_pool[q]:
                if v > 0:
                    targets.append((name, v))
        for e in ENGS:
            waits = []
            for s, v in targets:
                if s == "E_" + e:
                    continue
                if self.waited[e].get(s, 0) >= v:
                    continue
                self.waited[e][s] = v
                waits.append((s, v))
            if waits:
                self.ops[e].append((waits, None, None))

    @contextlib.contextmanager
    def scope(self):
        n0 = len(self._ctx)
        yield
        self.barrier()
        while len(self._ctx) > n0:
            cm = self._ctx.pop()
            cm.__exit__(None, None, None)

    def emit(self):
        nc = self.nc
        engmap = {"pe": "tensor", "act": "scalar", "dve": "vector", "pool": "gpsimd", "sp": "sync"}
        with nc.Block() as block:
            for e in ENGS:
                lst = self.ops[e]
                if not lst:
                    continue

                def body(eng, lst=lst):
                    for waits, fn, tok in lst:
                        if fn is None:
                            for s, v in waits:
                                eng.wait_ge(self.sems[s], v)
                            continue
                        for s, v in waits[1:]:
                            eng.wait_ge(self.sems[s], v)
                        ins = fn(eng)
                        if waits:
                            ins._wait_ge(self.sems[waits[0][0]], waits[0][1])
                        ins.then_inc(self.sems[tok[0]], 16 if tok[0].startswith("D_") else 1)
                getattr(block, engmap[e])(body)

    def close(self):
        while self._ctx:
            cm = self._ctx.pop()
            cm.__exit__(None, None, None)


def rev_last(ap):
    dims = [list(d) for d in ap.ap]
    step, cnt = dims[-1]
    return AP(ap.tensor, ap.offset + step * (cnt - 1), dims[:-1] + [[-step, cnt]])


def bcast(ap, axis, n):
    dims = [list(d) for d in ap.ap]
    dims.insert(axis, [0, n])
    return AP(ap.tensor, ap.offset, dims)


WEIGHT_SHAPES = [
    ("mod_w", (L, D, 6 * D)), ("mod_b", (L, 6 * D)),
    ("norm_pre_mix", (L, D)), ("norm_post_mix", (L, D)), ("norm_pre_ffn", (L, D)), ("norm_post_ffn", (L, D)),
    ("w_in", (L, D, INC)), ("b_merge", (L, 2 * D)),
    ("rwkv_mu", (L, 2, 3200)), ("rwkv_w0", (L, 2, D)), ("rwkv_w_up", (L, 2, 64, D)),
    ("rwkv_a0", (L, 2, D)), ("rwkv_a_up", (L, 2, 64, D)), ("rwkv_k_k", (L, D)), ("rwkv_k_a", (L, D)),
    ("rwkv_r_k", (L, 16, 64)), ("rwkv_g_up", (L, 128, D)), ("rwkv_lnx_w", (L, D)), ("rwkv_lnx_b", (L, D)),
    ("lru_conv_w", (L, 4, D)), ("lru_conv_b", (L, D)), ("lru_w_rg", (L, 2, 8, 128, 128)), ("lru_b_rg", (L, 2, D)),
    ("lru_w_ig", (L, 2, 8, 128, 128)), ("lru_b_ig", (L, 2, D)), ("lru_lam", (L, 2, D)),
    ("w_proj_a", (L, D, D)), ("w_proj_b", (L, D, D)), ("w_out", (L, D, D)),
    ("ffn_w1", (1, D, 4096)), ("ffn_w3", (1, D, 4096)), ("ffn_w2", (1, 4096, D)),
    ("moe_router", (1, D, 8)), ("moe_w1", (1, 8, D, 3584)), ("moe_w3", (1, 8, D, 3584)), ("moe_w2", (1, 8, 3584, D)),
]

C_ID = 0
C_US = 128
C_UI = 256
C_LS = 384
C_LI = 512
C_MF = 640
C_MB = 1152
C_ES = 1664
C_LM = C_ES + 8 * 128
C_LMT = C_LM + 7 * 128
NCST32 = C_LM
NCST = C_LMT + 7 * 128


def make_consts():
    c = np.zeros((128, NCST), np.float32)
    p = np.arange(128)[:, None]
    q = np.arange(128)[None, :]
    c[:, C_ID:C_ID + 128] = (p == q)
    c[:, C_US:C_US + 128] = (p < q)
    c[:, C_UI:C_UI + 128] = (p <= q)
    c[:, C_LS:C_LS + 128] = (p > q)
    c[:, C_LI:C_LI + 128] = (p >= q)
    t = np.arange(512)
    c[:, C_MF:C_MF + 512] = (t % 128 != 0)[None, :]
    c[:, C_MB:C_MB + 512] = (t % 128 != 127)[None, :]
    for e in range(8):
        c[e, C_ES + e * 128:C_ES + (e + 1) * 128] = 1.0
    for i in range(7):
        b = 1 << i
        m = ((p // (2 * b)) == (q // (2 * b))) & ((p % (2 * b)) < b) & ((q % (2 * b)) >= b)
        c[:, C_LM + i * 128:C_LM + (i + 1) * 128] = m
        c[:, C_LMT + i * 128:C_LMT + (i + 1) * 128] = m.T
    return c


def build_program(debug=False):
    nc = bass.Bass("TRN2", target_bir_lowering=False)

    def din(name, shape):
        return nc.dram_tensor(name, list(shape), F32, kind="ExternalInput").ap()

    def dout(name, shape):
        return nc.dram_tensor(name, list(shape), F32, kind="ExternalOutput").ap()

    xin = din("xin", [T, D])
    cond = din("cond", [2, D])
    st_rwkv = din("st_rwkv", [L, 2, 16, 64, 64])
    st_lru = din("st_lru", [L, 2, D])
    Wd = {n: din(n, s) for n, s in WEIGHT_SHAPES}
    cstd = din("cst", [128, NCST])
    y_out = dout("y", [T, D])
    ns_rwkv = dout("ns_rwkv", [2, L, 2, 16, 64, 64])
    ns_lru = dout("ns_lru", [2, L, 2, D])
    xs = nc.dram_tensor("xs_scr", [128, 8, T], F32).ap()
    ya_s = nc.dram_tensor("ya_scr", [128, 8, T], BF16).ap()

    P = Prog(nc)
    out_bufs = []

    def ACT(out, in_, func, R, Wr, scale=None, bias=None):
        kw = {}
        if scale is not None:
            kw["scale"] = scale
        if bias is not None:
            kw["bias"] = bias
        P.op("act", lambda e: e.activation(out=out, in_=in_, func=func, **kw), R, Wr)

    def TT(eng, out, in0, in1, op, R, Wr):
        P.op(eng, lambda e: e.tensor_tensor(out, in0, in1, op), R, Wr)

    def TS(eng, out, in0, s1, s2, op0, op1, R, Wr):
        if s2 is None:
            P.op(eng, lambda e: e.tensor_scalar(out, in0, s1, None, op0), R, Wr)
        else:
            P.op(eng, lambda e: e.tensor_scalar(out, in0, s1, s2, op0, op1), R, Wr)

    def STT(eng, out, in0, sc, in1, op0, op1, R, Wr):
        P.op(eng, lambda e: e.scalar_tensor_tensor(out, in0, sc, in1, op0, op1), R, Wr)

    def CP(eng, out, in_, R, Wr):
        if eng == "act":
            P.op("act", lambda e: e.activation(out=out, in_=in_, func=AF.Copy), R, Wr)
        else:
            P.op(eng, lambda e: e.tensor_copy(out, in_), R, Wr)

    def MM(out, lhsT, rhs, start, stop, R, Wr):
        P.op("pe", lambda e: e.matmul(out, lhsT, rhs, start=start, stop=stop), R, Wr)

    def TR(out, in_, ident, R, Wr):
        P.op("pe", lambda e: e.transpose(out, in_, ident), R, Wr)

    def MSET(eng, ap, val, Wr):
        P.op(eng, lambda e: e.memset(ap, val), (), Wr)

    class Tl:
        def __init__(self, name, shape, dtype):
            self.t = P.sbuf(name, shape, dtype)
            self.b = Buf(name)

    banks = [P.psum(f"bank{i}", [128, 512], F32) for i in range(8)]
    bbufs = [Buf(f"bank{i}") for i in range(8)]
    rr = [0]

    def bank():
        i = rr[0]
        rr[0] = (i + 1) % 8
        return banks[i], bbufs[i]

    NE = 1.0 / D

    cst = Tl("cst", [128, NCST32], F32)
    P.dma("sp", cst.t[:, :], cstd[:, 0:NCST32], writes=[cst.b])
    lmk = Tl("lmk", [128, 14 * 128], BF16)
    P.dma("pool", lmk.t[:, :], cstd[:, C_LM:NCST], writes=[lmk.b])
    ident = cst.t[:, C_ID:C_ID + 128]
    ones_bf = Tl("ones_bf", [128, 128], BF16)
    MSET("pool", ones_bf.t[:, :], 1.0, [ones_bf.b])
    bd_bf = Tl("bd_bf", [128, 128], BF16)
    MSET("pool", bd_bf.t[:, :], 0.0, [bd_bf.b])
    MSET("pool", bd_bf.t[0:64, 0:64], 1.0, [bd_bf.b])
    MSET("pool", bd_bf.t[64:128, 64:128], 1.0, [bd_bf.b])
    ident_bf = Tl("ident_bf", [128, 128], BF16)
    CP("dve", ident_bf.t[:, :], ident, [cst.b], [ident_bf.b])

    def load_fm(name, src2d, n):
        cols = src2d.shape[1] // 128
        tl = Tl(name, [128, n, cols], F32)
        for i in range(n):
            P.dma("sp", tl.t[:, i, :], src2d[i].rearrange("(k p) -> p k", p=128), writes=[tl.b],
                  allow_slow_non_contiguous=True)
        return tl

    npm = load_fm("npm", Wd["norm_pre_mix"], L)
    npo = load_fm("npo", Wd["norm_post_mix"], L)
    npf = load_fm("npf", Wd["norm_pre_ffn"], L)
    npg = load_fm("npg", Wd["norm_post_ffn"], L)
    modb = load_fm("modb", Wd["mod_b"], L)
    bmer = load_fm("bmer", Wd["b_merge"], L)
    mu = load_fm("mu", Wd["rwkv_mu"].rearrange("l d c -> (l d) c"), 2 * L)
    w0 = load_fm("w0", Wd["rwkv_w0"].rearrange("l d c -> (l d) c"), 2 * L)
    a0 = load_fm("a0", Wd["rwkv_a0"].rearrange("l d c -> (l d) c"), 2 * L)
    kk_ = load_fm("kk_", Wd["rwkv_k_k"], L)
    ka_ = load_fm("ka_", Wd["rwkv_k_a"], L)
    rk_ = load_fm("rk_", Wd["rwkv_r_k"].rearrange("l h c -> l (h c)"), L)
    lnw = load_fm("lnw", Wd["rwkv_lnx_w"], L)
    lnb = load_fm("lnb", Wd["rwkv_lnx_b"], L)
    cw = load_fm("cw", Wd["lru_conv_w"].rearrange("l j c -> (l j) c"), 4 * L)
    cb = load_fm("cb", Wd["lru_conv_b"], L)
    brg = load_fm("brg", Wd["lru_b_rg"].rearrange("l d c -> (l d) c"), 2 * L)
    big = load_fm("big", Wd["lru_b_ig"].rearrange("l d c -> (l d) c"), 2 * L)
    lam = load_fm("lam", Wd["lru_lam"].rearrange("l d c -> (l d) c"), 2 * L)
    h0l = load_fm("h0l", st_lru.rearrange("l d c -> (l d) c"), 2 * L)

    omu = Tl("omu", [128, 2 * L, 25], F32)
    TS("dve", omu.t[:, :, :], mu.t[:, :, :], -1.0, 1.0, ALU.mult, ALU.add, [mu.b], [omu.b])
    omka = Tl("omka", [128, L, 8], F32)
    TS("dve", omka.t[:, :, :], ka_.t[:, :, :], -1.0, 1.0, ALU.mult, ALU.add, [ka_.b], [omka.b])
    c8 = Tl("c8", [128, 2 * L, 8], F32)
    ACT(c8.t[:, :, :], lam.t[:, :, :], AF.Exp, [lam.b], [c8.b], scale=-1.0)
    ACT(c8.t[:, :, :], c8.t[:, :, :], AF.Ln, [c8.b], [c8.b], bias=1.0)
    TS("dve", c8.t[:, :, :], c8.t[:, :, :], -8.0, None, ALU.mult, None, [c8.b], [c8.b])

    A1 = Tl("A1", [128, L, 2, 8], F32)
    B1 = Tl("B1", [128, L, 2, 8], F32)
    G1 = Tl("G1", [128, L, 2, 8], F32)
    A2 = Tl("A2", [128, L, 2, 8], F32)
    B2 = Tl("B2", [128, L, 2, 8], F32)
    G2 = Tl("G2", [128, L, 2, 8], F32)
    with P.scope():
        condT = Tl("condT", [128, 8, 2], F32)
        for g in range(2):
            P.dma("sp", condT.t[:, :, g], cond[g].rearrange("(k p) -> p k", p=128), writes=[condT.b],
                  allow_slow_non_contiguous=True)
        scond = Tl("scond", [128, 8, 2], F32)
        ACT(scond.t[:, :, :], condT.t[:, :, :], AF.Silu, [condT.b], [scond.b])
        modv = Tl("modv", [128, L, 48, 2], F32)
        mwt = [Tl(f"mwt{i}", [128, 8, 512], F32) for i in range(2)]
        for l in range(L):
            pb, pbb = bank()
            for cbk in range(12):
                wt = mwt[cbk % 2]
                src = Wd["mod_w"][l].rearrange("(k p) c -> p k c", p=128)[:, :, cbk * 512:(cbk + 1) * 512]
                P.dma("sp", wt.t[:, :, :], src, writes=[wt.b])
                for jj in range(4):
                    col = (cbk * 4 + jj) * 2
                    for k in range(8):
                        MM(pb[:, col:col + 2], wt.t[:, k, jj * 128:(jj + 1) * 128], scond.t[:, k, :],
                           k == 0, k == 7, [wt.b, scond.b], [pbb])
            for g in range(2):
                src = pb[:, 0:96].rearrange("p (c g) -> p c g", g=2)[:, :, g]
                TT("dve", modv.t[:, l, :, g], src, modb.t[:, l, :], ALU.add, [pbb, modb.b], [modv.b])
            for g in range(2):
                mv = modv.t[:, l, :, g]
                STT("dve", A1.t[:, l, g, :], mv[:, 8:16], 1.0, npm.t[:, l, :], ALU.add, ALU.mult, [modv.b, npm.b], [A1.b])
                CP("dve", B1.t[:, l, g, :], mv[:, 0:8], [modv.b], [B1.b])
                TT("dve", G1.t[:, l, g, :], mv[:, 16:24], npo.t[:, l, :], ALU.mult, [modv.b, npo.b], [G1.b])
                STT("dve", A2.t[:, l, g, :], mv[:, 32:40], 1.0, npf.t[:, l, :], ALU.add, ALU.mult, [modv.b, npf.b], [A2.b])
                CP("dve", B2.t[:, l, g, :], mv[:, 24:32], [modv.b], [B2.b])
                TT("dve", G2.t[:, l, g, :], mv[:, 40:48], npg.t[:, l, :], ALU.mult, [modv.b, npg.b], [G2.b])

    xs_b = [Buf(f"xs{i}") for i in range(NBLK)]
    with P.scope():
        xrow = [Tl(f"xrow{i}", [128, D], F32) for i in range(2)]
        xfm = [Tl(f"xfm{i}", [128, 8, 512], F32) for i in range(2)]
        for tb in range(NBLK):
            xf = xfm[tb % 2]
            for tt in range(4):
                xr_ = xrow[(tb * 4 + tt) % 2]
                r0 = tb * 512 + tt * 128
                P.dma("sp", xr_.t[:, :], xin[r0:r0 + 128, :], writes=[xr_.b])
                for half in range(2):
                    pb, pbb = bank()
                    for kk2 in range(4):
                        k = half * 4 + kk2
                        TR(pb[:, kk2 * 128:(kk2 + 1) * 128], xr_.t[:, k * 128:(k + 1) * 128], ident, [xr_.b, cst.b], [pbb])
                    dst = xf.t[:, half * 4:(half + 1) * 4, tt * 128:(tt + 1) * 128]
                    src = pb[:, :].rearrange("p (k t) -> p k t", t=128)
                    if half == 0:
                        CP("act", dst, src, [pbb], [xf.b])
                    else:
                        CP("dve", dst, src, [pbb], [xf.b])
            P.dma("sp", xs[:, :, tb * 512:(tb + 1) * 512], xf.t[:, :, :], reads=[xf.b], writes=[xs_b[tb]])

    def blk_group(t0):
        return 0 if t0 < 2048 else 1

    def SCAN(out, d0, d1, init, R, Wr):
        P.op("dve", lambda e: e.tensor_tensor_scan(out, d0, d1, init, ALU.mult, ALU.add), R, Wr)

    def RSUM(out, in_, R, Wr):
        P.op("dve", lambda e: e.reduce_sum(out, in_, AX.X), R, Wr)

    def RMAX(out, in_, R, Wr):
        P.op("dve", lambda e: e.reduce_max(out, in_, AX.X), R, Wr)

    def hbuf(Hb, t0):
        return Hb[t0 // 512] if isinstance(Hb, list) else Hb

    def norm_mod(l, A, B, H, Hb, blocks, hoff, tmp, hf32=None, cbk=None):
        xbt, sq_t, rstd_t, xn_t = tmp
        for bi, (t0, n) in enumerate(blocks):
            g = blk_group(t0)
            xb = xbt[bi % 2]
            P.dma("sp", xb.t[:, :, 0:n], xs[:, :, t0:t0 + n], reads=[xs_b[t0 // 512]], writes=[xb.b])
            ACT(sq_t.t[:, :, 0:n], xb.t[:, :, 0:n], AF.Square, [xb.b], [sq_t.b])
            pb, pbb = bank()
            for k in range(8):
                MM(pb[:, 0:n], ones_bf.t[:, :], sq_t.t[:, k, 0:n], k == 0, k == 7, [ones_bf.b, sq_t.b], [pbb])
            TS("dve", rstd_t.t[:, 0:n], pb[:, 0:n], NE, 1e-6, ALU.mult, ALU.add, [pbb], [rstd_t.b])
            ACT(rstd_t.t[:, 0:n], rstd_t.t[:, 0:n], AF.Ln, [rstd_t.b], [rstd_t.b])
            ACT(rstd_t.t[:, 0:n], rstd_t.t[:, 0:n], AF.Exp, [rstd_t.b], [rstd_t.b], scale=-0.5)
            TT("dve", xn_t.t[:, :, 0:n], xb.t[:, :, 0:n], bcast(rstd_t.t[:, 0:n], 1, 8), ALU.mult,
               [xb.b, rstd_t.b], [xn_t.b])
            for k in range(8):
                dst = H.t[:, k, t0 - hoff:t0 - hoff + n]
                ACT(dst, xn_t.t[:, k, 0:n], AF.Identity, [xn_t.b, A.b, B.b], [hbuf(Hb, t0)],
                    scale=A.t[:, l, g, k:k + 1], bias=B.t[:, l, g, k:k + 1])
                if hf32 is not None:
                    ACT(hf32.t[:, k, 0:n], xn_t.t[:, k, 0:n], AF.Identity, [xn_t.b, A.b, B.b], [hf32.b],
                        scale=A.t[:, l, g, k:k + 1], bias=B.t[:, l, g, k:k + 1])
            if cbk is not None:
                cbk(t0, n)

    def norm_tmps():
        return ([Tl(f"xb{i}", [128, 8, 512], F32) for i in range(2)], Tl("sq", [128, 8, 512], BF16),
                Tl("rstd", [128, 512], F32), Tl("xn", [128, 8, 512], F32))

    def win_cols(l, c0, n=128):
        return Wd["w_in"][l].rearrange("(k p) c -> p k c", p=128)[:, :, c0:c0 + n]

    def proj(wt3, wb, H, Hb, t0, n, hoff=0):
        pb, pbb = bank()
        for k in range(8):
            MM(pb[:, 0:n], wt3[:, k, :], H.t[:, k, t0 - hoff:t0 - hoff + n], k == 0, k == 7, [wb, hbuf(Hb, t0)], [pbb])
        return pb, pbb

    def rowview(ap2d, t0):
        w = 64 if t0 < 2048 else 256
        return ap2d.rearrange("p (r w) -> p r w", w=w)

    def mix(pb, pbb, dst, mu_ap, omu_ap, d, t0, n):
        ACT(dst.t[:, 0:n], pb[:, 0:n], AF.Identity, [pbb, mu.b, omu.b], [dst.b], scale=omu_ap)
        dv = rowview(dst.t[:, 0:n], t0)
        sv = rowview(pb[:, 0:n], t0)
        if d == 0:
            STT("dve", dv[:, :, 1:], sv[:, :, :-1], mu_ap, dv[:, :, 1:], ALU.mult, ALU.add, [pbb, dst.b, mu.b], [dst.b])
        else:
            STT("dve", dv[:, :, :-1], sv[:, :, 1:], mu_ap, dv[:, :, :-1], ALU.mult, ALU.add, [pbb, dst.b, mu.b], [dst.b])

    BLK5 = [(i * 512, 512) for i in range(NBLK)]
    lruo = Tl("lruo", [128, 2, L, 2, 8], F32)

    def residual_update(l, O, Ob, G, t0, n, final, tmp):
        xb, sq_t, rstd_t, on_t, yrow = tmp
        g = blk_group(t0)
        P.dma("sp", xb.t[:, :, 0:n], xs[:, :, t0:t0 + n], reads=[xs_b[t0 // 512]], writes=[xb.b])
        ACT(sq_t.t[:, :, 0:n], O, AF.Square, [Ob], [sq_t.b])
        pb, pbb = bank()
        for k in range(8):
            MM(pb[:, 0:n], ones_bf.t[:, :], sq_t.t[:, k, 0:n], k == 0, k == 7, [ones_bf.b, sq_t.b], [pbb])
        TS("dve", rstd_t.t[:, 0:n], pb[:, 0:n], NE, 1e-6, ALU.mult, ALU.add, [pbb], [rstd_t.b])
        ACT(rstd_t.t[:, 0:n], rstd_t.t[:, 0:n], AF.Ln, [rstd_t.b], [rstd_t.b])
        ACT(rstd_t.t[:, 0:n], rstd_t.t[:, 0:n], AF.Exp, [rstd_t.b], [rstd_t.b], scale=-0.5)
        TT("dve", on_t.t[:, :, 0:n], O, bcast(rstd_t.t[:, 0:n], 1, 8), ALU.mult, [Ob, rstd_t.b], [on_t.b])
        for k in range(8):
            STT("dve", xb.t[:, k, 0:n], on_t.t[:, k, 0:n], G.t[:, l, g, k:k + 1], xb.t[:, k, 0:n],
                ALU.mult, ALU.add, [on_t.b, G.b, xb.b], [xb.b])
        if not final:
            P.dma("sp", xs[:, :, t0:t0 + n], xb.t[:, :, 0:n], reads=[xb.b], writes=[xs_b[t0 // 512]])
        else:
            for tt in range(n // 128):
                yr = yrow[tt % len(yrow)]
                for half in range(2):
                    pt, ptb = bank()
                    for k4 in range(4):
                        k = half * 4 + k4
                        TR(pt[:, k4 * 128:(k4 + 1) * 128], xb.t[:, k, tt * 128:(tt + 1) * 128], ident, [xb.b, cst.b], [ptb])
                    CP("act" if half else "dve", yr.t[:, half * 512:(half + 1) * 512], pt[:, :], [ptb], [yr.b])
                ob = Buf("yout")
                out_bufs.append(ob)
                r0 = t0 + tt * 128
                P.dma("sp", y_out[r0:r0 + 128, :], yr.t[:, :], reads=[yr.b], writes=[ob])

    def res_tmps(final):
        return (Tl("rxb", [128, 8, 512], F32), Tl("rsq", [128, 8, 512], BF16), Tl("rrs", [128, 512], F32),
                Tl("ron", [128, 8, 512], F32), [Tl(f"yrow{i}", [128, D], F32) for i in range(1)] if final else None)

    def rwkv_phase(l, H, Hb, YB):
        LORA = Tl("LORA", [128, 2, T], BF16)
        SG = Tl("SG", [128, T], BF16)
        LW = Tl("LW", [128, 2, D], BF16)
        GUP = Tl("GUP", [128, D], BF16)
        for d in range(2):
            P.dma("pool", LW.t[0:64, d, :], Wd["rwkv_w_up"][l, d], writes=[LW.b])
            P.dma("pool", LW.t[64:128, d, :], Wd["rwkv_a_up"][l, d], writes=[LW.b])
        P.dma("pool", GUP.t[:, :], Wd["rwkv_g_up"][l], writes=[GUP.b])
        wch = [Tl(f"wch{i}", [128, 8, 128], BF16) for i in range(4)]
        wrr = [0]

        def next_w(c0):
            tl = wch[wrr[0] % 4]
            wrr[0] += 1
            P.dma("pool", tl.t[:, :, :], win_cols(l, c0), writes=[tl.b])
            return tl

        tn = ["xr", "xk", "xv", "lw", "aa", "cum", "e1", "e2", "kq", "kk", "kf", "bb"]
        tm = {n_: Tl("t_" + n_, [128, 512], F32) for n_ in tn}
        sqb = Tl("sqb", [128, 512], BF16)
        rkb = Tl("rkb", [128, 512], BF16)

        ybf = YB.t[:, :, :].rearrange("p a t -> p (a t)")
        ybuf = Buf("ybalias")

        class V:
            def __init__(self, ap):
                self.t = ap
                self.b = Buf()
        QR = V(ybf[:, 0:5120].rearrange("p (c x) -> p c x", x=256))
        KTz = V(ybf[:, 5120:10240].rearrange("p (h t) -> p h t", h=2))
        BTz = V(ybf[:, 10240:15360].rearrange("p (h t) -> p h t", h=2))
        VT = V(ybf[:, 15360:17920].rearrange("p (c x) -> p c x", x=128))
        KH = V(ybf[:, 17920:20480].rearrange("p (c x) -> p c x", x=128))
        BH = Tl("BH", [128, 20, 128], BF16)
        WEND = Tl("WEND", [128, 20], F32)
        MSET("pool", KTz.t[:, :, :], 0.0, [KTz.b])
        MSET("pool", BTz.t[:, :, :], 0.0, [BTz.b])

        wt = next_w(3072)
        x0 = tm["xr"]
        for (t0, n) in BLK5:
            pb, pbb = proj(wt.t, wt.b, H, Hb, t0, n)
            for d in range(2):
                mi = 2 * l + d
                mix(pb, pbb, x0, mu.t[:, mi, 24:25], omu.t[:, mi, 24:25], d, t0, n)
                ACT(LORA.t[0:64, d, t0:t0 + n], x0.t[0:64, 0:n], AF.Tanh, [x0.b], [LORA.b])
                CP("dve", LORA.t[64:128, d, t0:t0 + n], x0.t[64:128, 0:n], [x0.b], [LORA.b])
        wt = next_w(3200)
        for (t0, n) in BLK5:
            pb, pbb = proj(wt.t, wt.b, H, Hb, t0, n)
            ACT(SG.t[:, t0:t0 + n], pb[:, 0:n], AF.Sigmoid, [pbb], [SG.b])

        BON = Tl("BON", [128, T], F32)
        YACC = Tl("YACC", [128, 20, 128], F32)
        YAst = Tl("YAst", [128, T], BF16)
        GSL = 3

        class Slot:
            def __init__(self, nm):
                self.NPt = Tl("NPt" + nm, [128, 2, 256], BF16)
                self.MOt = Tl("MOt" + nm, [128, 2, 256], BF16)
                self.NTt = Tl("NTt" + nm, [128, 2, 128], BF16)
                self.Tp = [Tl(f"T{i}" + nm, [128, 2, 128], BF16) for i in range(2)]
                self.TTp = [Tl(f"TT{i}" + nm, [128, 2, 128], BF16) for i in range(2)]
                self.Xa = Tl("Xa" + nm, [128, 2, 128], BF16)
                self.Xb = Tl("Xb" + nm, [128, 2, 128], BF16)
                self.Tfin = None
        slots = [Slot(f"{j}") for j in range(GSL)]
        Btb = Tl("Btb", [128, 128], BF16)
        Utb = Tl("Utb", [128, 128], BF16)
        S32 = Tl("S32", [128, 64], F32)
        SBz = Tl("SBz", [128, 2, 64], BF16)
        BD = Tl("BD", [128, 128], F32)
        OUTS = Tl("OUTS", [128, 64], F32)
        MSET("pool", SBz.t[:, :, :], 0.0, [SBz.b])
        MSET("pool", BD.t[:, :], 0.0, [BD.b])
        st1 = Tl("st1", [128, 8], F32)
        st2 = Tl("st2", [128, 8], F32)

        v3 = lambda ap, x: ap.rearrange("p (h x) -> p h x", x=x)

        def prep_group(chunks, d):
            mNP = cst.t[:, C_US:C_US + 256] if d == 0 else cst.t[:, C_LS:C_LS + 256]
            mNT = cst.t[:, C_LS:C_LS + 128] if d == 0 else cst.t[:, C_US:C_US + 128]

            def lm(i, tr):
                nat = (d == 0) != tr
                base = (0 if nat else 7 * 128) + i * 128
                return lmk.t[:, base:base + 128]
            cs = list(zip(chunks, slots))
            for c, sl in cs:
                cc = slice(c * 128, (c + 1) * 128)
                pNP, bNP = bank()
                for h in range(2):
                    MM(pNP[:, h * 256:(h + 1) * 256], BTz.t[:, h, cc], QR.t[:, c, :], True, True, [BTz.b, QR.b], [bNP])
                pMO, bMO = bank()
                for h in range(2):
                    MM(pMO[:, h * 256:(h + 1) * 256], KTz.t[:, h, cc], QR.t[:, c, :], True, True, [KTz.b, QR.b], [bMO])
                pNT, bNT = bank()
                for h in range(2):
                    MM(pNT[:, h * 128:(h + 1) * 128], QR.t[:, c, 0:128], BTz.t[:, h, cc], True, True, [BTz.b, QR.b], [bNT])
                TT("dve", sl.NPt.t[:, :, :], v3(pNP[:, 0:512], 256), bcast(mNP, 1, 2), ALU.mult, [bNP, cst.b], [sl.NPt.b])
                TT("dve", sl.NTt.t[:, :, :], v3(pNT[:, 0:256], 128), bcast(mNT, 1, 2), ALU.mult, [bNT, cst.b], [sl.NTt.b])
                TT("dve", sl.MOt.t[:, :, :], v3(pMO[:, 0:512], 256), bcast(mNP, 1, 2), ALU.mult, [bMO, cst.b], [sl.MOt.b])
            for c, sl in cs:
                Tc, TTc = sl.Tp[0], sl.TTp[0]
                TT("pool", sl.Xa.t[:, :, :], sl.NPt.t[:, :, 0:128], bcast(lm(0, False), 1, 2), ALU.mult, [sl.NPt.b, lmk.b], [sl.Xa.b])
                TT("pool", Tc.t[:, :, :], sl.Xa.t[:, :, :], bcast(ident_bf.t[:, :], 1, 2), ALU.add, [sl.Xa.b, ident_bf.b], [Tc.b])
                TT("pool", sl.Xb.t[:, :, :], sl.NTt.t[:, :, :], bcast(lm(0, True), 1, 2), ALU.mult, [sl.NTt.b, lmk.b], [sl.Xb.b])
                TT("pool", TTc.t[:, :, :], sl.Xb.t[:, :, :], bcast(ident_bf.t[:, :], 1, 2), ALU.add, [sl.Xb.b, ident_bf.b], [TTc.b])
            cur = 0
            for lev in range(1, 7):
                pend = []
                for c, sl in cs:
                    Tc, TTc = sl.Tp[cur], sl.TTp[cur]
                    pX, bX = bank()
                    for h in range(2):
                        MM(pX[:, h * 128:(h + 1) * 128], sl.NTt.t[:, h, :], Tc.t[:, h, :], True, True, [sl.NTt.b, Tc.b], [bX])
                    for h in range(2):
                        MM(pX[:, 256 + h * 128:256 + (h + 1) * 128], sl.NPt.t[:, h, 0:128], TTc.t[:, h, :], True, True,
                           [sl.NPt.b, TTc.b], [bX])
                    pend.append((pX, bX))
                for (c, sl), (pX, bX) in zip(cs, pend):
                    TT("dve", sl.Xa.t[:, :, :], v3(pX[:, 0:256], 128), bcast(lm(lev, False), 1, 2), ALU.mult, [bX, lmk.b], [sl.Xa.b])
                    TT("dve", sl.Xb.t[:, :, :], v3(pX[:, 256:512], 128), bcast(lm(lev, True), 1, 2), ALU.mult, [bX, lmk.b], [sl.Xb.b])
                pend = []
                for c, sl in cs:
                    Tc, TTc = sl.Tp[cur], sl.TTp[cur]
                    pY, bY_ = bank()
                    for h in range(2):
                        hc2 = slice(h * 128, (h + 1) * 128)
                        MM(pY[:, hc2], TTc.t[:, h, :], sl.Xa.t[:, h, :], True, False, [TTc.b, sl.Xa.b], [bY_])
                        MM(pY[:, hc2], ident_bf.t[:, :], Tc.t[:, h, :], False, True, [ident_bf.b, Tc.b], [bY_])
                    for h in range(2):
                        hc2 = slice(256 + h * 128, 256 + (h + 1) * 128)
                        MM(pY[:, hc2], Tc.t[:, h, :], sl.Xb.t[:, h, :], True, False, [Tc.b, sl.Xb.b], [bY_])
                        MM(pY[:, hc2], ident_bf.t[:, :], TTc.t[:, h, :], False, True, [ident_bf.b, TTc.b], [bY_])
                    pend.append((pY, bY_))
                for (c, sl), (pY, bY_) in zip(cs, pend):
                    Tn, TTn = sl.Tp[1 - cur], sl.TTp[1 - cur]
                    CP("act", Tn.t[:, :, :], v3(pY[:, 0:256], 128), [bY_], [Tn.b])
                    CP("act", TTn.t[:, :, :], v3(pY[:, 256:512], 128), [bY_], [TTn.b])
                cur = 1 - cur
            for c, sl in cs:
                sl.Tfin = sl.Tp[cur]

        def seq_of(c):
            for si, (s0, slen, _) in enumerate(SEQS):
                if s0 // 128 <= c < (s0 + slen) // 128:
                    return si, s0 // 128, slen // 128
            raise ValueError

        def chain_one(c, sl, d, hp):
            si, cb, nch = seq_of(c)
            first = (c == cb) if d == 0 else (c == cb + nch - 1)
            lastc = (c == cb + nch - 1) if d == 0 else (c == cb)
            if first:
                if si == 0:
                    for h in range(2):
                        hs = slice(h * 64, (h + 1) * 64)
                        P.dma("sp", BD.t[hs, hs], st_rwkv[l, d, 2 * hp + h], writes=[BD.b])
                    pt, ptb = bank()
                    TR(pt[:, 0:128], BD.t[:, :], ident, [BD.b, cst.b], [ptb])
                    for h in range(2):
                        hs = slice(h * 64, (h + 1) * 64)
                        CP("dve", S32.t[hs, :], pt[hs, hs], [ptb], [S32.b])
                else:
                    MSET("pool", S32.t[:, :], 0.0, [S32.b])
                CP("act", SBz.t[0:64, 0, :], S32.t[0:64, :], [S32.b], [SBz.b])
                CP("act", SBz.t[64:128, 1, :], S32.t[64:128, :], [S32.b], [SBz.b])
            NPt, MOt, Tfin = sl.NPt, sl.MOt, sl.Tfin
            pBt, bBt = bank()
            for h in range(2):
                hc = slice(h * 64, (h + 1) * 64)
                MM(pBt[:, hc], QR.t[:, c, 0:128], SBz.t[:, h, :], True, False, [QR.b, SBz.b], [bBt])
                MM(pBt[:, hc], MOt.t[:, h, 0:128], VT.t[:, c, hc], False, True, [MOt.b, VT.b], [bBt])
            CP("act", Btb.t[:, :], pBt[:, 0:128], [bBt], [Btb.b])
            pU, bU = bank()
            for h in range(2):
                hc = slice(h * 64, (h + 1) * 64)
                MM(pU[:, hc], Tfin.t[:, h, :], Btb.t[:, hc], True, True, [Tfin.b, Btb.b], [bU])
            CP("dve", Utb.t[:, :], pU[:, 0:128], [bU], [Utb.b])
            pY, bY = bank()
            for h in range(2):
                hc = slice(h * 64, (h + 1) * 64)
                MM(pY[:, hc], QR.t[:, c, 128:256], SBz.t[:, h, :], True, False, [QR.b, SBz.b], [bY])
                MM(pY[:, hc], NPt.t[:, h, 128:256], Utb.t[:, hc], False, False, [NPt.b, Utb.b], [bY])
                MM(pY[:, hc], MOt.t[:, h, 128:256], VT.t[:, c, hc], False, True, [MOt.b, VT.b], [bY])
            if d == 0:
                CP("act", YACC.t[:, c, :], pY[:, 0:128], [bY], [YACC.b])
            else:
                TT("dve", YACC.t[:, c, :], pY[:, 0:128], YACC.t[:, c, :], ALU.add, [bY, YACC.b], [YACC.b])
            pS, bS = bank()
            for h in range(2):
                hc = slice(h * 64, (h + 1) * 64)
                MM(pS[:, hc], BH.t[:, c, :], Utb.t[:, hc], True, False, [BH.b, Utb.b], [bS])
                MM(pS[:, hc], KH.t[:, c, :], VT.t[:, c, hc], False, True, [KH.b, VT.b], [bS])
            for h in range(2):
                hs = slice(h * 64, (h + 1) * 64)
                STT("dve", S32.t[hs, :], S32.t[hs, :], WEND.t[hs, c:c + 1], pS[hs, h * 64:(h + 1) * 64], ALU.mult, ALU.add,
                    [S32.b, WEND.b, bS], [S32.b])
            CP("act", SBz.t[0:64, 0, :], S32.t[0:64, :], [S32.b], [SBz.b])
            CP("act", SBz.t[64:128, 1, :], S32.t[64:128, :], [S32.b], [SBz.b])
            if lastc and si > 0:
                for h in range(2):
                    hs = slice(h * 64, (h + 1) * 64)
                    CP("dve", BD.t[hs, hs], S32.t[hs, :], [S32.b], [BD.b])
                pt, ptb = bank()
                TR(pt[:, 0:128], BD.t[:, :], ident, [BD.b, cst.b], [ptb])
                for h in range(2):
                    hs = slice(h * 64, (h + 1) * 64)
                    CP("dve", OUTS.t[hs, :], pt[hs, hs], [ptb], [OUTS.b])
                ob = Buf("nsr")
                out_bufs.append(ob)
                P.dma("sp", ns_rwkv[si - 1, l, d, 2 * hp:2 * hp + 2].rearrange("h i j -> (h i) j"), OUTS.t[:, :],
                      reads=[OUTS.b], writes=[ob])

        for hp in range(8):
            hcols = slice(hp * 128, (hp + 1) * 128)
            for d in range(2):
                mi = 2 * l + d
                wr = next_w(hp * 128)
                wk = next_w(1024 + hp * 128)
                wv = next_w(2048 + hp * 128)
                for (t0, n) in BLK5:
                    c4 = t0 // 128
                    bl = slice(t0, t0 + n)
                    xr, xk, xv, lw, aa, cum, e1, e2, kq, kkt, kf, bb = [tm[x_] for x_ in tn]
                    pr, prb = proj(wr.t, wr.b, H, Hb, t0, n)
                    mix(pr, prb, xr, mu.t[:, mi, hp:hp + 1], omu.t[:, mi, hp:hp + 1], d, t0, n)
                    pk, pkb = proj(wk.t, wk.b, H, Hb, t0, n)
                    mix(pk, pkb, xk, mu.t[:, mi, 8 + hp:9 + hp], omu.t[:, mi, 8 + hp:9 + hp], d, t0, n)
                    pv, pvb = proj(wv.t, wv.b, H, Hb, t0, n)
                    mix(pv, pvb, xv, mu.t[:, mi, 16 + hp:17 + hp], omu.t[:, mi, 16 + hp:17 + hp], d, t0, n)
                    pw, pwb = bank()
                    MM(pw[:, 0:n], LW.t[0:64, d, hcols], LORA.t[0:64, d, bl], True, True, [LW.b, LORA.b], [pwb])
                    pa, pab = bank()
                    MM(pa[:, 0:n], LW.t[64:128, d, hcols], LORA.t[64:128, d, bl], True, True, [LW.b, LORA.b], [pab])
                    ACT(lw.t[:, :], pw[:, 0:n], AF.Sigmoid, [pwb, w0.b], [lw.b], bias=w0.t[:, mi, hp:hp + 1])
                    ACT(aa.t[:, :], pa[:, 0:n], AF.Sigmoid, [pab, a0.b], [aa.b], bias=a0.t[:, mi, hp:hp + 1])
                    TS("pool", lw.t[:, :], lw.t[:, :], -0.6065306597126334, None, ALU.mult, None, [lw.b], [lw.b])
                    if d == 0:
                        SCAN(cum.t[:, :], cst.t[:, C_MF:C_MF + 512], lw.t[:, :], 0.0, [cst.b, lw.b], [cum.b])
                        cend = cum.t[:, :].rearrange("p (c x) -> p c x", x=128)[:, :, 127]
                    else:
                        SCAN(rev_last(cum.t[:, :]), rev_last(cst.t[:, C_MB:C_MB + 512]), rev_last(lw.t[:, :]), 0.0,
                             [cst.b, lw.b], [cum.b])
                        cend = cum.t[:, :].rearrange("p (c x) -> p c x", x=128)[:, :, 0]
                    c3 = lambda ap: ap.rearrange("p (c x) -> p c x", x=128)
                    ACT(WEND.t[:, c4:c4 + 4], cend, AF.Exp, [cum.b], [WEND.b])
                    TS("pool", kq.t[:, :], xk.t[:, :], kk_.t[:, l, hp:hp + 1], None, ALU.mult, None, [xk.b, kk_.b], [kq.b])
                    ACT(sqb.t[:, :], kq.t[:, :], AF.Square, [kq.b], [sqb.b])
                    pn, pnb = bank()
                    MM(pn[:, 0:n], bd_bf.t[:, :], sqb.t[:, :], True, True, [bd_bf.b, sqb.b], [pnb])
                    TS("dve", kkt.t[:, :], pn[:, 0:n], 1e-24, None, ALU.add, None, [pnb], [kkt.b])
                    ACT(kkt.t[:, :], kkt.t[:, :], AF.Ln, [kkt.b], [kkt.b])
                    ACT(kkt.t[:, :], kkt.t[:, :], AF.Exp, [kkt.b], [kkt.b], scale=-0.5)
                    TT("dve", kkt.t[:, :], kkt.t[:, :], kq.t[:, :], ALU.mult, [kkt.b, kq.b], [kkt.b])
                    TS("dve", kf.t[:, :], aa.t[:, :], ka_.t[:, l, hp:hp + 1], omka.t[:, l, hp:hp + 1], ALU.mult, ALU.add,
                       [aa.b, ka_.b, omka.b], [kf.b])
                    TT("pool", kf.t[:, :], kf.t[:, :], xk.t[:, :], ALU.mult, [kf.b, xk.b], [kf.b])
                    TT("pool", bb.t[:, :], kkt.t[:, :], aa.t[:, :], ALU.mult, [kkt.b, aa.b], [bb.b])
                    STT("dve", rkb.t[:, :], xr.t[:, :], rk_.t[:, l, hp:hp + 1], kf.t[:, :], ALU.mult, ALU.mult,
                        [xr.b, rk_.b, kf.b], [rkb.b])
                    pbn, pbnb = bank()
                    MM(pbn[:, 0:n], bd_bf.t[:, :], rkb.t[:, :], True, True, [bd_bf.b, rkb.b], [pbnb])
                    if d == 0:
                        TT("dve", BON.t[:, bl], pbn[:, 0:n], xv.t[:, :], ALU.mult, [pbnb, xv.b], [BON.b])
                    else:
                        TT("dve", kq.t[:, :], pbn[:, 0:n], xv.t[:, :], ALU.mult, [pbnb, xv.b], [kq.b])
                        TT("pool", BON.t[:, bl], BON.t[:, bl], kq.t[:, :], ALU.add, [BON.b, kq.b], [BON.b])
                    pt, ptb = bank()
                    for c_ in range(4):
                        TR(pt[:, c_ * 128:(c_ + 1) * 128], xv.t[:, c_ * 128:(c_ + 1) * 128], ident, [xv.b, cst.b], [ptb])
                    CP("act", VT.t[:, c4:c4 + 4, :], c3(pt[:, 0:512]), [ptb], [VT.b])
                    ACT(e1.t[:, :], cum.t[:, :], AF.Exp, [cum.b], [e1.b])
                    TT("dve", QR.t[:, c4:c4 + 4, 128:256], c3(xr.t[:, :]), c3(e1.t[:, :]), ALU.mult, [xr.b, e1.b], [QR.b])
                    TT("pool", e2.t[:, :], cum.t[:, :], lw.t[:, :], ALU.subtract, [cum.b, lw.b], [e2.b])
                    ACT(e2.t[:, :], e2.t[:, :], AF.Exp, [e2.b], [e2.b])
                    TT("dve", QR.t[:, c4:c4 + 4, 0:128], c3(kkt.t[:, :]), c3(e2.t[:, :]), ALU.mult, [kkt.b, e2.b], [QR.b])
                    ACT(e1.t[:, :], cum.t[:, :], AF.Exp, [cum.b], [e1.b], scale=-1.0)
                    for h in range(2):
                        hs = slice(h * 64, (h + 1) * 64)
                        TT("pool", KTz.t[hs, h, bl], kf.t[hs, :], e1.t[hs, :], ALU.mult, [kf.b, e1.b], [KTz.b])
                        STT("dve", BTz.t[hs, h, bl], bb.t[hs, :], -1.0, e1.t[hs, :], ALU.mult, ALU.mult, [bb.b, e1.b], [BTz.b])
                    TT("dve", c3(e2.t[:, :]), bcast(cend, 2, 128), c3(cum.t[:, :]), ALU.subtract, [cum.b], [e2.b])
                    ACT(e2.t[:, :], e2.t[:, :], AF.Exp, [e2.b], [e2.b])
                    TT("pool", kf.t[:, :], kf.t[:, :], e2.t[:, :], ALU.mult, [kf.b, e2.b], [kf.b])
                    STT("dve", bb.t[:, :], bb.t[:, :], -1.0, e2.t[:, :], ALU.mult, ALU.mult, [bb.b, e2.b], [bb.b])
                    pt, ptb = bank()
                    for c_ in range(4):
                        TR(pt[:, c_ * 128:(c_ + 1) * 128], kf.t[:, c_ * 128:(c_ + 1) * 128], ident, [kf.b, cst.b], [ptb])
                    CP("act", KH.t[:, c4:c4 + 4, :], c3(pt[:, 0:512]), [ptb], [KH.b])
                    pt, ptb = bank()
                    for c_ in range(4):
                        TR(pt[:, c_ * 128:(c_ + 1) * 128], bb.t[:, c_ * 128:(c_ + 1) * 128], ident, [bb.b, cst.b], [ptb])
                    CP("dve", BH.t[:, c4:c4 + 4, :], c3(pt[:, 0:512]), [ptb], [BH.b])
                order = []
                for si, (s0, slen, _) in enumerate(SEQS):
                    nch = slen // 128
                    cbase = s0 // 128
                    order += [cbase + ci for ci in (range(nch) if d == 0 else range(nch - 1, -1, -1))]
                for gi in range(0, len(order), GSL):
                    g = order[gi:gi + GSL]
                    prep_group(g, d)
                    for c_, sl_ in zip(g, slots):
                        chain_one(c_, sl_, d, hp)
            yc, ysq, yn, t1 = tm["xr"], tm["xk"], tm["xv"], tm["lw"]
            for (t0, n) in BLK5:
                c4 = t0 // 128
                bl = slice(t0, t0 + n)
                yv = YACC.t[:, c4:c4 + 4, :].rearrange("p c (h i) -> p (c h) i", i=64)
                v8 = lambda ap: ap.rearrange("p (g i) -> p g i", i=64)
                RSUM(st1.t[:, :], yv, [YACC.b], [st1.b])
                TS("dve", st1.t[:, :], st1.t[:, :], 1.0 / 64, None, ALU.mult, None, [st1.b], [st1.b])
                TT("dve", v8(yc.t[:, :]), yv, bcast(st1.t[:, :], 2, 64), ALU.subtract, [YACC.b, st1.b], [yc.b])
                TT("pool", ysq.t[:, :], yc.t[:, :], yc.t[:, :], ALU.mult, [yc.b], [ysq.b])
                RSUM(st2.t[:, :], v8(ysq.t[:, :]), [ysq.b], [st2.b])
                TS("dve", st2.t[:, :], st2.t[:, :], 1.0 / 64, 64e-5, ALU.mult, ALU.add, [st2.b], [st2.b])
                ACT(st2.t[:, :], st2.t[:, :], AF.Ln, [st2.b], [st2.b])
                ACT(st2.t[:, :], st2.t[:, :], AF.Exp, [st2.b], [st2.b], scale=-0.5)
                TT("dve", v8(yn.t[:, :]), v8(yc.t[:, :]), bcast(st2.t[:, :], 2, 64), ALU.mult, [yc.b, st2.b], [yn.b])
                pt, ptb = bank()
                for c_ in range(4):
                    TR(pt[:, c_ * 128:(c_ + 1) * 128], yn.t[:, c_ * 128:(c_ + 1) * 128], ident, [yn.b, cst.b], [ptb])
                pg, pgb = bank()
                MM(pg[:, 0:n], GUP.t[:, hcols], SG.t[:, bl], True, True, [GUP.b, SG.b], [pgb])
                ACT(t1.t[:, :], pt[:, 0:n], AF.Identity, [ptb, lnw.b, lnb.b], [t1.b],
                    scale=lnw.t[:, l, hp:hp + 1], bias=lnb.t[:, l, hp:hp + 1])
                TT("pool", t1.t[:, :], t1.t[:, :], BON.t[:, bl], ALU.add, [t1.b, BON.b], [t1.b])
                TT("dve", YAst.t[:, bl], t1.t[:, :], pg[:, 0:n], ALU.mult, [t1.b, pgb], [YAst.b])
            P.dma("sp", ya_s[:, hp, :], YAst.t[:, :], reads=[YAst.b], writes=[ya_sb])

    ya_sb = Buf("ya_s")

    def lru_phase(l, H, Hb, YB, YBb):
        wch = [Tl(f"lwch{i}", [128, 8, 128], BF16) for i in range(4)]
        wrr = [0]

        def next_w(c0):
            tl = wch[wrr[0] % 4]
            wrr[0] += 1
            P.dma("pool", tl.t[:, :, :], win_cols(l, c0), writes=[tl.b])
            return tl
        wrg = [Tl(f"wrg{i}", [128, 2, 2, 128], BF16) for i in range(2)]
        GT = 2048
        Aa = [Tl(f"lA{d}", [128, GT], F32) for d in range(2)]
        Uu = [Tl(f"lU{d}", [128, GT], F32) for d in range(2)]
        HF = Tl("lHF", [128, GT], F32)
        GL = Tl("lGL", [128, GT], F32)
        xc = Tl("lxc", [128, 512], F32)
        xcb = Tl("lxcb", [128, 512], BF16)
        g1 = Tl("lg1", [128, 512], F32)
        rg = Tl("lrg", [128, 512], F32)
        ig = Tl("lig", [128, 512], F32)
        a2 = Tl("la2", [128, 512], F32)
        groups = [(BLK5[0:4], [(0, SEQS[0])], 0, 2048), (BLK5[4:5], [(1, SEQS[1]), (2, SEQS[2])], 2048, 512)]
        for j in range(8):
            jc = slice(j, j + 1)
            wx = next_w(3328 + j * 128)
            wg = next_w(4352 + j * 128)
            wr_ = wrg[j % 2]
            for d in range(2):
                P.dma("pool", wr_.t[:, d, 0, :], Wd["lru_w_rg"][l, d, j], writes=[wr_.b])
                P.dma("pool", wr_.t[:, d, 1, :], Wd["lru_w_ig"][l, d, j], writes=[wr_.b])
            for (gblocks, gseqs, g0, gn) in groups:
                for (t0, n) in gblocks:
                    o = t0 - g0
                    ol = slice(o, o + n)
                    px, pxb = proj(wx.t, wx.b, H, Hb, t0, n)
                    pg_, pgb = proj(wg.t, wg.b, H, Hb, t0, n)
                    ACT(xc.t[:, :], px[:, 0:n], AF.Identity, [pxb, cw.b, cb.b], [xc.b],
                        scale=cw.t[:, 4 * l + 2, jc], bias=cb.t[:, l, jc])
                    xv3 = rowview(xc.t[:, 0:n], t0)
                    pv3 = rowview(px[:, 0:n], t0)
                    STT("dve", xv3[:, :, 2:], pv3[:, :, :-2], cw.t[:, 4 * l + 0, jc], xv3[:, :, 2:], ALU.mult, ALU.add,
                        [pxb, xc.b, cw.b], [xc.b])
                    STT("dve", xv3[:, :, 1:], pv3[:, :, :-1], cw.t[:, 4 * l + 1, jc], xv3[:, :, 1:], ALU.mult, ALU.add,
                        [pxb, xc.b, cw.b], [xc.b])
                    STT("dve", xv3[:, :, :-1], pv3[:, :, 1:], cw.t[:, 4 * l + 3, jc], xv3[:, :, :-1], ALU.mult, ALU.add,
                        [pxb, xc.b, cw.b], [xc.b])
                    CP("pool", xcb.t[:, :], xc.t[:, :], [xc.b], [xcb.b])
                    ACT(g1.t[:, :], pg_[:, 0:n], AF.Square, [pgb], [g1.b])
                    TS("pool", g1.t[:, :], g1.t[:, :], 0.044715, 1.0, ALU.mult, ALU.add, [g1.b], [g1.b])
                    TT("dve", g1.t[:, :], g1.t[:, :], pg_[:, 0:n], ALU.mult, [g1.b, pgb], [g1.b])
                    ACT(g1.t[:, :], g1.t[:, :], AF.Sigmoid, [g1.b], [g1.b], scale=1.5957691216057308)
                    TT("dve", GL.t[:, ol], g1.t[:, :], pg_[:, 0:n], ALU.mult, [g1.b, pgb], [GL.b])
                    for d in range(2):
                        mi = 2 * l + d
                        pr_, prb = bank()
                        MM(pr_[:, 0:n], wr_.t[:, d, 0, :], xcb.t[:, :], True, True, [wr_.b, xcb.b], [prb])
                        pi_, pib = bank()
                        MM(pi_[:, 0:n], wr_.t[:, d, 1, :], xcb.t[:, :], True, True, [wr_.b, xcb.b], [pib])
                        ACT(rg.t[:, :], pr_[:, 0:n], AF.Sigmoid, [prb, brg.b], [rg.b], bias=brg.t[:, mi, jc])
                        ACT(ig.t[:, :], pi_[:, 0:n], AF.Sigmoid, [pib, big.b], [ig.b], bias=big.t[:, mi, jc])
                        ACT(Aa[d].t[:, ol], rg.t[:, :], AF.Exp, [rg.b, c8.b], [Aa[d].b], scale=c8.t[:, mi, jc])
                        TT("pool", a2.t[:, :], Aa[d].t[:, ol], Aa[d].t[:, ol], ALU.mult, [Aa[d].b], [a2.b])
                        ACT(a2.t[:, :], a2.t[:, :], AF.Sqrt, [a2.b], [a2.b], scale=-1.0, bias=1.0)
                        TT("pool", a2.t[:, :], a2.t[:, :], ig.t[:, :], ALU.mult, [a2.b, ig.b], [a2.b])
                        TT("dve", Uu[d].t[:, ol], a2.t[:, :], xc.t[:, :], ALU.mult, [a2.b, xc.b], [Uu[d].b])
                for (si, (s0, slen, _)) in gseqs:
                    o = s0 - g0
                    sl = slice(o, o + slen)
                    i0 = h0l.t[:, 2 * l + 0, jc] if si == 0 else 0.0
                    i1 = h0l.t[:, 2 * l + 1, jc] if si == 0 else 0.0
                    SCAN(HF.t[:, sl], Aa[0].t[:, sl], Uu[0].t[:, sl], i0, [Aa[0].b, Uu[0].b, h0l.b], [HF.b])
                    SCAN(rev_last(Aa[0].t[:, sl]), rev_last(Aa[1].t[:, sl]), rev_last(Uu[1].t[:, sl]), i1,
                         [Aa[1].b, Uu[1].b, h0l.b, HF.b], [Aa[0].b])
                    if si > 0:
                        CP("act", lruo.t[:, si - 1, l, 0, jc], HF.t[:, o + slen - 1:o + slen], [HF.b], [lruo.b])
                        CP("act", lruo.t[:, si - 1, l, 1, jc], Aa[0].t[:, o:o + 1], [Aa[0].b], [lruo.b])
                TT("pool", HF.t[:, 0:gn], HF.t[:, 0:gn], Aa[0].t[:, 0:gn], ALU.add, [HF.b, Aa[0].b], [HF.b])
                TT("dve", YB.t[:, j, g0:g0 + gn], HF.t[:, 0:gn], GL.t[:, 0:gn], ALU.mult, [HF.b, GL.b],
                   [YBb[i] for i in range(g0 // 512, (g0 + gn) // 512)])
        for b in range(2):
            for d in range(2):
                ob = Buf("nsl")
                out_bufs.append(ob)
                P.dma("sp", ns_lru[b, l, d].rearrange("(k p) -> p k", p=128), lruo.t[:, b, l, d, :], reads=[lruo.b],
                      writes=[ob], allow_slow_non_contiguous=True)

    def merge_b(l, H, Hb, YB, YBb):
        WB = Tl("WB", [128, 8, D], BF16)
        WGB = Tl("WGB", [128, 8, D], BF16)
        mtmp = Tl("mtmp", [128, 8, 512], BF16)
        sg = Tl("msg", [128, 512], F32)
        for k in range(8):
            P.dma("pool", WB.t[:, k, :], Wd["w_proj_b"][l, k * 128:(k + 1) * 128, :], writes=[WB.b])
            P.dma("pool", WGB.t[:, k, :], Wd["w_in"][l, k * 128:(k + 1) * 128, 6400:7424], writes=[WGB.b])
        for (t0, n) in BLK5:
            ybb = YBb[t0 // 512]
            for j in range(8):
                js = slice(j * 128, (j + 1) * 128)
                pg_, pgb = proj(WGB.t[:, :, js], WGB.b, H, Hb, t0, n)
                pb_, pbb = proj(WB.t[:, :, js], WB.b, YB, ybb, t0, n)
                ACT(sg.t[:, :], pg_[:, 0:n], AF.Sigmoid, [pgb, bmer.b], [sg.b], bias=bmer.t[:, l, 8 + j:9 + j])
                TT("dve", mtmp.t[:, j, :], sg.t[:, :], pb_[:, 0:n], ALU.mult, [sg.b, pbb], [mtmp.b])
            CP("pool", YB.t[:, :, t0:t0 + n], mtmp.t[:, :, :], [mtmp.b], [ybb])

    def merge_a(l, H, Hb, YB, YBb):
        YA = Tl("YA", [128, 8, T], BF16)
        for k in range(8):
            P.dma("sp", YA.t[:, k, :], ya_s[:, k, :], reads=[ya_sb], writes=[YA.b])
        wch = [Tl(f"mwch{i}", [128, 8, 128], BF16) for i in range(4)]
        sg = Tl("msg2", [128, 512], F32)
        tt_ = Tl("mtt", [128, 512], F32)
        for j in range(8):
            wa = wch[(2 * j) % 4]
            wga = wch[(2 * j + 1) % 4]
            P.dma("pool", wa.t[:, :, :], Wd["w_proj_a"][l].rearrange("(k p) c -> p k c", p=128)[:, :, j * 128:(j + 1) * 128],
                  writes=[wa.b])
            P.dma("pool", wga.t[:, :, :], win_cols(l, 5376 + j * 128), writes=[wga.b])
            for (t0, n) in BLK5:
                ybb = YBb[t0 // 512]
                pg_, pgb = proj(wga.t, wga.b, H, Hb, t0, n)
                pa_, pab = proj(wa.t, wa.b, YA, YA.b, t0, n)
                ACT(sg.t[:, :], pg_[:, 0:n], AF.Sigmoid, [pgb, bmer.b], [sg.b], bias=bmer.t[:, l, j:j + 1])
                TT("dve", tt_.t[:, :], sg.t[:, :], pa_[:, 0:n], ALU.mult, [sg.b, pab], [tt_.b])
                TT("pool", YB.t[:, j, t0:t0 + n], tt_.t[:, :], YB.t[:, j, t0:t0 + n], ALU.add, [tt_.b, ybb], [ybb])

    def out_proj(l, YB, YBb):
        WO = Tl("WO", [128, 8, D], BF16)
        for k in range(8):
            P.dma("pool", WO.t[:, k, :], Wd["w_out"][l, k * 128:(k + 1) * 128, :], writes=[WO.b])
        O = Tl("O", [128, 8, 512], F32)
        rt = res_tmps(False)
        for (t0, n) in BLK5:
            for j in range(8):
                po, pob = proj(WO.t[:, :, j * 128:(j + 1) * 128], WO.b, YB, YBb[t0 // 512], t0, n)
                CP("act" if j % 2 else "dve", O.t[:, j, 0:n], po[:, 0:n], [pob], [O.b])
            residual_update(l, O.t[:, :, 0:n], O.b, G1, t0, n, False, rt)

    def ffn_phase(l):
        moe = (l % 2 == 1)
        final = (l == L - 1)
        if not moe:
            i = l // 2
            groups = [(Wd["ffn_w1"][i][:, g * 2048:(g + 1) * 2048], Wd["ffn_w3"][i][:, g * 2048:(g + 1) * 2048],
                       Wd["ffn_w2"][i][g * 2048:(g + 1) * 2048, :], 16, None) for g in range(2)]
        else:
            i = l // 2
            groups = [(Wd["moe_w1"][i, e], Wd["moe_w3"][i, e], Wd["moe_w2"][i, e], 28, e) for e in range(8)]
        halves = [BLK5]
        HT = T
        FG = 4
        for hf, blocks in enumerate(halves):
            hoff = hf * HT
            with P.scope():
                H2 = Tl("H2", [128, 8, HT], BF16)
                GTt = Tl("GTt", [32, HT], F32)
                MSET("pool", GTt.t[:, :], 0.0, [GTt.b])
                GB = Tl("GB", [128, HT], F32)
                with P.scope():
                    tmp = norm_tmps()
                    if moe:
                        hf32 = Tl("hf32", [128, 8, 512], F32)
                        RT = Tl("RT", [128, 8, 8], F32)
                        P.dma("sp", RT.t[:, :, :], Wd["moe_router"][i].rearrange("(k p) e -> p k e", p=128), writes=[RT.b])
                        lg = Tl("lg", [128, 8], F32)
                        lg2 = Tl("lg2", [128, 8], F32)
                        m1 = Tl("m1", [128, 1], F32)
                        m2 = Tl("m2", [128, 1], F32)

                        def gates(t0, n):
                            for tt in range(n // 128):
                                ts_ = slice(tt * 128, (tt + 1) * 128)
                                pl, plb = bank()
                                for k in range(8):
                                    MM(pl[:, 0:8], hf32.t[:, k, ts_], RT.t[:, k, :], k == 0, k == 7, [hf32.b, RT.b], [plb])
                                CP("act", lg.t[:, :], pl[:, 0:8], [plb], [lg.b])
                                RMAX(m1.t[:, :], lg.t[:, :], [lg.b], [m1.b])
                                TS("dve", lg2.t[:, :], lg.t[:, :], m1.t[:, 0:1], None, ALU.is_equal, None, [lg.b, m1.b], [lg2.b])
                                STT("dve", lg2.t[:, :], lg2.t[:, :], -1e30, lg.t[:, :], ALU.mult, ALU.add, [lg2.b, lg.b], [lg2.b])
                                RMAX(m2.t[:, :], lg2.t[:, :], [lg2.b], [m2.b])
                                TS("dve", lg2.t[:, :], lg.t[:, :], m2.t[:, 0:1], None, ALU.is_ge, None, [lg.b, m2.b], [lg2.b])
                                TS("dve", m1.t[:, :], m1.t[:, :], -1.0, None, ALU.mult, None, [m1.b], [m1.b])
                                ACT(lg.t[:, :], lg.t[:, :], AF.Exp, [lg.b, m1.b], [lg.b], bias=m1.t[:, 0:1])
                                TT("dve", lg.t[:, :], lg.t[:, :], lg2.t[:, :], ALU.mult, [lg.b, lg2.b], [lg.b])
                                RSUM(m2.t[:, :], lg.t[:, :], [lg.b], [m2.b])
                                P.op("dve", lambda e: e.reciprocal(m2.t[:, :], m2.t[:, :]), [m2.b], [m2.b])
                                TS("dve", lg.t[:, :], lg.t[:, :], m2.t[:, 0:1], None, ALU.mult, None, [lg.b, m2.b], [lg.b])
                                pt, ptb = bank()
                                TR(pt[0:8, 0:128], lg.t[:, :], ident, [lg.b, cst.b], [ptb])
                                o = t0 - hoff + tt * 128
                                CP("act", GTt.t[0:8, o:o + 128], pt[0:8, 0:128], [ptb], [GTt.b])
                        norm_mod(l, A2, B2, H2, H2.b, blocks, hoff, tmp, hf32=hf32, cbk=gates)
                    else:
                        norm_mod(l, A2, B2, H2, H2.b, blocks, hoff, tmp)
                OACC = Tl("OACC", [128, 8, HT], F32)
                with P.scope():
                    HID = Tl("HID", [128, FG, HT], BF16)
                    w13 = [Tl(f"w13_{i_}", [128, 8, 128], BF16) for i_ in range(4)]
                    w2g = [Tl(f"w2g{i_}", [128, FG, D], BF16) for i_ in range(2)]
                    sil = [Tl(f"sil{i_}", [128, 512], F32) for i_ in range(2)]
                    wc = [0]
                    ng = [0]
                    for gi, (w1, w3, w2, nf, ex) in enumerate(groups):
                        if ex is not None:
                            for (t0, n) in blocks:
                                o = t0 - hoff
                                pg, pgb = bank()
                                MM(pg[:, 0:n], cst.t[0:8, C_ES + ex * 128:C_ES + (ex + 1) * 128], GTt.t[0:8, o:o + n], True, True,
                                   [cst.b, GTt.b], [pgb])
                                CP("act", GB.t[:, o:o + n], pg[:, 0:n], [pgb], [GB.b])
                        w1v = w1.rearrange("(k p) c -> p k c", p=128)
                        w3v = w3.rearrange("(k p) c -> p k c", p=128)
                        w2v = w2.rearrange("(f p) c -> p f c", p=128)
                        for fg in range(nf // FG):
                            wt2 = w2g[ng[0] % 2]
                            P.dma("pool", wt2.t[:, :, :], w2v[:, fg * FG:(fg + 1) * FG, :], writes=[wt2.b])
                            for fl in range(FG):
                                f = fg * FG + fl
                                fs = slice(f * 128, (f + 1) * 128)
                                w1t = w13[wc[0] % 4]
                                w3t = w13[(wc[0] + 1) % 4]
                                wc[0] += 2
                                P.dma("pool", w1t.t[:, :, :], w1v[:, :, fs], writes=[w1t.b])
                                P.dma("pool", w3t.t[:, :, :], w3v[:, :, fs], writes=[w3t.b])
                                for bi, (t0, n) in enumerate(blocks):
                                    o = t0 - hoff
                                    s_ = sil[(f + bi) % 2]
                                    p1, p1b = proj(w1t.t, w1t.b, H2, H2.b, t0, n, hoff)
                                    p3, p3b = proj(w3t.t, w3t.b, H2, H2.b, t0, n, hoff)
                                    ACT(s_.t[:, 0:n], p1[:, 0:n], AF.Silu, [p1b], [s_.b])
                                    if ex is None:
                                        TT("dve", HID.t[:, fl, o:o + n], s_.t[:, 0:n], p3[:, 0:n], ALU.mult, [s_.b, p3b], [HID.b])
                                    else:
                                        TT("dve", s_.t[:, 0:n], s_.t[:, 0:n], p3[:, 0:n], ALU.mult, [s_.b, p3b], [s_.b])
                                        TT("pool", HID.t[:, fl, o:o + n], s_.t[:, 0:n], GB.t[:, o:o + n], ALU.mult, [s_.b, GB.b], [HID.b])
                            for j in range(8):
                                for (t0, n) in blocks:
                                    o = t0 - hoff
                                    po, pob = bank()
                                    for fl in range(FG):
                                        MM(po[:, 0:n], wt2.t[:, fl, j * 128:(j + 1) * 128], HID.t[:, fl, o:o + n], fl == 0, fl == FG - 1,
                                           [wt2.b, HID.b], [pob])
                                    if ng[0] == 0:
                                        CP("act", OACC.t[:, j, o:o + n], po[:, 0:n], [pob], [OACC.b])
                                    else:
                                        TT("dve", OACC.t[:, j, o:o + n], po[:, 0:n], OACC.t[:, j, o:o + n], ALU.add, [pob, OACC.b], [OACC.b])
                            ng[0] += 1
                with P.scope():
                    rt = res_tmps(final)
                    for (t0, n) in blocks:
                        o = t0 - hoff
                        residual_update(l, OACC.t[:, :, o:o + n], OACC.b, G2, t0, n, final, rt)

    for l in range(L):
        with P.scope():
            YB = Tl("YB", [128, 8, T], BF16)
            YBb = [Buf(f"YB{i}") for i in range(NBLK)]
            with P.scope():
                H = Tl("H", [128, 8, T], BF16)
                Hb = [Buf(f"H{i}") for i in range(NBLK)]
                P.mark(f"L{l} start")
                with P.scope():
                    norm_mod(l, A1, B1, H, Hb, BLK5, 0, norm_tmps())
                P.mark(f"L{l} norm1 done")
                with P.scope():
                    rwkv_phase(l, H, Hb, YB)
                P.mark(f"L{l} rwkv done")
                with P.scope():
                    lru_phase(l, H, Hb, YB, YBb)
                P.mark(f"L{l} lru done")
                with P.scope():
                    merge_b(l, H, Hb, YB, YBb)
                P.mark(f"L{l} merge_b done")
                with P.scope():
                    merge_a(l, H, Hb, YB, YBb)
                P.mark(f"L{l} merge_a done")
            with P.scope():
                out_proj(l, YB, YBb)
            P.mark(f"L{l} out_proj done")
        with P.scope():
            ffn_phase(l)
        P.mark(f"L{l} ffn done")

    P.barrier()
    P.emit()
    P.close()
    nc._marks = getattr(P, "marks", [])
    return nc


_CACHE = {}


def kernel(**inputs):
    inp = {k: np.ascontiguousarray(np.asarray(v)) for k, v in inputs.items()}
    if "nc" not in _CACHE:
        _CACHE["nc"] = build_program()
    nc = _CACHE["nc"]
    cst = make_consts()
    in_maps = []
    for c in range(NCORES):
        m = {}
        m["xin"] = np.ascontiguousarray(np.concatenate(
            [inp["x_sample"][c], inp["x_prompt"][2 * c], inp["x_prompt"][2 * c + 1]], axis=0))
        m["cond"] = np.ascontiguousarray(np.stack([inp["c"][c], inp["c_ctx"]], axis=0))
        m["st_rwkv"] = np.ascontiguousarray(inp["state_rwkv"][c])
        m["st_lru"] = np.ascontiguousarray(inp["state_lru"][c])
        for n_, _s in WEIGHT_SHAPES:
            m[n_] = inp[n_]
        m["cst"] = cst
        in_maps.append(m)
    res = run_bass_kernel_spmd(nc, in_maps, core_ids=list(range(NCORES)))
    R = res.results
    y_sample = np.stack([R[c]["y"][0:2048] for c in range(NCORES)], axis=0)
    y_prompt = np.concatenate([R[c]["y"][2048:2560].reshape(2, 256, D) for c in range(NCORES)], axis=0)
    ns_rwkv = np.concatenate([R[c]["ns_rwkv"] for c in range(NCORES)], axis=0)
    ns_lru = np.concatenate([R[c]["ns_lru"] for c in range(NCORES)], axis=0)
    return (y_prompt.astype(np.float32), y_sample.astype(np.float32), ns_rwkv.astype(np.float32), ns_lru.astype(np.float32))
```

```python
import contextlib
import numpy as np
import concourse.bass as bass
import concourse.mybir as mybir
from concourse.ap import AP
from concourse.bass_utils import run_bass_kernel_spmd

F32 = mybir.dt.float32
BF16 = mybir.dt.bfloat16
ALU = mybir.AluOpType
AF = mybir.ActivationFunctionType
AX = mybir.AxisListType

ENGS = ("pe", "act", "dve", "pool", "sp")
NCORES = 8
T = 2560
NBLK = 5
SEQS = [(0, 2048, 64), (2048, 256, 256), (2304, 256, 256)]
D = 1024
INC = 7424
L = 2


class Buf:
    __slots__ = ("name", "last_write", "reads")

    def __init__(self, name=""):
        self.name = name
        self.last_write = None
        self.reads = []


class Prog:
    def __init__(self, nc, n_dma_sems=(24, 8, 24)):
        self.nc = nc
        self.ops = {e: [] for e in ENGS}
        self.count = {e: 0 for e in ENGS}
        self.waited = {e: {} for e in ENGS}
        self.sems = {}
        self._ctx = []
        for e in ENGS:
            self.sems["E_" + e] = self._sem("E_" + e)
        self.dma_pool = {}
        for q, n in zip(("sp", "act", "pool"), n_dma_sems):
            self.dma_pool[q] = []
            for i in range(n):
                self.sems[f"D_{q}{i}"] = self._sem(f"D_{q}{i}")
                self.dma_pool[q].append([f"D_{q}{i}", 0])
        self.dma_rr = {q: 0 for q in ("sp", "act", "pool")}
        self.n_ops = 0

    def _sem(self, name):
        cm = self.nc.semaphore(name)
        s = cm.__enter__()
        self._ctx.append(cm)
        return s

    def sbuf(self, name, shape, dtype):
        self._uid = getattr(self, "_uid", 0) + 1
        name = f"s{self._uid}_{name}"
        cm = self.nc.sbuf_tensor(name, list(shape), dtype)
        t = cm.__enter__()
        self._ctx.append(cm)
        return t

    def psum(self, name, shape, dtype):
        cm = self.nc.psum_tensor(name, list(shape), dtype)
        t = cm.__enter__()
        self._ctx.append(cm)
        return t

    def _deps(self, eng, reads, writes):
        deps = {}
        own = "E_" + eng

        def add(tok, same_ok):
            if tok is None:
                return
            s, v = tok
            if s == own and not same_ok:
                return
            if deps.get(s, 0) < v:
                deps[s] = v

        for b in reads:
            add(b.last_write, eng != "pe")
        for b in writes:
            add(b.last_write, False)
            for t in b.reads:
                add(t, False)
        waits = []
        w = self.waited[eng]
        for s, v in deps.items():
            if w.get(s, 0) >= v:
                continue
            w[s] = v
            waits.append((s, v))
        return waits

    def _commit(self, tok, reads, writes):
        for b in reads:
            b.reads.append(tok)
            if len(b.reads) > 48:
                latest = {}
                for s, v in b.reads:
                    if latest.get(s, 0) < v:
                        latest[s] = v
                b.reads = list(latest.items())
        for b in writes:
            b.last_write = tok
            b.reads = []

    def op(self, eng, fn, reads=(), writes=()):
        waits = self._deps(eng, reads, writes)
        self.count[eng] += 1
        tok = ("E_" + eng, self.count[eng])
        self.ops[eng].append((waits, fn, tok))
        self._commit(tok, reads, writes)
        self.n_ops += 1
        return tok

    def dma(self, q, out, in_, reads=(), writes=(), **kw):
        pool = self.dma_pool[q]
        i = self.dma_rr[q]
        self.dma_rr[q] = (i + 1) % len(pool)
        ent = pool[i]
        semname = ent[0]
        waits = self._deps(q, reads, writes)
        if ent[1] > 0 and self.waited[q].get(semname, 0) < ent[1]:
            self.waited[q][semname] = ent[1]
            waits.append((semname, ent[1]))
        ent[1] += 16
        tok = (semname, ent[1])

        def fn(e, out=out, in_=in_, kw=kw):
            return e.dma_start(out=out, in_=in_, **kw)
        self.ops[q].append((waits, fn, tok))
        self._commit(tok, reads, writes)
        self.n_ops += 1
        return tok

    def mark(self, name):
        if not hasattr(self, "marks"):
            self.marks = []
        self.marks.append((name, dict(self.count)))

    def barrier(self):
        targets = []
        for e in ENGS:
            if self.count[e] > 0:
                targets.append(("E_" + e, self.count[e]))
        for q in self.dma_pool:
            for name, v in self.dma_pool[q]:
                if v > 0:
                    targets.append((name, v))
        for e in ENGS:
            waits = []
            for s, v in targets:
                if s == "E_" + e:
                    continue
                if self.waited[e].get(s, 0) >= v:
                    continue
                self.waited[e][s] = v
                waits.append((s, v))
            if waits:
                self.ops[e].append((waits, None, None))

    @contextlib.contextmanager
    def scope(self):
        n0 = len(self._ctx)
        yield
        self.barrier()
        while len(self._ctx) > n0:
            cm = self._ctx.pop()
            cm.__exit__(None, None, None)

    def emit(self):
        nc = self.nc
        engmap = {"pe": "tensor", "act": "scalar", "dve": "vector", "pool": "gpsimd", "sp": "sync"}
        with nc.Block() as block:
            for e in ENGS:
                lst = self.ops[e]
                if not lst:
                    continue

                def body(eng, lst=lst):
                    for waits, fn, tok in lst:
                        if fn is None:
                            for s, v in waits:
                                eng.wait_ge(self.sems[s], v)
                            continue
                        for s, v in waits[1:]:
                            eng.wait_ge(self.sems[s], v)
                        ins = fn(eng)
                        if waits:
                            ins._wait_ge(self.sems[waits[0][0]], waits[0][1])
                        ins.then_inc(self.sems[tok[0]], 16 if tok[0].startswith("D_") else 1)
                getattr(block, engmap[e])(body)

    def close(self):
        while self._ctx:
            cm = self._ctx.pop()
            cm.__exit__(None, None, None)


def rev_last(ap):
    dims = [list(d) for d in ap.ap]
    step, cnt = dims[-1]
    return AP(ap.tensor, ap.offset + step * (cnt - 1), dims[:-1] + [[-step, cnt]])


def bcast(ap, axis, n):
    dims = [list(d) for d in ap.ap]
    dims.insert(axis, [0, n])
    return AP(ap.tensor, ap.offset, dims)


WEIGHT_SHAPES = [
    ("mod_w", (L, D, 6 * D)), ("mod_b", (L, 6 * D)),
    ("norm_pre_mix", (L, D)), ("norm_post_mix", (L, D)), ("norm_pre_ffn", (L, D)), ("norm_post_ffn", (L, D)),
    ("w_in", (L, D, INC)), ("b_merge", (L, 2 * D)),
    ("rwkv_mu", (L, 2, 3200)), ("rwkv_w0", (L, 2, D)), ("rwkv_w_up", (L, 2, 64, D)),
    ("rwkv_a0", (L, 2, D)), ("rwkv_a_up", (L, 2, 64, D)), ("rwkv_k_k", (L, D)), ("rwkv_k_a", (L, D)),
    ("rwkv_r_k", (L, 16, 64)), ("rwkv_g_up", (L, 128, D)), ("rwkv_lnx_w", (L, D)), ("rwkv_lnx_b", (L, D)),
    ("lru_conv_w", (L, 4, D)), ("lru_conv_b", (L, D)), ("lru_w_rg", (L, 2, 8, 128, 128)), ("lru_b_rg", (L, 2, D)),
    ("lru_w_ig", (L, 2, 8, 128, 128)), ("lru_b_ig", (L, 2, D)), ("lru_lam", (L, 2, D)),
    ("w_proj_a", (L, D, D)), ("w_proj_b", (L, D, D)), ("w_out", (L, D, D)),
    ("ffn_w1", (1, D, 4096)), ("ffn_w3", (1, D, 4096)), ("ffn_w2", (1, 4096, D)),
    ("moe_router", (1, D, 8)), ("moe_w1", (1, 8, D, 3584)), ("moe_w3", (1, 8, D, 3584)), ("moe_w2", (1, 8, 3584, D)),
]

C_ID = 0
C_US = 128
C_UI = 256
C_LS = 384
C_LI = 512
C_MF = 640
C_MB = 1152
C_ES = 1664
C_LM = C_ES + 8 * 128
C_LMT = C_LM + 7 * 128
NCST32 = C_LM
NCST = C_LMT + 7 * 128


def make_consts():
    c = np.zeros((128, NCST), np.float32)
    p = np.arange(128)[:, None]
    q = np.arange(128)[None, :]
    c[:, C_ID:C_ID + 128] = (p == q)
    c[:, C_US:C_US + 128] = (p < q)
    c[:, C_UI:C_UI + 128] = (p <= q)
    c[:, C_LS:C_LS + 128] = (p > q)
    c[:, C_LI:C_LI + 128] = (p >= q)
    t = np.arange(512)
    c[:, C_MF:C_MF + 512] = (t % 128 != 0)[None, :]
    c[:, C_MB:C_MB + 512] = (t % 128 != 127)[None, :]
    for e in range(8):
        c[e, C_ES + e * 128:C_ES + (e + 1) * 128] = 1.0
    for i in range(7):
        b = 1 << i
        m = ((p // (2 * b)) == (q // (2 * b))) & ((p % (2 * b)) < b) & ((q % (2 * b)) >= b)
        c[:, C_LM + i * 128:C_LM + (i + 1) * 128] = m
        c[:, C_LMT + i * 128:C_LMT + (i + 1) * 128] = m.T
    return c


def build_program(debug=False):
    nc = bass.Bass("TRN2", target_bir_lowering=False)

    def din(name, shape):
        return nc.dram_tensor(name, list(shape), F32, kind="ExternalInput").ap()

    def dout(name, shape):
        return nc.dram_tensor(name, list(shape), F32, kind="ExternalOutput").ap()

    xin = din("xin", [T, D])
    cond = din("cond", [2, D])
    st_rwkv = din("st_rwkv", [L, 2, 16, 64, 64])
    st_lru = din("st_lru", [L, 2, D])
    Wd = {n: din(n, s) for n, s in WEIGHT_SHAPES}
    cstd = din("cst", [128, NCST])
    y_out = dout("y", [T, D])
    ns_rwkv = dout("ns_rwkv", [2, L, 2, 16, 64, 64])
    ns_lru = dout("ns_lru", [2, L, 2, D])
    xs = nc.dram_tensor("xs_scr", [128, 8, T], F32).ap()
    ya_s = nc.dram_tensor("ya_scr", [128, 8, T], BF16).ap()

    P = Prog(nc)
    out_bufs = []

    def ACT(out, in_, func, R, Wr, scale=None, bias=None):
        kw = {}
        if scale is not None:
            kw["scale"] = scale
        if bias is not None:
            kw["bias"] = bias
        P.op("act", lambda e: e.activation(out=out, in_=in_, func=func, **kw), R, Wr)

    def TT(eng, out, in0, in1, op, R, Wr):
        P.op(eng, lambda e: e.tensor_tensor(out, in0, in1, op), R, Wr)

    def TS(eng, out, in0, s1, s2, op0, op1, R, Wr):
        if s2 is None:
            P.op(eng, lambda e: e.tensor_scalar(out, in0, s1, None, op0), R, Wr)
        else:
            P.op(eng, lambda e: e.tensor_scalar(out, in0, s1, s2, op0, op1), R, Wr)

    def STT(eng, out, in0, sc, in1, op0, op1, R, Wr):
        P.op(eng, lambda e: e.scalar_tensor_tensor(out, in0, sc, in1, op0, op1), R, Wr)

    def CP(eng, out, in_, R, Wr):
        if eng == "act":
            P.op("act", lambda e: e.activation(out=out, in_=in_, func=AF.Copy), R, Wr)
        else:
            P.op(eng, lambda e: e.tensor_copy(out, in_), R, Wr)

    def MM(out, lhsT, rhs, start, stop, R, Wr):
        P.op("pe", lambda e: e.matmul(out, lhsT, rhs, start=start, stop=stop), R, Wr)

    def TR(out, in_, ident, R, Wr):
        P.op("pe", lambda e: e.transpose(out, in_, ident), R, Wr)

    def MSET(eng, ap, val, Wr):
        P.op(eng, lambda e: e.memset(ap, val), (), Wr)

    class Tl:
        def __init__(self, name, shape, dtype):
            self.t = P.sbuf(name, shape, dtype)
            self.b = Buf(name)

    banks = [P.psum(f"bank{i}", [128, 512], F32) for i in range(8)]
    bbufs = [Buf(f"bank{i}") for i in range(8)]
    rr = [0]

    def bank():
        i = rr[0]
        rr[0] = (i + 1) % 8
        return banks[i], bbufs[i]

    NE = 1.0 / D

    cst = Tl("cst", [128, NCST32], F32)
    P.dma("sp", cst.t[:, :], cstd[:, 0:NCST32], writes=[cst.b])
    lmk = Tl("lmk", [128, 14 * 128], BF16)
    P.dma("pool", lmk.t[:, :], cstd[:, C_LM:NCST], writes=[lmk.b])
    ident = cst.t[:, C_ID:C_ID + 128]
    ones_bf = Tl("ones_bf", [128, 128], BF16)
    MSET("pool", ones_bf.t[:, :], 1.0, [ones_bf.b])
    bd_bf = Tl("bd_bf", [128, 128], BF16)
    MSET("pool", bd_bf.t[:, :], 0.0, [bd_bf.b])
    MSET("pool", bd_bf.t[0:64, 0:64], 1.0, [bd_bf.b])
    MSET("pool", bd_bf.t[64:128, 64:128], 1.0, [bd_bf.b])
    ident_bf = Tl("ident_bf", [128, 128], BF16)
    CP("dve", ident_bf.t[:, :], ident, [cst.b], [ident_bf.b])

    def load_fm(name, src2d, n):
        cols = src2d.shape[1] // 128
        tl = Tl(name, [128, n, cols], F32)
        for i in range(n):
            P.dma("sp", tl.t[:, i, :], src2d[i].rearrange("(k p) -> p k", p=128), writes=[tl.b],
                  allow_slow_non_contiguous=True)
        return tl

    npm = load_fm("npm", Wd["norm_pre_mix"], L)
    npo = load_fm("npo", Wd["norm_post_mix"], L)
    npf = load_fm("npf", Wd["norm_pre_ffn"], L)
    npg = load_fm("npg", Wd["norm_post_ffn"], L)
    modb = load_fm("modb", Wd["mod_b"], L)
    bmer = load_fm("bmer", Wd["b_merge"], L)
    mu = load_fm("mu", Wd["rwkv_mu"].rearrange("l d c -> (l d) c"), 2 * L)
    w0 = load_fm("w0", Wd["rwkv_w0"].rearrange("l d c -> (l d) c"), 2 * L)
    a0 = load_fm("a0", Wd["rwkv_a0"].rearrange("l d c -> (l d) c"), 2 * L)
    kk_ = load_fm("kk_", Wd["rwkv_k_k"], L)
    ka_ = load_fm("ka_", Wd["rwkv_k_a"], L)
    rk_ = load_fm("rk_", Wd["rwkv_r_k"].rearrange("l h c -> l (h c)"), L)
    lnw = load_fm("lnw", Wd["rwkv_lnx_w"], L)
    lnb = load_fm("lnb", Wd["rwkv_lnx_b"], L)
    cw = load_fm("cw", Wd["lru_conv_w"].rearrange("l j c -> (l j) c"), 4 * L)
    cb = load_fm("cb", Wd["lru_conv_b"], L)
    brg = load_fm("brg", Wd["lru_b_rg"].rearrange("l d c -> (l d) c"), 2 * L)
    big = load_fm("big", Wd["lru_b_ig"].rearrange("l d c -> (l d) c"), 2 * L)
    lam = load_fm("lam", Wd["lru_lam"].rearrange("l d c -> (l d) c"), 2 * L)
    h0l = load_fm("h0l", st_lru.rearrange("l d c -> (l d) c"), 2 * L)

    omu = Tl("omu", [128, 2 * L, 25], F32)
    TS("dve", omu.t[:, :, :], mu.t[:, :, :], -1.0, 1.0, ALU.mult, ALU.add, [mu.b], [omu.b])
    omka = Tl("omka", [128, L, 8], F32)
    TS("dve", omka.t[:, :, :], ka_.t[:, :, :], -1.0, 1.0, ALU.mult, ALU.add, [ka_.b], [omka.b])
    c8 = Tl("c8", [128, 2 * L, 8], F32)
    ACT(c8.t[:, :, :], lam.t[:, :, :], AF.Exp, [lam.b], [c8.b], scale=-1.0)
    ACT(c8.t[:, :, :], c8.t[:, :, :], AF.Ln, [c8.b], [c8.b], bias=1.0)
    TS("dve", c8.t[:, :, :], c8.t[:, :, :], -8.0, None, ALU.mult, None, [c8.b], [c8.b])

    A1 = Tl("A1", [128, L, 2, 8], F32)
    B1 = Tl("B1", [128, L, 2, 8], F32)
    G1 = Tl("G1", [128, L, 2, 8], F32)
    A2 = Tl("A2", [128, L, 2, 8], F32)
    B2 = Tl("B2", [128, L, 2, 8], F32)
    G2 = Tl("G2", [128, L, 2, 8], F32)
    with P.scope():
        condT = Tl("condT", [128, 8, 2], F32)
        for g in range(2):
            P.dma("sp", condT.t[:, :, g], cond[g].rearrange("(k p) -> p k", p=128), writes=[condT.b],
                  allow_slow_non_contiguous=True)
        scond = Tl("scond", [128, 8, 2], F32)
        ACT(scond.t[:, :, :], condT.t[:, :, :], AF.Silu, [condT.b], [scond.b])
        modv = Tl("modv", [128, L, 48, 2], F32)
        mwt = [Tl(f"mwt{i}", [128, 8, 512], F32) for i in range(2)]
        for l in range(L):
            pb, pbb = bank()
            for cbk in range(12):
                wt = mwt[cbk % 2]
                src = Wd["mod_w"][l].rearrange("(k p) c -> p k c", p=128)[:, :, cbk * 512:(cbk + 1) * 512]
                P.dma("sp", wt.t[:, :, :], src, writes=[wt.b])
                for jj in range(4):
                    col = (cbk * 4 + jj) * 2
                    for k in range(8):
                        MM(pb[:, col:col + 2], wt.t[:, k, jj * 128:(jj + 1) * 128], scond.t[:, k, :],
                           k == 0, k == 7, [wt.b, scond.b], [pbb])
            for g in range(2):
                src = pb[:, 0:96].rearrange("p (c g) -> p c g", g=2)[:, :, g]
                TT("dve", modv.t[:, l, :, g], src, modb.t[:, l, :], ALU.add, [pbb, modb.b], [modv.b])
            for g in range(2):
                mv = modv.t[:, l, :, g]
                STT("dve", A1.t[:, l, g, :], mv[:, 8:16], 1.0, npm.t[:, l, :], ALU.add, ALU.mult, [modv.b, npm.b], [A1.b])
                CP("dve", B1.t[:, l, g, :], mv[:, 0:8], [modv.b], [B1.b])
                TT("dve", G1.t[:, l, g, :], mv[:, 16:24], npo.t[:, l, :], ALU.mult, [modv.b, npo.b], [G1.b])
                STT("dve", A2.t[:, l, g, :], mv[:, 32:40], 1.0, npf.t[:, l, :], ALU.add, ALU.mult, [modv.b, npf.b], [A2.b])
                CP("dve", B2.t[:, l, g, :], mv[:, 24:32], [modv.b], [B2.b])
                TT("dve", G2.t[:, l, g, :], mv[:, 40:48], npg.t[:, l, :], ALU.mult, [modv.b, npg.b], [G2.b])

    xs_b = [Buf(f"xs{i}") for i in range(NBLK)]
    with P.scope():
        xrow = [Tl(f"xrow{i}", [128, D], F32) for i in range(2)]
        xfm = [Tl(f"xfm{i}", [128, 8, 512], F32) for i in range(2)]
        for tb in range(NBLK):
            xf = xfm[tb % 2]
            for tt in range(4):
                xr_ = xrow[(tb * 4 + tt) % 2]
                r0 = tb * 512 + tt * 128
                P.dma("sp", xr_.t[:, :], xin[r0:r0 + 128, :], writes=[xr_.b])
                for half in range(2):
                    pb, pbb = bank()
                    for kk2 in range(4):
                        k = half * 4 + kk2
                        TR(pb[:, kk2 * 128:(kk2 + 1) * 128], xr_.t[:, k * 128:(k + 1) * 128], ident, [xr_.b, cst.b], [pbb])
                    dst = xf.t[:, half * 4:(half + 1) * 4, tt * 128:(tt + 1) * 128]
                    src = pb[:, :].rearrange("p (k t) -> p k t", t=128)
                    if half == 0:
                        CP("act", dst, src, [pbb], [xf.b])
                    else:
                        CP("dve", dst, src, [pbb], [xf.b])
            P.dma("sp", xs[:, :, tb * 512:(tb + 1) * 512], xf.t[:, :, :], reads=[xf.b], writes=[xs_b[tb]])

    def blk_group(t0):
        return 0 if t0 < 2048 else 1

    def SCAN(out, d0, d1, init, R, Wr):
        P.op("dve", lambda e: e.tensor_tensor_scan(out, d0, d1, init, ALU.mult, ALU.add), R, Wr)

    def RSUM(out, in_, R, Wr):
        P.op("dve", lambda e: e.reduce_sum(out, in_, AX.X), R, Wr)

    def RMAX(out, in_, R, Wr):
        P.op("dve", lambda e: e.reduce_max(out, in_, AX.X), R, Wr)

    def hbuf(Hb, t0):
        return Hb[t0 // 512] if isinstance(Hb, list) else Hb

    def norm_mod(l, A, B, H, Hb, blocks, hoff, tmp, hf32=None, cbk=None):
        xbt, sq_t, rstd_t, xn_t = tmp
        for bi, (t0, n) in enumerate(blocks):
            g = blk_group(t0)
            xb = xbt[bi % 2]
            P.dma("sp", xb.t[:, :, 0:n], xs[:, :, t0:t0 + n], reads=[xs_b[t0 // 512]], writes=[xb.b])
            ACT(sq_t.t[:, :, 0:n], xb.t[:, :, 0:n], AF.Square, [xb.b], [sq_t.b])
            pb, pbb = bank()
            for k in range(8):
                MM(pb[:, 0:n], ones_bf.t[:, :], sq_t.t[:, k, 0:n], k == 0, k == 7, [ones_bf.b, sq_t.b], [pbb])
            TS("dve", rstd_t.t[:, 0:n], pb[:, 0:n], NE, 1e-6, ALU.mult, ALU.add, [pbb], [rstd_t.b])
            ACT(rstd_t.t[:, 0:n], rstd_t.t[:, 0:n], AF.Ln, [rstd_t.b], [rstd_t.b])
            ACT(rstd_t.t[:, 0:n], rstd_t.t[:, 0:n], AF.Exp, [rstd_t.b], [rstd_t.b], scale=-0.5)
            TT("dve", xn_t.t[:, :, 0:n], xb.t[:, :, 0:n], bcast(rstd_t.t[:, 0:n], 1, 8), ALU.mult,
               [xb.b, rstd_t.b], [xn_t.b])
            for k in range(8):
                dst = H.t[:, k, t0 - hoff:t0 - hoff + n]
                ACT(dst, xn_t.t[:, k, 0:n], AF.Identity, [xn_t.b, A.b, B.b], [hbuf(Hb, t0)],
                    scale=A.t[:, l, g, k:k + 1], bias=B.t[:, l, g, k:k + 1])
                if hf32 is not None:
                    ACT(hf32.t[:, k, 0:n], xn_t.t[:, k, 0:n], AF.Identity, [xn_t.b, A.b, B.b], [hf32.b],
                        scale=A.t[:, l, g, k:k + 1], bias=B.t[:, l, g, k:k + 1])
            if cbk is not None:
                cbk(t0, n)

    def norm_tmps():
        return ([Tl(f"xb{i}", [128, 8, 512], F32) for i in range(2)], Tl("sq", [128, 8, 512], BF16),
                Tl("rstd", [128, 512], F32), Tl("xn", [128, 8, 512], F32))

    def win_cols(l, c0, n=128):
        return Wd["w_in"][l].rearrange("(k p) c -> p k c", p=128)[:, :, c0:c0 + n]

    def proj(wt3, wb, H, Hb, t0, n, hoff=0):
        pb, pbb = bank()
        for k in range(8):
            MM(pb[:, 0:n], wt3[:, k, :], H.t[:, k, t0 - hoff:t0 - hoff + n], k == 0, k == 7, [wb, hbuf(Hb, t0)], [pbb])
        return pb, pbb

    def rowview(ap2d, t0):
        w = 64 if t0 < 2048 else 256
        return ap2d.rearrange("p (r w) -> p r w", w=w)

    def mix(pb, pbb, dst, mu_ap, omu_ap, d, t0, n):
        ACT(dst.t[:, 0:n], pb[:, 0:n], AF.Identity, [pbb, mu.b, omu.b], [dst.b], scale=omu_ap)
        dv = rowview(dst.t[:, 0:n], t0)
        sv = rowview(pb[:, 0:n], t0)
        if d == 0:
            STT("dve", dv[:, :, 1:], sv[:, :, :-1], mu_ap, dv[:, :, 1:], ALU.mult, ALU.add, [pbb, dst.b, mu.b], [dst.b])
        else:
            STT("dve", dv[:, :, :-1], sv[:, :, 1:], mu_ap, dv[:, :, :-1], ALU.mult, ALU.add, [pbb, dst.b, mu.b], [dst.b])

    BLK5 = [(i * 512, 512) for i in range(NBLK)]
    lruo = Tl("lruo", [128, 2, L, 2, 8], F32)

    def residual_update(l, O, Ob, G, t0, n, final, tmp):
        xb, sq_t, rstd_t, on_t, yrow = tmp
        g = blk_group(t0)
        P.dma("sp", xb.t[:, :, 0:n], xs[:, :, t0:t0 + n], reads=[xs_b[t0 // 512]], writes=[xb.b])
        ACT(sq_t.t[:, :, 0:n], O, AF.Square, [Ob], [sq_t.b])
        pb, pbb = bank()
        for k in range(8):
            MM(pb[:, 0:n], ones_bf.t[:, :], sq_t.t[:, k, 0:n], k == 0, k == 7, [ones_bf.b, sq_t.b], [pbb])
        TS("dve", rstd_t.t[:, 0:n], pb[:, 0:n], NE, 1e-6, ALU.mult, ALU.add, [pbb], [rstd_t.b])
        ACT(rstd_t.t[:, 0:n], rstd_t.t[:, 0:n], AF.Ln, [rstd_t.b], [rstd_t.b])
        ACT(rstd_t.t[:, 0:n], rstd_t.t[:, 0:n], AF.Exp, [rstd_t.b], [rstd_t.b], scale=-0.5)
        TT("dve", on_t.t[:, :, 0:n], O, bcast(rstd_t.t[:, 0:n], 1, 8), ALU.mult, [Ob, rstd_t.b], [on_t.b])
        for k in range(8):
            STT("dve", xb.t[:, k, 0:n], on_t.t[:, k, 0:n], G.t[:, l, g, k:k + 1], xb.t[:, k, 0:n],
                ALU.mult, ALU.add, [on_t.b, G.b, xb.b], [xb.b])
        if not final:
            P.dma("sp", xs[:, :, t0:t0 + n], xb.t[:, :, 0:n], reads=[xb.b], writes=[xs_b[t0 // 512]])
        else:
            for tt in range(n // 128):
                yr = yrow[tt % len(yrow)]
                for half in range(2):
                    pt, ptb = bank()
                    for k4 in range(4):
                        k = half * 4 + k4
                        TR(pt[:, k4 * 128:(k4 + 1) * 128], xb.t[:, k, tt * 128:(tt + 1) * 128], ident, [xb.b, cst.b], [ptb])
                    CP("act" if half else "dve", yr.t[:, half * 512:(half + 1) * 512], pt[:, :], [ptb], [yr.b])
                ob = Buf("yout")
                out_bufs.append(ob)
                r0 = t0 + tt * 128
                P.dma("sp", y_out[r0:r0 + 128, :], yr.t[:, :], reads=[yr.b], writes=[ob])

    def res_tmps(final):
        return (Tl("rxb", [128, 8, 512], F32), Tl("rsq", [128, 8, 512], BF16), Tl("rrs", [128, 512], F32),
                Tl("ron", [128, 8, 512], F32), [Tl(f"yrow{i}", [128, D], F32) for i in range(1)] if final else None)

    def rwkv_phase(l, H, Hb, YB):
        LORA = Tl("LORA", [128, 2, T], BF16)
        SG = Tl("SG", [128, T], BF16)
        LW = Tl("LW", [128, 2, D], BF16)
        GUP = Tl("GUP", [128, D], BF16)
        for d in range(2):
            P.dma("pool", LW.t[0:64, d, :], Wd["rwkv_w_up"][l, d], writes=[LW.b])
            P.dma("pool", LW.t[64:128, d, :], Wd["rwkv_a_up"][l, d], writes=[LW.b])
        P.dma("pool", GUP.t[:, :], Wd["rwkv_g_up"][l], writes=[GUP.b])
        wch = [Tl(f"wch{i}", [128, 8, 128], BF16) for i in range(4)]
        wrr = [0]

        def next_w(c0):
            tl = wch[wrr[0] % 4]
            wrr[0] += 1
            P.dma("pool", tl.t[:, :, :], win_cols(l, c0), writes=[tl.b])
            return tl

        tn = ["xr", "xk", "xv", "lw", "aa", "cum", "e1", "e2", "kq", "kk", "kf", "bb"]
        tm = {n_: Tl("t_" + n_, [128, 512], F32) for n_ in tn}
        sqb = Tl("sqb", [128, 512], BF16)
        rkb = Tl("rkb", [128, 512], BF16)

        ybf = YB.t[:, :, :].rearrange("p a t -> p (a t)")
        ybuf = Buf("ybalias")

        class V:
            def __init__(self, ap):
                self.t = ap
                self.b = Buf()
        QR = V(ybf[:, 0:5120].rearrange("p (c x) -> p c x", x=256))
        KTz = V(ybf[:, 5120:10240].rearrange("p (h t) -> p h t", h=2))
        BTz = V(ybf[:, 10240:15360].rearrange("p (h t) -> p h t", h=2))
        VT = V(ybf[:, 15360:17920].rearrange("p (c x) -> p c x", x=128))
        KH = V(ybf[:, 17920:20480].rearrange("p (c x) -> p c x", x=128))
        BH = Tl("BH", [128, 20, 128], BF16)
        WEND = Tl("WEND", [128, 20], F32)
        MSET("pool", KTz.t[:, :, :], 0.0, [KTz.b])
        MSET("pool", BTz.t[:, :, :], 0.0, [BTz.b])

        wt = next_w(3072)
        x0 = tm["xr"]
        for (t0, n) in BLK5:
            pb, pbb = proj(wt.t, wt.b, H, Hb, t0, n)
            for d in range(2):
                mi = 2 * l + d
                mix(pb, pbb, x0, mu.t[:, mi, 24:25], omu.t[:, mi, 24:25], d, t0, n)
                ACT(LORA.t[0:64, d, t0:t0 + n], x0.t[0:64, 0:n], AF.Tanh, [x0.b], [LORA.b])
                CP("dve", LORA.t[64:128, d, t0:t0 + n], x0.t[64:128, 0:n], [x0.b], [LORA.b])
        wt = next_w(3200)
        for (t0, n) in BLK5:
            pb, pbb = proj(wt.t, wt.b, H, Hb, t0, n)
            ACT(SG.t[:, t0:t0 + n], pb[:, 0:n], AF.Sigmoid, [pbb], [SG.b])

        BON = Tl("BON", [128, T], F32)
        YACC = Tl("YACC", [128, 20, 128], F32)
        YAst = Tl("YAst", [128, T], BF16)
        GSL = 4

        class Slot:
            def __init__(self, nm):
                self.NPt = Tl("NPt" + nm, [128, 2, 256], BF16)
                self.MOt = Tl("MOt" + nm, [128, 2, 256], BF16)
                self.NTt = Tl("NTt" + nm, [128, 2, 128], BF16)
                self.Tp = [Tl(f"T{i}" + nm, [128, 2, 128], BF16) for i in range(2)]
                self.TTp = [Tl(f"TT{i}" + nm, [128, 2, 128], BF16) for i in range(2)]
                self.Xa = Tl("Xa" + nm, [128, 2, 128], BF16)
                self.Xb = Tl("Xb" + nm, [128, 2, 128], BF16)
                self.Tfin = None
        slots = [Slot(f"{j}") for j in range(GSL)]
        Btb = Tl("Btb", [128, 128], BF16)
        Utb = Tl("Utb", [128, 128], BF16)
        S32 = Tl("S32", [128, 64], F32)
        SBz = Tl("SBz", [128, 2, 64], BF16)
        BD = Tl("BD", [128, 128], F32)
        OUTS = Tl("OUTS", [128, 64], F32)
        MSET("pool", SBz.t[:, :, :], 0.0, [SBz.b])
        MSET("pool", BD.t[:, :], 0.0, [BD.b])
        st1 = Tl("st1", [128, 8], F32)
        st2 = Tl("st2", [128, 8], F32)

        v3 = lambda ap, x: ap.rearrange("p (h x) -> p h x", x=x)

        def prep_group(chunks, d):
            mNP = cst.t[:, C_US:C_US + 256] if d == 0 else cst.t[:, C_LS:C_LS + 256]
            mNT = cst.t[:, C_LS:C_LS + 128] if d == 0 else cst.t[:, C_US:C_US + 128]

            def lm(i, tr):
                nat = (d == 0) != tr
                base = (0 if nat else 7 * 128) + i * 128
                return lmk.t[:, base:base + 128]
            cs = list(zip(chunks, slots))
            for c, sl in cs:
                cc = slice(c * 128, (c + 1) * 128)
                pNP, bNP = bank()
                for h in range(2):
                    MM(pNP[:, h * 256:(h + 1) * 256], BTz.t[:, h, cc], QR.t[:, c, :], True, True, [BTz.b, QR.b], [bNP])
                pMO, bMO = bank()
                for h in range(2):
                    MM(pMO[:, h * 256:(h + 1) * 256], KTz.t[:, h, cc], QR.t[:, c, :], True, True, [KTz.b, QR.b], [bMO])
                pNT, bNT = bank()
                for h in range(2):
                    MM(pNT[:, h * 128:(h + 1) * 128], QR.t[:, c, 0:128], BTz.t[:, h, cc], True, True, [BTz.b, QR.b], [bNT])
                TT("dve", sl.NPt.t[:, :, :], v3(pNP[:, 0:512], 256), bcast(mNP, 1, 2), ALU.mult, [bNP, cst.b], [sl.NPt.b])
                TT("dve", sl.NTt.t[:, :, :], v3(pNT[:, 0:256], 128), bcast(mNT, 1, 2), ALU.mult, [bNT, cst.b], [sl.NTt.b])
                TT("dve", sl.MOt.t[:, :, :], v3(pMO[:, 0:512], 256), bcast(mNP, 1, 2), ALU.mult, [bMO, cst.b], [sl.MOt.b])
            for c, sl in cs:
                Tc, TTc = sl.Tp[0], sl.TTp[0]
                TT("pool", sl.Xa.t[:, :, :], sl.NPt.t[:, :, 0:128], bcast(lm(0, False), 1, 2), ALU.mult, [sl.NPt.b, lmk.b], [sl.Xa.b])
                TT("pool", Tc.t[:, :, :], sl.Xa.t[:, :, :], bcast(ident_bf.t[:, :], 1, 2), ALU.add, [sl.Xa.b, ident_bf.b], [Tc.b])
                TT("pool", sl.Xb.t[:, :, :], sl.NTt.t[:, :, :], bcast(lm(0, True), 1, 2), ALU.mult, [sl.NTt.b, lmk.b], [sl.Xb.b])
                TT("pool", TTc.t[:, :, :], sl.Xb.t[:, :, :], bcast(ident_bf.t[:, :], 1, 2), ALU.add, [sl.Xb.b, ident_bf.b], [TTc.b])
            cur = 0
            for lev in range(1, 7):
                pend = []
                for c, sl in cs:
                    Tc, TTc = sl.Tp[cur], sl.TTp[cur]
                    pX, bX = bank()
                    for h in range(2):
                        MM(pX[:, h * 128:(h + 1) * 128], sl.NTt.t[:, h, :], Tc.t[:, h, :], True, True, [sl.NTt.b, Tc.b], [bX])
                    for h in range(2):
                        MM(pX[:, 256 + h * 128:256 + (h + 1) * 128], sl.NPt.t[:, h, 0:128], TTc.t[:, h, :], True, True,
                           [sl.NPt.b, TTc.b], [bX])
                    pend.append((pX, bX))
                for (c, sl), (pX, bX) in zip(cs, pend):
                    TT("dve", sl.Xa.t[:, :, :], v3(pX[:, 0:256], 128), bcast(lm(lev, False), 1, 2), ALU.mult, [bX, lmk.b], [sl.Xa.b])
                    TT("dve", sl.Xb.t[:, :, :], v3(pX[:, 256:512], 128), bcast(lm(lev, True), 1, 2), ALU.mult, [bX, lmk.b], [sl.Xb.b])
                pend = []
                for c, sl in cs:
                    Tc, TTc = sl.Tp[cur], sl.TTp[cur]
                    pY, bY_ = bank()
                    for h in range(2):
                        hc2 = slice(h * 128, (h + 1) * 128)
                        MM(pY[:, hc2], TTc.t[:, h, :], sl.Xa.t[:, h, :], True, False, [TTc.b, sl.Xa.b], [bY_])
                        MM(pY[:, hc2], ident_bf.t[:, :], Tc.t[:, h, :], False, True, [ident_bf.b, Tc.b], [bY_])
                    for h in range(2):
                        hc2 = slice(256 + h * 128, 256 + (h + 1) * 128)
                        MM(pY[:, hc2], Tc.t[:, h, :], sl.Xb.t[:, h, :], True, False, [Tc.b, sl.Xb.b], [bY_])
                        MM(pY[:, hc2], ident_bf.t[:, :], TTc.t[:, h, :], False, True, [ident_bf.b, TTc.b], [bY_])
                    pend.append((pY, bY_))
                for (c, sl), (pY, bY_) in zip(cs, pend):
                    Tn, TTn = sl.Tp[1 - cur], sl.TTp[1 - cur]
                    CP("act", Tn.t[:, :, :], v3(pY[:, 0:256], 128), [bY_], [Tn.b])
                    CP("act", TTn.t[:, :, :], v3(pY[:, 256:512], 128), [bY_], [TTn.b])
                cur = 1 - cur
            for c, sl in cs:
                sl.Tfin = sl.Tp[cur]

        def seq_of(c):
            for si, (s0, slen, _) in enumerate(SEQS):
                if s0 // 128 <= c < (s0 + slen) // 128:
                    return si, s0 // 128, slen // 128
            raise ValueError

        def chain_one(c, sl, d, hp):
            si, cb, nch = seq_of(c)
            first = (c == cb) if d == 0 else (c == cb + nch - 1)
            lastc = (c == cb + nch - 1) if d == 0 else (c == cb)
            if first:
                if si == 0:
                    for h in range(2):
                        hs = slice(h * 64, (h + 1) * 64)
                        P.dma("sp", BD.t[hs, hs], st_rwkv[l, d, 2 * hp + h], writes=[BD.b])
                    pt, ptb = bank()
                    TR(pt[:, 0:128], BD.t[:, :], ident, [BD.b, cst.b], [ptb])
                    for h in range(2):
                        hs = slice(h * 64, (h + 1) * 64)
                        CP("dve", S32.t[hs, :], pt[hs, hs], [ptb], [S32.b])
                else:
                    MSET("pool", S32.t[:, :], 0.0, [S32.b])
                CP("act", SBz.t[0:64, 0, :], S32.t[0:64, :], [S32.b], [SBz.b])
                CP("act", SBz.t[64:128, 1, :], S32.t[64:128, :], [S32.b], [SBz.b])
            NPt, MOt, Tfin = sl.NPt, sl.MOt, sl.Tfin
            pBt, bBt = bank()
            for h in range(2):
                hc = slice(h * 64, (h + 1) * 64)
                MM(pBt[:, hc], QR.t[:, c, 0:128], SBz.t[:, h, :], True, False, [QR.b, SBz.b], [bBt])
                MM(pBt[:, hc], MOt.t[:, h, 0:128], VT.t[:, c, hc], False, True, [MOt.b, VT.b], [bBt])
            CP("act", Btb.t[:, :], pBt[:, 0:128], [bBt], [Btb.b])
            pU, bU = bank()
            for h in range(2):
                hc = slice(h * 64, (h + 1) * 64)
                MM(pU[:, hc], Tfin.t[:, h, :], Btb.t[:, hc], True, True, [Tfin.b, Btb.b], [bU])
            CP("dve", Utb.t[:, :], pU[:, 0:128], [bU], [Utb.b])
            pY, bY = bank()
            for h in range(2):
                hc = slice(h * 64, (h + 1) * 64)
                MM(pY[:, hc], QR.t[:, c, 128:256], SBz.t[:, h, :], True, False, [QR.b, SBz.b], [bY])
                MM(pY[:, hc], NPt.t[:, h, 128:256], Utb.t[:, hc], False, False, [NPt.b, Utb.b], [bY])
                MM(pY[:, hc], MOt.t[:, h, 128:256], VT.t[:, c, hc], False, True, [MOt.b, VT.b], [bY])
            if d == 0:
                CP("act", YACC.t[:, c, :], pY[:, 0:128], [bY], [YACC.b])
            else:
                TT("dve", YACC.t[:, c, :], pY[:, 0:128], YACC.t[:, c, :], ALU.add, [bY, YACC.b], [YACC.b])
            pS, bS = bank()
            for h in range(2):
                hc = slice(h * 64, (h + 1) * 64)
                MM(pS[:, hc], BH.t[:, c, :], Utb.t[:, hc], True, False, [BH.b, Utb.b], [bS])
                MM(pS[:, hc], KH.t[:, c, :], VT.t[:, c, hc], False, True, [KH.b, VT.b], [bS])
            for h in range(2):
                hs = slice(h * 64, (h + 1) * 64)
                STT("dve", S32.t[hs, :], S32.t[hs, :], WEND.t[hs, c:c + 1], pS[hs, h * 64:(h + 1) * 64], ALU.mult, ALU.add,
                    [S32.b, WEND.b, bS], [S32.b])
            CP("act", SBz.t[0:64, 0, :], S32.t[0:64, :], [S32.b], [SBz.b])
            CP("act", SBz.t[64:128, 1, :], S32.t[64:128, :], [S32.b], [SBz.b])
            if lastc and si > 0:
                for h in range(2):
                    hs = slice(h * 64, (h + 1) * 64)
                    CP("dve", BD.t[hs, hs], S32.t[hs, :], [S32.b], [BD.b])
                pt, ptb = bank()
                TR(pt[:, 0:128], BD.t[:, :], ident, [BD.b, cst.b], [ptb])
                for h in range(2):
                    hs = slice(h * 64, (h + 1) * 64)
                    CP("dve", OUTS.t[hs, :], pt[hs, hs], [ptb], [OUTS.b])
                ob = Buf("nsr")
                out_bufs.append(ob)
                P.dma("sp", ns_rwkv[si - 1, l, d, 2 * hp:2 * hp + 2].rearrange("h i j -> (h i) j"), OUTS.t[:, :],
                      reads=[OUTS.b], writes=[ob])

        for hp in range(8):
            hcols = slice(hp * 128, (hp + 1) * 128)
            for d in range(2):
                mi = 2 * l + d
                wr = next_w(hp * 128)
                wk = next_w(1024 + hp * 128)
                wv = next_w(2048 + hp * 128)
                for (t0, n) in BLK5:
                    c4 = t0 // 128
                    bl = slice(t0, t0 + n)
                    xr, xk, xv, lw, aa, cum, e1, e2, kq, kkt, kf, bb = [tm[x_] for x_ in tn]
                    pr, prb = proj(wr.t, wr.b, H, Hb, t0, n)
                    mix(pr, prb, xr, mu.t[:, mi, hp:hp + 1], omu.t[:, mi, hp:hp + 1], d, t0, n)
                    pk, pkb = proj(wk.t, wk.b, H, Hb, t0, n)
                    mix(pk, pkb, xk, mu.t[:, mi, 8 + hp:9 + hp], omu.t[:, mi, 8 + hp:9 + hp], d, t0, n)
                    pv, pvb = proj(wv.t, wv.b, H, Hb, t0, n)
                    mix(pv, pvb, xv, mu.t[:, mi, 16 + hp:17 + hp], omu.t[:, mi, 16 + hp:17 + hp], d, t0, n)
                    pw, pwb = bank()
                    MM(pw[:, 0:n], LW.t[0:64, d, hcols], LORA.t[0:64, d, bl], True, True, [LW.b, LORA.b], [pwb])
                    pa, pab = bank()
                    MM(pa[:, 0:n], LW.t[64:128, d, hcols], LORA.t[64:128, d, bl], True, True, [LW.b, LORA.b], [pab])
                    ACT(lw.t[:, :], pw[:, 0:n], AF.Sigmoid, [pwb, w0.b], [lw.b], bias=w0.t[:, mi, hp:hp + 1])
                    ACT(aa.t[:, :], pa[:, 0:n], AF.Sigmoid, [pab, a0.b], [aa.b], bias=a0.t[:, mi, hp:hp + 1])
                    TS("pool", lw.t[:, :], lw.t[:, :], -0.6065306597126334, None, ALU.mult, None, [lw.b], [lw.b])
                    if d == 0:
                        SCAN(cum.t[:, :], cst.t[:, C_MF:C_MF + 512], lw.t[:, :], 0.0, [cst.b, lw.b], [cum.b])
                        cend = cum.t[:, :].rearrange("p (c x) -> p c x", x=128)[:, :, 127]
                    else:
                        SCAN(rev_last(cum.t[:, :]), rev_last(cst.t[:, C_MB:C_MB + 512]), rev_last(lw.t[:, :]), 0.0,
                             [cst.b, lw.b], [cum.b])
                        cend = cum.t[:, :].rearrange("p (c x) -> p c x", x=128)[:, :, 0]
                    c3 = lambda ap: ap.rearrange("p (c x) -> p c x", x=128)
                    ACT(WEND.t[:, c4:c4 + 4], cend, AF.Exp, [cum.b], [WEND.b])
                    TS("pool", kq.t[:, :], xk.t[:, :], kk_.t[:, l, hp:hp + 1], None, ALU.mult, None, [xk.b, kk_.b], [kq.b])
                    ACT(sqb.t[:, :], kq.t[:, :], AF.Square, [kq.b], [sqb.b])
                    pn, pnb = bank()
                    MM(pn[:, 0:n], bd_bf.t[:, :], sqb.t[:, :], True, True, [bd_bf.b, sqb.b], [pnb])
                    TS("dve", kkt.t[:, :], pn[:, 0:n], 1e-24, None, ALU.add, None, [pnb], [kkt.b])
                    ACT(kkt.t[:, :], kkt.t[:, :], AF.Ln, [kkt.b], [kkt.b])
                    ACT(kkt.t[:, :], kkt.t[:, :], AF.Exp, [kkt.b], [kkt.b], scale=-0.5)
                    TT("dve", kkt.t[:, :], kkt.t[:, :], kq.t[:, :], ALU.mult, [kkt.b, kq.b], [kkt.b])
                    TS("dve", kf.t[:, :], aa.t[:, :], ka_.t[:, l, hp:hp + 1], omka.t[:, l, hp:hp + 1], ALU.mult, ALU.add,
                       [aa.b, ka_.b, omka.b], [kf.b])
                    TT("pool", kf.t[:, :], kf.t[:, :], xk.t[:, :], ALU.mult, [kf.b, xk.b], [kf.b])
                    TT("pool", bb.t[:, :], kkt.t[:, :], aa.t[:, :], ALU.mult, [kkt.b, aa.b], [bb.b])
                    STT("dve", rkb.t[:, :], xr.t[:, :], rk_.t[:, l, hp:hp + 1], kf.t[:, :], ALU.mult, ALU.mult,
                        [xr.b, rk_.b, kf.b], [rkb.b])
                    pbn, pbnb = bank()
                    MM(pbn[:, 0:n], bd_bf.t[:, :], rkb.t[:, :], True, True, [bd_bf.b, rkb.b], [pbnb])
                    if d == 0:
                        TT("dve", BON.t[:, bl], pbn[:, 0:n], xv.t[:, :], ALU.mult, [pbnb, xv.b], [BON.b])
                    else:
                        TT("dve", kq.t[:, :], pbn[:, 0:n], xv.t[:, :], ALU.mult, [pbnb, xv.b], [kq.b])
                        TT("pool", BON.t[:, bl], BON.t[:, bl], kq.t[:, :], ALU.add, [BON.b, kq.b], [BON.b])
                    pt, ptb = bank()
                    for c_ in range(4):
                        TR(pt[:, c_ * 128:(c_ + 1) * 128], xv.t[:, c_ * 128:(c_ + 1) * 128], ident, [xv.b, cst.b], [ptb])
                    CP("act", VT.t[:, c4:c4 + 4, :], c3(pt[:, 0:512]), [ptb], [VT.b])
                    ACT(e1.t[:, :], cum.t[:, :], AF.Exp, [cum.b], [e1.b])
                    TT("dve", QR.t[:, c4:c4 + 4, 128:256], c3(xr.t[:, :]), c3(e1.t[:, :]), ALU.mult, [xr.b, e1.b], [QR.b])
                    TT("pool", e2.t[:, :], cum.t[:, :], lw.t[:, :], ALU.subtract, [cum.b, lw.b], [e2.b])
                    ACT(e2.t[:, :], e2.t[:, :], AF.Exp, [e2.b], [e2.b])
                    TT("dve", QR.t[:, c4:c4 + 4, 0:128], c3(kkt.t[:, :]), c3(e2.t[:, :]), ALU.mult, [kkt.b, e2.b], [QR.b])
                    ACT(e1.t[:, :], cum.t[:, :], AF.Exp, [cum.b], [e1.b], scale=-1.0)
                    for h in range(2):
                        hs = slice(h * 64, (h + 1) * 64)
                        TT("pool", KTz.t[hs, h, bl], kf.t[hs, :], e1.t[hs, :], ALU.mult, [kf.b, e1.b], [KTz.b])
                        STT("dve", BTz.t[hs, h, bl], bb.t[hs, :], -1.0, e1.t[hs, :], ALU.mult, ALU.mult, [bb.b, e1.b], [BTz.b])
                    TT("dve", c3(e2.t[:, :]), bcast(cend, 2, 128), c3(cum.t[:, :]), ALU.subtract, [cum.b], [e2.b])
                    ACT(e2.t[:, :], e2.t[:, :], AF.Exp, [e2.b], [e2.b])
                    TT("pool", kf.t[:, :], kf.t[:, :], e2.t[:, :], ALU.mult, [kf.b, e2.b], [kf.b])
                    STT("dve", bb.t[:, :], bb.t[:, :], -1.0, e2.t[:, :], ALU.mult, ALU.mult, [bb.b, e2.b], [bb.b])
                    pt, ptb = bank()
                    for c_ in range(4):
                        TR(pt[:, c_ * 128:(c_ + 1) * 128], kf.t[:, c_ * 128:(c_ + 1) * 128], ident, [kf.b, cst.b], [ptb])
                    CP("act", KH.t[:, c4:c4 + 4, :], c3(pt[:, 0:512]), [ptb], [KH.b])
                    pt, ptb = bank()
                    for c_ in range(4):
                        TR(pt[:, c_ * 128:(c_ + 1) * 128], bb.t[:, c_ * 128:(c_ + 1) * 128], ident, [bb.b, cst.b], [ptb])
                    CP("dve", BH.t[:, c4:c4 + 4, :], c3(pt[:, 0:512]), [ptb], [BH.b])
                order = []
                for si, (s0, slen, _) in enumerate(SEQS):
                    nch = slen // 128
                    cbase = s0 // 128
                    order += [cbase + ci for ci in (range(nch) if d == 0 else range(nch - 1, -1, -1))]
                for gi in range(0, len(order), GSL):
                    g = order[gi:gi + GSL]
                    prep_group(g, d)
                    for c_, sl_ in zip(g, slots):
                        chain_one(c_, sl_, d, hp)
            yc, ysq, yn, t1 = tm["xr"], tm["xk"], tm["xv"], tm["lw"]
            for (t0, n) in BLK5:
                c4 = t0 // 128
                bl = slice(t0, t0 + n)
                yv = YACC.t[:, c4:c4 + 4, :].rearrange("p c (h i) -> p (c h) i", i=64)
                v8 = lambda ap: ap.rearrange("p (g i) -> p g i", i=64)
                RSUM(st1.t[:, :], yv, [YACC.b], [st1.b])
                TS("dve", st1.t[:, :], st1.t[:, :], 1.0 / 64, None, ALU.mult, None, [st1.b], [st1.b])
                TT("dve", v8(yc.t[:, :]), yv, bcast(st1.t[:, :], 2, 64), ALU.subtract, [YACC.b, st1.b], [yc.b])
                TT("pool", ysq.t[:, :], yc.t[:, :], yc.t[:, :], ALU.mult, [yc.b], [ysq.b])
                RSUM(st2.t[:, :], v8(ysq.t[:, :]), [ysq.b], [st2.b])
                TS("dve", st2.t[:, :], st2.t[:, :], 1.0 / 64, 64e-5, ALU.mult, ALU.add, [st2.b], [st2.b])
                ACT(st2.t[:, :], st2.t[:, :], AF.Ln, [st2.b], [st2.b])
                ACT(st2.t[:, :], st2.t[:, :], AF.Exp, [st2.b], [st2.b], scale=-0.5)
                TT("dve", v8(yn.t[:, :]), v8(yc.t[:, :]), bcast(st2.t[:, :], 2, 64), ALU.mult, [yc.b, st2.b], [yn.b])
                pt, ptb = bank()
                for c_ in range(4):
                    TR(pt[:, c_ * 128:(c_ + 1) * 128], yn.t[:, c_ * 128:(c_ + 1) * 128], ident, [yn.b, cst.b], [ptb])
                pg, pgb = bank()
                MM(pg[:, 0:n], GUP.t[:, hcols], SG.t[:, bl], True, True, [GUP.b, SG.b], [pgb])
                ACT(t1.t[:, :], pt[:, 0:n], AF.Identity, [ptb, lnw.b, lnb.b], [t1.b],
                    scale=lnw.t[:, l, hp:hp + 1], bias=lnb.t[:, l, hp:hp + 1])
                TT("pool", t1.t[:, :], t1.t[:, :], BON.t[:, bl], ALU.add, [t1.b, BON.b], [t1.b])
                TT("dve", YAst.t[:, bl], t1.t[:, :], pg[:, 0:n], ALU.mult, [t1.b, pgb], [YAst.b])
            P.dma("sp", ya_s[:, hp, :], YAst.t[:, :], reads=[YAst.b], writes=[ya_sb])

    ya_sb = Buf("ya_s")

    def lru_phase(l, H, Hb, YB, YBb):
        wch = [Tl(f"lwch{i}", [128, 8, 128], BF16) for i in range(4)]
        wrr = [0]

        def next_w(c0):
            tl = wch[wrr[0] % 4]
            wrr[0] += 1
            P.dma("pool", tl.t[:, :, :], win_cols(l, c0), writes=[tl.b])
            return tl
        wrg = [Tl(f"wrg{i}", [128, 2, 2, 128], BF16) for i in range(2)]
        GT = 2048
        Aa = [Tl(f"lA{d}", [128, GT], F32) for d in range(2)]
        Uu = [Tl(f"lU{d}", [128, GT], F32) for d in range(2)]
        HF = Tl("lHF", [128, GT], F32)
        GL = Tl("lGL", [128, GT], F32)
        xc = Tl("lxc", [128, 512], F32)
        xcb = Tl("lxcb", [128, 512], BF16)
        g1 = Tl("lg1", [128, 512], F32)
        rg = Tl("lrg", [128, 512], F32)
        ig = Tl("lig", [128, 512], F32)
        a2 = Tl("la2", [128, 512], F32)
        groups = [(BLK5[0:4], [(0, SEQS[0])], 0, 2048), (BLK5[4:5], [(1, SEQS[1]), (2, SEQS[2])], 2048, 512)]
        for j in range(8):
            jc = slice(j, j + 1)
            wx = next_w(3328 + j * 128)
            wg = next_w(4352 + j * 128)
            wr_ = wrg[j % 2]
            for d in range(2):
                P.dma("pool", wr_.t[:, d, 0, :], Wd["lru_w_rg"][l, d, j], writes=[wr_.b])
                P.dma("pool", wr_.t[:, d, 1, :], Wd["lru_w_ig"][l, d, j], writes=[wr_.b])
            for (gblocks, gseqs, g0, gn) in groups:
                for (t0, n) in gblocks:
                    o = t0 - g0
                    ol = slice(o, o + n)
                    px, pxb = proj(wx.t, wx.b, H, Hb, t0, n)
                    pg_, pgb = proj(wg.t, wg.b, H, Hb, t0, n)
                    ACT(xc.t[:, :], px[:, 0:n], AF.Identity, [pxb, cw.b, cb.b], [xc.b],
                        scale=cw.t[:, 4 * l + 2, jc], bias=cb.t[:, l, jc])
                    xv3 = rowview(xc.t[:, 0:n], t0)
                    pv3 = rowview(px[:, 0:n], t0)
                    STT("dve", xv3[:, :, 2:], pv3[:, :, :-2], cw.t[:, 4 * l + 0, jc], xv3[:, :, 2:], ALU.mult, ALU.add,
                        [pxb, xc.b, cw.b], [xc.b])
                    STT("dve", xv3[:, :, 1:], pv3[:, :, :-1], cw.t[:, 4 * l + 1, jc], xv3[:, :, 1:], ALU.mult, ALU.add,
                        [pxb, xc.b, cw.b], [xc.b])
                    STT("dve", xv3[:, :, :-1], pv3[:, :, 1:], cw.t[:, 4 * l + 3, jc], xv3[:, :, :-1], ALU.mult, ALU.add,
                        [pxb, xc.b, cw.b], [xc.b])
                    CP("pool", xcb.t[:, :], xc.t[:, :], [xc.b], [xcb.b])
                    ACT(g1.t[:, :], pg_[:, 0:n], AF.Square, [pgb], [g1.b])
                    TS("pool", g1.t[:, :], g1.t[:, :], 0.044715, 1.0, ALU.mult, ALU.add, [g1.b], [g1.b])
                    TT("dve", g1.t[:, :], g1.t[:, :], pg_[:, 0:n], ALU.mult, [g1.b, pgb], [g1.b])
                    ACT(g1.t[:, :], g1.t[:, :], AF.Sigmoid, [g1.b], [g1.b], scale=1.5957691216057308)
                    TT("dve", GL.t[:, ol], g1.t[:, :], pg_[:, 0:n], ALU.mult, [g1.b, pgb], [GL.b])
                    for d in range(2):
                        mi = 2 * l + d
                        pr_, prb = bank()
                        MM(pr_[:, 0:n], wr_.t[:, d, 0, :], xcb.t[:, :], True, True, [wr_.b, xcb.b], [prb])
                        pi_, pib = bank()
                        MM(pi_[:, 0:n], wr_.t[:, d, 1, :], xcb.t[:, :], True, True, [wr_.b, xcb.b], [pib])
                        ACT(rg.t[:, :], pr_[:, 0:n], AF.Sigmoid, [prb, brg.b], [rg.b], bias=brg.t[:, mi, jc])
                        ACT(ig.t[:, :], pi_[:, 0:n], AF.Sigmoid, [pib, big.b], [ig.b], bias=big.t[:, mi, jc])
                        ACT(Aa[d].t[:, ol], rg.t[:, :], AF.Exp, [rg.b, c8.b], [Aa[d].b], scale=c8.t[:, mi, jc])
                        TT("pool", a2.t[:, :], Aa[d].t[:, ol], Aa[d].t[:, ol], ALU.mult, [Aa[d].b], [a2.b])
                        ACT(a2.t[:, :], a2.t[:, :], AF.Sqrt, [a2.b], [a2.b], scale=-1.0, bias=1.0)
                        TT("pool", a2.t[:, :], a2.t[:, :], ig.t[:, :], ALU.mult, [a2.b, ig.b], [a2.b])
                        TT("dve", Uu[d].t[:, ol], a2.t[:, :], xc.t[:, :], ALU.mult, [a2.b, xc.b], [Uu[d].b])
                for (si, (s0, slen, _)) in gseqs:
                    o = s0 - g0
                    sl = slice(o, o + slen)
                    i0 = h0l.t[:, 2 * l + 0, jc] if si == 0 else 0.0
                    i1 = h0l.t[:, 2 * l + 1, jc] if si == 0 else 0.0
                    SCAN(HF.t[:, sl], Aa[0].t[:, sl], Uu[0].t[:, sl], i0, [Aa[0].b, Uu[0].b, h0l.b], [HF.b])
                    SCAN(rev_last(Aa[0].t[:, sl]), rev_last(Aa[1].t[:, sl]), rev_last(Uu[1].t[:, sl]), i1,
                         [Aa[1].b, Uu[1].b, h0l.b, HF.b], [Aa[0].b])
                    if si > 0:
                        CP("act", lruo.t[:, si - 1, l, 0, jc], HF.t[:, o + slen - 1:o + slen], [HF.b], [lruo.b])
                        CP("act", lruo.t[:, si - 1, l, 1, jc], Aa[0].t[:, o:o + 1], [Aa[0].b], [lruo.b])
                TT("pool", HF.t[:, 0:gn], HF.t[:, 0:gn], Aa[0].t[:, 0:gn], ALU.add, [HF.b, Aa[0].b], [HF.b])
                TT("dve", YB.t[:, j, g0:g0 + gn], HF.t[:, 0:gn], GL.t[:, 0:gn], ALU.mult, [HF.b, GL.b],
                   [YBb[i] for i in range(g0 // 512, (g0 + gn) // 512)])
        for b in range(2):
            for d in range(2):
                ob = Buf("nsl")
                out_bufs.append(ob)
                P.dma("sp", ns_lru[b, l, d].rearrange("(k p) -> p k", p=128), lruo.t[:, b, l, d, :], reads=[lruo.b],
                      writes=[ob], allow_slow_non_contiguous=True)

    def merge_b(l, H, Hb, YB, YBb):
        WB = Tl("WB", [128, 8, D], BF16)
        WGB = Tl("WGB", [128, 8, D], BF16)
        mtmp = Tl("mtmp", [128, 8, 512], BF16)
        sg = Tl("msg", [128, 512], F32)
        for k in range(8):
            P.dma("pool", WB.t[:, k, :], Wd["w_proj_b"][l, k * 128:(k + 1) * 128, :], writes=[WB.b])
            P.dma("pool", WGB.t[:, k, :], Wd["w_in"][l, k * 128:(k + 1) * 128, 6400:7424], writes=[WGB.b])
        for (t0, n) in BLK5:
            ybb = YBb[t0 // 512]
            for j in range(8):
                js = slice(j * 128, (j + 1) * 128)
                pg_, pgb = proj(WGB.t[:, :, js], WGB.b, H, Hb, t0, n)
                pb_, pbb = proj(WB.t[:, :, js], WB.b, YB, ybb, t0, n)
                ACT(sg.t[:, :], pg_[:, 0:n], AF.Sigmoid, [pgb, bmer.b], [sg.b], bias=bmer.t[:, l, 8 + j:9 + j])
                TT("dve", mtmp.t[:, j, :], sg.t[:, :], pb_[:, 0:n], ALU.mult, [sg.b, pbb], [mtmp.b])
            CP("pool", YB.t[:, :, t0:t0 + n], mtmp.t[:, :, :], [mtmp.b], [ybb])

    def merge_a(l, H, Hb, YB, YBb):
        YA = Tl("YA", [128, 8, T], BF16)
        for k in range(8):
            P.dma("sp", YA.t[:, k, :], ya_s[:, k, :], reads=[ya_sb], writes=[YA.b])
        wch = [Tl(f"mwch{i}", [128, 8, 128], BF16) for i in range(4)]
        sg = Tl("msg2", [128, 512], F32)
        tt_ = Tl("mtt", [128, 512], F32)
        for j in range(8):
            wa = wch[(2 * j) % 4]
            wga = wch[(2 * j + 1) % 4]
            P.dma("pool", wa.t[:, :, :], Wd["w_proj_a"][l].rearrange("(k p) c -> p k c", p=128)[:, :, j * 128:(j + 1) * 128],
                  writes=[wa.b])
            P.dma("pool", wga.t[:, :, :], win_cols(l, 5376 + j * 128), writes=[wga.b])
            for (t0, n) in BLK5:
                ybb = YBb[t0 // 512]
                pg_, pgb = proj(wga.t, wga.b, H, Hb, t0, n)
                pa_, pab = proj(wa.t, wa.b, YA, YA.b, t0, n)
                ACT(sg.t[:, :], pg_[:, 0:n], AF.Sigmoid, [pgb, bmer.b], [sg.b], bias=bmer.t[:, l, j:j + 1])
                TT("dve", tt_.t[:, :], sg.t[:, :], pa_[:, 0:n], ALU.mult, [sg.b, pab], [tt_.b])
                TT("pool", YB.t[:, j, t0:t0 + n], tt_.t[:, :], YB.t[:, j, t0:t0 + n], ALU.add, [tt_.b, ybb], [ybb])

    def out_proj(l, YB, YBb):
        WO = Tl("WO", [128, 8, D], BF16)
        for k in range(8):
            P.dma("pool", WO.t[:, k, :], Wd["w_out"][l, k * 128:(k + 1) * 128, :], writes=[WO.b])
        O = Tl("O", [128, 8, 512], F32)
        rt = res_tmps(False)
        for (t0, n) in BLK5:
            for j in range(8):
                po, pob = proj(WO.t[:, :, j * 128:(j + 1) * 128], WO.b, YB, YBb[t0 // 512], t0, n)
                CP("act" if j % 2 else "dve", O.t[:, j, 0:n], po[:, 0:n], [pob], [O.b])
            residual_update(l, O.t[:, :, 0:n], O.b, G1, t0, n, False, rt)

    def ffn_phase(l):
        moe = (l % 2 == 1)
        final = (l == L - 1)
        if not moe:
            i = l // 2
            groups = [(Wd["ffn_w1"][i][:, g * 2048:(g + 1) * 2048], Wd["ffn_w3"][i][:, g * 2048:(g + 1) * 2048],
                       Wd["ffn_w2"][i][g * 2048:(g + 1) * 2048, :], 16, None) for g in range(2)]
        else:
            i = l // 2
            groups = [(Wd["moe_w1"][i, e], Wd["moe_w3"][i, e], Wd["moe_w2"][i, e], 28, e) for e in range(8)]
        halves = [BLK5]
        HT = T
        FG = 4
        for hf, blocks in enumerate(halves):
            hoff = hf * HT
            with P.scope():
                H2 = Tl("H2", [128, 8, HT], BF16)
                GTt = Tl("GTt", [32, HT], F32)
                MSET("pool", GTt.t[:, :], 0.0, [GTt.b])
                GB = Tl("GB", [128, HT], F32)
                with P.scope():
                    tmp = norm_tmps()
                    if moe:
                        hf32 = Tl("hf32", [128, 8, 512], F32)
                        RT = Tl("RT", [128, 8, 8], F32)
                        P.dma("sp", RT.t[:, :, :], Wd["moe_router"][i].rearrange("(k p) e -> p k e", p=128), writes=[RT.b])
                        lg = Tl("lg", [128, 8], F32)
                        lg2 = Tl("lg2", [128, 8], F32)
                        m1 = Tl("m1", [128, 1], F32)
                        m2 = Tl("m2", [128, 1], F32)

                        def gates(t0, n):
                            for tt in range(n // 128):
                                ts_ = slice(tt * 128, (tt + 1) * 128)
                                pl, plb = bank()
                                for k in range(8):
                                    MM(pl[:, 0:8], hf32.t[:, k, ts_], RT.t[:, k, :], k == 0, k == 7, [hf32.b, RT.b], [plb])
                                CP("act", lg.t[:, :], pl[:, 0:8], [plb], [lg.b])
                                RMAX(m1.t[:, :], lg.t[:, :], [lg.b], [m1.b])
                                TS("dve", lg2.t[:, :], lg.t[:, :], m1.t[:, 0:1], None, ALU.is_equal, None, [lg.b, m1.b], [lg2.b])
                                STT("dve", lg2.t[:, :], lg2.t[:, :], -1e30, lg.t[:, :], ALU.mult, ALU.add, [lg2.b, lg.b], [lg2.b])
                                RMAX(m2.t[:, :], lg2.t[:, :], [lg2.b], [m2.b])
                                TS("dve", lg2.t[:, :], lg.t[:, :], m2.t[:, 0:1], None, ALU.is_ge, None, [lg.b, m2.b], [lg2.b])
                                TS("dve", m1.t[:, :], m1.t[:, :], -1.0, None, ALU.mult, None, [m1.b], [m1.b])
                                ACT(lg.t[:, :], lg.t[:, :], AF.Exp, [lg.b, m1.b], [lg.b], bias=m1.t[:, 0:1])
                                TT("dve", lg.t[:, :], lg.t[:, :], lg2.t[:, :], ALU.mult, [lg.b, lg2.b], [lg.b])
                                RSUM(m2.t[:, :], lg.t[:, :], [lg.b], [m2.b])
                                P.op("dve", lambda e: e.reciprocal(m2.t[:, :], m2.t[:, :]), [m2.b], [m2.b])
                                TS("dve", lg.t[:, :], lg.t[:, :], m2.t[:, 0:1], None, ALU.mult, None, [lg.b, m2.b], [lg.b])
                                pt, ptb = bank()
                                TR(pt[0:8, 0:128], lg.t[:, :], ident, [lg.b, cst.b], [ptb])
                                o = t0 - hoff + tt * 128
                                CP("act", GTt.t[0:8, o:o + 128], pt[0:8, 0:128], [ptb], [GTt.b])
                        norm_mod(l, A2, B2, H2, H2.b, blocks, hoff, tmp, hf32=hf32, cbk=gates)
                    else:
                        norm_mod(l, A2, B2, H2, H2.b, blocks, hoff, tmp)
                OACC = Tl("OACC", [128, 8, HT], F32)
                with P.scope():
                    HID = Tl("HID", [128, FG, HT], BF16)
                    w13 = [Tl(f"w13_{i_}", [128, 8, 128], BF16) for i_ in range(4)]
                    w2g = [Tl(f"w2g{i_}", [128, FG, D], BF16) for i_ in range(2)]
                    sil = [Tl(f"sil{i_}", [128, 512], F32) for i_ in range(2)]
                    wc = [0]
                    ng = [0]
                    for gi, (w1, w3, w2, nf, ex) in enumerate(groups):
                        if ex is not None:
                            for (t0, n) in blocks:
                                o = t0 - hoff
                                pg, pgb = bank()
                                MM(pg[:, 0:n], cst.t[0:8, C_ES + ex * 128:C_ES + (ex + 1) * 128], GTt.t[0:8, o:o + n], True, True,
                                   [cst.b, GTt.b], [pgb])
                                CP("act", GB.t[:, o:o + n], pg[:, 0:n], [pgb], [GB.b])
                        w1v = w1.rearrange("(k p) c -> p k c", p=128)
                        w3v = w3.rearrange("(k p) c -> p k c", p=128)
                        w2v = w2.rearrange("(f p) c -> p f c", p=128)
                        for fg in range(nf // FG):
                            wt2 = w2g[ng[0] % 2]
                            P.dma("pool", wt2.t[:, :, :], w2v[:, fg * FG:(fg + 1) * FG, :], writes=[wt2.b])
                            for fl in range(FG):
                                f = fg * FG + fl
                                fs = slice(f * 128, (f + 1) * 128)
                                w1t = w13[wc[0] % 4]
                                w3t = w13[(wc[0] + 1) % 4]
                                wc[0] += 2
                                P.dma("pool", w1t.t[:, :, :], w1v[:, :, fs], writes=[w1t.b])
                                P.dma("pool", w3t.t[:, :, :], w3v[:, :, fs], writes=[w3t.b])
                                for bi, (t0, n) in enumerate(blocks):
                                    o = t0 - hoff
                                    s_ = sil[(f + bi) % 2]
                                    p1, p1b = proj(w1t.t, w1t.b, H2, H2.b, t0, n, hoff)
                                    p3, p3b = proj(w3t.t, w3t.b, H2, H2.b, t0, n, hoff)
                                    ACT(s_.t[:, 0:n], p1[:, 0:n], AF.Silu, [p1b], [s_.b])
                                    if ex is None:
                                        TT("dve", HID.t[:, fl, o:o + n], s_.t[:, 0:n], p3[:, 0:n], ALU.mult, [s_.b, p3b], [HID.b])
                                    else:
                                        TT("dve", s_.t[:, 0:n], s_.t[:, 0:n], p3[:, 0:n], ALU.mult, [s_.b, p3b], [s_.b])
                                        TT("pool", HID.t[:, fl, o:o + n], s_.t[:, 0:n], GB.t[:, o:o + n], ALU.mult, [s_.b, GB.b], [HID.b])
                            for j in range(8):
                                for (t0, n) in blocks:
                                    o = t0 - hoff
                                    po, pob = bank()
                                    for fl in range(FG):
                                        MM(po[:, 0:n], wt2.t[:, fl, j * 128:(j + 1) * 128], HID.t[:, fl, o:o + n], fl == 0, fl == FG - 1,
                                           [wt2.b, HID.b], [pob])
                                    if ng[0] == 0:
                                        CP("act", OACC.t[:, j, o:o + n], po[:, 0:n], [pob], [OACC.b])
                                    else:
                                        TT("dve", OACC.t[:, j, o:o + n], po[:, 0:n], OACC.t[:, j, o:o + n], ALU.add, [pob, OACC.b], [OACC.b])
                            ng[0] += 1
                with P.scope():
                    rt = res_tmps(final)
                    for (t0, n) in blocks:
                        o = t0 - hoff
                        residual_update(l, OACC.t[:, :, o:o + n], OACC.b, G2, t0, n, final, rt)

    for l in range(L):
        with P.scope():
            YB = Tl("YB", [128, 8, T], BF16)
            YBb = [Buf(f"YB{i}") for i in range(NBLK)]
            with P.scope():
                H = Tl("H", [128, 8, T], BF16)
                Hb = [Buf(f"H{i}") for i in range(NBLK)]
                P.mark(f"L{l} start")
                with P.scope():
                    norm_mod(l, A1, B1, H, Hb, BLK5, 0, norm_tmps())
                P.mark(f"L{l} norm1 done")
                with P.scope():
                    rwkv_phase(l, H, Hb, YB)
                P.mark(f"L{l} rwkv done")
                with P.scope():
                    lru_phase(l, H, Hb, YB, YBb)
                P.mark(f"L{l} lru done")
                with P.scope():
                    merge_b(l, H, Hb, YB, YBb)
                P.mark(f"L{l} merge_b done")
                with P.scope():
                    merge_a(l, H, Hb, YB, YBb)
                P.mark(f"L{l} merge_a done")
            with P.scope():
                out_proj(l, YB, YBb)
            P.mark(f"L{l} out_proj done")
        with P.scope():
            ffn_phase(l)
        P.mark(f"L{l} ffn done")

    P.barrier()
    P.emit()
    P.close()
    nc._marks = getattr(P, "marks", [])
    return nc


_CACHE = {}


def kernel(**inputs):
    inp = {k: np.ascontiguousarray(np.asarray(v)) for k, v in inputs.items()}
    if "nc" not in _CACHE:
        _CACHE["nc"] = build_program()
    nc = _CACHE["nc"]
    cst = make_consts()
    in_maps = []
    for c in range(NCORES):
        m = {}
        m["xin"] = np.ascontiguousarray(np.concatenate(
            [inp["x_sample"][c], inp["x_prompt"][2 * c], inp["x_prompt"][2 * c + 1]], axis=0))
        m["cond"] = np.ascontiguousarray(np.stack([inp["c"][c], inp["c_ctx"]], axis=0))
        m["st_rwkv"] = np.ascontiguousarray(inp["state_rwkv"][c])
        m["st_lru"] = np.ascontiguousarray(inp["state_lru"][c])
        for n_, _s in WEIGHT_SHAPES:
            m[n_] = inp[n_]
        m["cst"] = cst
        in_maps.append(m)
    res = run_bass_kernel_spmd(nc, in_maps, core_ids=list(range(NCORES)))
    R = res.results
    y_sample = np.stack([R[c]["y"][0:2048] for c in range(NCORES)], axis=0)
    y_prompt = np.concatenate([R[c]["y"][2048:2560].reshape(2, 256, D) for c in range(NCORES)], axis=0)
    ns_rwkv = np.concatenate([R[c]["ns_rwkv"] for c in range(NCORES)], axis=0)
    ns_lru = np.concatenate([R[c]["ns_lru"] for c in range(NCORES)], axis=0)
    return (y_prompt.astype(np.float32), y_sample.astype(np.float32), ns_rwkv.astype(np.float32), ns_lru.astype(np.float32))
```
